# Optimizing a Trainium2 kernel written in Bass

```python
import jax
import jax.numpy as jnp
from jax import lax
import numpy as np

D_MODEL = 1024
BATCH = 16
SEQ = 2048
DEPTH = 2

GRID_W = 64
CTX_LEN = 256
N_MOD = 6
EPS = 1e-6
NEG_INF = -1e30

HEAD_DIM = 64
NA_WIDTH = D_MODEL // 2
NA_HEADS = NA_WIDTH // HEAD_DIM
LRU_WIDTH = D_MODEL // 4
LRU_HEADS = 4
LRU_BLOCK = LRU_WIDTH // LRU_HEADS
SC_WIDTH = D_MODEL - NA_WIDTH - LRU_WIDTH

COL_Q = 0
COL_K = COL_Q + NA_WIDTH
COL_V = COL_K + NA_WIDTH
COL_RX = COL_V + NA_WIDTH
COL_RG = COL_RX + LRU_WIDTH
COL_SB = COL_RG + LRU_WIDTH
COL_SC = COL_SB + SC_WIDTH
COL_SX = COL_SC + SC_WIDTH
IN_WIDTH = COL_SX + SC_WIDTH

LRU_CONV = 4
LRU_CONV_LEFT = 2
RG_C = 8.0
SC_CONV = 3
SC_CONV_LEFT = 1
WIN_R = 8
WIN_C = 16
Q_COLS = 16
K_COLS = 32
N_EXPERTS = 16
N_GROUPS = 4
E_PER_GROUP = N_EXPERTS // N_GROUPS
TOP_K = 2
D_EXPERT = 512

kernel_name = 'hybrid_na_rglru_shortconv_moe_dit'


def rms_norm(x, g):
    xf = x.astype(jnp.float32)
    y = xf * lax.rsqrt(jnp.mean(xf * xf, axis=-1, keepdims=True) + EPS)
    return (y * g.astype(jnp.float32)).astype(x.dtype)


def modulate(x, shift, scale):
    return x * (1.0 + scale) + shift


def dwconv(x, w, b, left):
    width, length = w.shape[0], x.shape[1]
    xp = jnp.pad(x, ((0, 0), (left, width - 1 - left), (0, 0)))
    y = xp[:, :length] * w[0] + b
    for k in range(1, width):
        y = y + xp[:, k:k + length] * w[k]
    return y


def _affine_combine(left, right):
    a_l, h_l = left
    a_r, h_r = right
    return a_l * a_r, a_r * h_l + h_r


def linear_scan(a, b, h0, reverse):
    if reverse:
        a, b = jnp.flip(a, 1), jnp.flip(b, 1)
    b = b.at[:, 0].add(a[:, 0] * h0)
    _, h = lax.associative_scan(_affine_combine, (a, b), axis=1)
    return jnp.flip(h, 1) if reverse else h


def rglru_coeffs(xc, w, bias, lam):
    bsz, length, _ = xc.shape
    xb = xc.reshape(bsz, length, LRU_HEADS, LRU_BLOCK)
    pre = jnp.einsum('blnc,gncd->gblnd', xb, w.astype(jnp.float32)).reshape(2, bsz, length, LRU_WIDTH)
    gates = jax.nn.sigmoid(pre + bias.astype(jnp.float32)[:, None, None, :])
    r, i = gates[0], gates[1]
    log_a = -RG_C * r * jax.nn.softplus(-lam.astype(jnp.float32))
    a = jnp.exp(log_a)
    b = jnp.sqrt(-jnp.expm1(2.0 * log_a)) * (i * xc)
    return a, b


def rglru_bidir(x_lat, x_ctx, w, bias, lam, need_ctx):
    y_lat = jnp.zeros_like(x_lat)
    y_ctx = jnp.zeros_like(x_ctx) if need_ctx else None
    for d, rev in enumerate((False, True)):
        a_c, b_c = rglru_coeffs(x_ctx, w[d], bias[d], lam[d])
        h_c = linear_scan(a_c, b_c, jnp.zeros_like(x_ctx[:, 0]), rev)
        a_l, b_l = rglru_coeffs(x_lat, w[d], bias[d], lam[d])
        y_lat = y_lat + linear_scan(a_l, b_l, h_c[:, 0] if rev else h_c[:, -1], rev)
        if need_ctx:
            y_ctx = y_ctx + h_c
    return y_lat, y_ctx


def neighbourhood_attention(q, k, v, kc, vc, rpb):
    bsz, seq, nh, dh = q.shape
    rows = seq // GRID_W
    wr = min(WIN_R, rows)
    ncb = GRID_W // Q_COLS
    scale = dh ** -0.5
    qg = q.reshape(bsz, rows, ncb, Q_COLS, nh, dh)
    kg = k.reshape(bsz, rows, GRID_W, nh, dh)
    vg = v.reshape(bsz, rows, GRID_W, nh, dh)
    q_col = jnp.arange(GRID_W).reshape(ncb, Q_COLS)
    c_start = jnp.clip(q_col - WIN_C // 2, 0, GRID_W - WIN_C)
    k_col = jnp.clip(jnp.arange(ncb) * Q_COLS - WIN_C // 2, 0, GRID_W - K_COLS)[:, None] + jnp.arange(K_COLS)
    col_ok = (k_col[:, None, :] >= c_start[..., None]) & (k_col[:, None, :] < c_start[..., None] + WIN_C)
    rel_c = jnp.clip(k_col[:, None, :] - q_col[..., None] + WIN_C - 1, 0, 2 * WIN_C - 2)
    bias_c = rpb[:, :, rel_c]
    n_loc = wr * K_COLS

    def row_block(r):
        r0 = jnp.clip(r - wr // 2, 0, rows - wr)
        kb = lax.dynamic_slice_in_dim(kg, r0, wr, axis=1)[:, :, k_col]
        vb = lax.dynamic_slice_in_dim(vg, r0, wr, axis=1)[:, :, k_col]
        qr = lax.dynamic_index_in_dim(qg, r, axis=1, keepdims=False)
        bias = jnp.take(bias_c, r0 + jnp.arange(wr) - r + WIN_R - 1, axis=1)
        s_loc = jnp.einsum('bnqhd,brnkhd->bhnqrk', qr, kb, preferred_element_type=jnp.float32) * scale
        s_loc = jnp.where(col_ok[:, :, None, :], s_loc + bias.transpose(0, 2, 3, 1, 4), NEG_INF)
        s_ctx = jnp.einsum('bnqhd,bchd->bhnqc', qr, kc, preferred_element_type=jnp.float32) * scale
        s = jnp.concatenate([s_loc.reshape(bsz, nh, ncb, Q_COLS, n_loc), s_ctx], axis=-1)
        p = jax.nn.softmax(s, axis=-1).astype(v.dtype)
        p_loc = p[..., :n_loc].reshape(bsz, nh, ncb, Q_COLS, wr, K_COLS)
        return (jnp.einsum('bhnqrk,brnkhd->bnqhd', p_loc, vb)
                + jnp.einsum('bhnqc,bchd->bnqhd', p[..., n_loc:], vc))

    out = lax.map(row_block, jnp.arange(rows))
    return jnp.moveaxis(out, 0, 1).reshape(bsz, seq, nh, dh)


def context_attention(qc, kc, vc):
    s = jnp.einsum('bqhd,bkhd->bhqk', qc, kc, preferred_element_type=jnp.float32) * (HEAD_DIM ** -0.5)
    p = jax.nn.softmax(s, axis=-1).astype(vc.dtype)
    return jnp.einsum('bhqk,bkhd->bqhd', p, vc)


def merge_groups(y_na, y_lru, y_sc, g):
    return jnp.concatenate([
        rms_norm(y_na, g[:NA_WIDTH]),
        rms_norm(y_lru, g[NA_WIDTH:NA_WIDTH + LRU_WIDTH]),
        rms_norm(y_sc, g[NA_WIDTH + LRU_WIDTH:])], axis=-1)


def hybrid_mixer(h, hc, w_in, lru_conv_w, lru_conv_b, rg_w, rg_b, rg_lam, rpb,
                 sc_conv_w, sc_conv_b, out_g, w_out, need_ctx):
    bsz, seq, _ = h.shape
    n_ctx = hc.shape[1]
    u = h @ w_in
    c_lo, c_hi = (0, IN_WIDTH) if need_ctx else (COL_K, COL_RG)
    uc = hc @ w_in[:, c_lo:c_hi]

    def lat(lo, hi):
        return u[..., lo:hi]

    def cx(lo, hi):
        return uc[..., lo - c_lo:hi - c_lo]

    def heads(t):
        return t.reshape(t.shape[0], t.shape[1], NA_HEADS, HEAD_DIM)

    kc, vc = heads(cx(COL_K, COL_V)), heads(cx(COL_V, COL_RX))
    y_na = neighbourhood_attention(heads(lat(COL_Q, COL_K)), heads(lat(COL_K, COL_V)),
                                   heads(lat(COL_V, COL_RX)), kc, vc, rpb).reshape(bsz, seq, NA_WIDTH)
    rx = dwconv(lat(COL_RX, COL_RG), lru_conv_w, lru_conv_b, LRU_CONV_LEFT).astype(jnp.float32)
    rxc = dwconv(cx(COL_RX, COL_RG), lru_conv_w, lru_conv_b, LRU_CONV_LEFT).astype(jnp.float32)
    h_lru, h_lru_c = rglru_bidir(rx, rxc, rg_w, rg_b, rg_lam, need_ctx)
    y_lru = h_lru.astype(h.dtype) * jax.nn.gelu(lat(COL_RG, COL_SB))
    y_sc = lat(COL_SB, COL_SC) * dwconv(lat(COL_SC, COL_SX) * lat(COL_SX, IN_WIDTH), sc_conv_w, sc_conv_b, SC_CONV_LEFT)
    y = merge_groups(y_na, y_lru, y_sc, out_g) @ w_out
    if not need_ctx:
        return y, None
    y_na_c = context_attention(heads(cx(COL_Q, COL_K)), kc, vc).reshape(bsz, n_ctx, NA_WIDTH)
    y_lru_c = h_lru_c.astype(hc.dtype) * jax.nn.gelu(cx(COL_RG, COL_SB))
    y_sc_c = cx(COL_SB, COL_SC) * dwconv(cx(COL_SC, COL_SX) * cx(COL_SX, IN_WIDTH), sc_conv_w, sc_conv_b, SC_CONV_LEFT)
    yc = merge_groups(y_na_c, y_lru_c, y_sc_c, out_g) @ w_out
    return y, yc


def moe_ffn(h, w_router, b_router, w_gate, w_up, w_down):
    logits = jnp.einsum('btd,de->bte', h, w_router, preferred_element_type=jnp.float32) + b_router.astype(jnp.float32)
    probs = jax.nn.softmax(logits, axis=-1)
    pg = probs.reshape(*probs.shape[:-1], N_GROUPS, E_PER_GROUP)
    group_score = jnp.sum(lax.top_k(pg, TOP_K)[0], axis=-1)
    g_sel = jnp.argmax(group_score, axis=-1)
    p_in = jnp.take_along_axis(pg, g_sel[..., None, None], axis=-2)[..., 0, :]
    w_top, e_loc = lax.top_k(p_in, TOP_K)
    w_top = w_top / jnp.sum(w_top, axis=-1, keepdims=True)
    e_idx = g_sel[..., None] * E_PER_GROUP + e_loc
    combine = jnp.sum(jax.nn.one_hot(e_idx, N_EXPERTS, dtype=jnp.float32) * w_top[..., None], axis=-2).astype(h.dtype)
    out = jnp.zeros_like(h)
    for e in range(N_EXPERTS):
        hid = jax.nn.silu(h @ w_gate[e]) * (h @ w_up[e])
        out = out + combine[..., e:e + 1] * (hid @ w_down[e])
    return out


def setup_inputs(seed: int = 0) -> dict:
    key = jax.random.key(seed)
    ks = jax.random.split(key, 25)
    L = DEPTH

    def nrm(k, shape, fan_in, gain=1.0):
        return jax.random.normal(k, shape, jnp.float32) * (gain * fan_in ** -0.5)

    def small(k, shape):
        return 0.01 * jax.random.normal(k, shape, jnp.float32)

    def gain(k, shape):
        return 1.0 + 0.02 * jax.random.normal(k, shape, jnp.float32)

    a_pow = jax.random.uniform(ks[12], (L, 2, LRU_WIDTH), jnp.float32, 0.9, 0.999)
    a_base = a_pow ** (1.0 / RG_C)
    return {
        'x': jax.random.normal(ks[0], (BATCH, SEQ, D_MODEL), jnp.float32),
        'c': jax.random.normal(ks[1], (BATCH, D_MODEL), jnp.float32),
        'ctx': jax.random.normal(ks[2], (BATCH, CTX_LEN, D_MODEL), jnp.float32),
        'c_ctx': jax.random.normal(ks[3], (D_MODEL,), jnp.float32),
        'w_ada': nrm(ks[4], (L, D_MODEL, N_MOD * D_MODEL), D_MODEL, 0.5),
        'b_ada': small(ks[5], (L, N_MOD * D_MODEL)),
        'norm_mix_g': gain(ks[6], (L, D_MODEL)),
        'w_in': nrm(ks[7], (L, D_MODEL, IN_WIDTH), D_MODEL),
        'lru_conv_w': nrm(ks[8], (L, LRU_CONV, LRU_WIDTH), LRU_CONV),
        'lru_conv_b': small(ks[9], (L, LRU_WIDTH)),
        'rg_w': nrm(ks[10], (L, 2, 2, LRU_HEADS, LRU_BLOCK, LRU_BLOCK), LRU_BLOCK),
        'rg_b': small(ks[11], (L, 2, 2, LRU_WIDTH)),
        'rg_lam': jnp.log(a_base) - jnp.log1p(-a_base),
        'na_rpb': 0.02 * jax.random.normal(ks[13], (L, NA_HEADS, 2 * WIN_R - 1, 2 * WIN_C - 1), jnp.float32),
        'sc_conv_w': nrm(ks[14], (L, SC_CONV, SC_WIDTH), SC_CONV),
        'sc_conv_b': small(ks[15], (L, SC_WIDTH)),
        'mix_out_g': gain(ks[16], (L, D_MODEL)),
        'w_out': nrm(ks[17], (L, D_MODEL, D_MODEL), D_MODEL),
        'norm_ffn_g': gain(ks[18], (L, D_MODEL)),
        'w_router': nrm(ks[19], (D_MODEL, N_EXPERTS), D_MODEL),
        'b_router': small(ks[20], (N_EXPERTS,)),
        'w_gate': nrm(ks[21], (L, N_EXPERTS, D_MODEL, D_EXPERT), D_MODEL),
        'w_up': nrm(ks[22], (L, N_EXPERTS, D_MODEL, D_EXPERT), D_MODEL),
        'w_down': nrm(ks[23], (L, N_EXPERTS, D_EXPERT, D_MODEL), D_EXPERT),
        'final_g': gain(ks[24], (D_MODEL,)),
    }


def reference(x, c, ctx, c_ctx, w_ada, b_ada, norm_mix_g, w_in, lru_conv_w, lru_conv_b,
              rg_w, rg_b, rg_lam, na_rpb, sc_conv_w, sc_conv_b, mix_out_g, w_out,
              norm_ffn_g, w_router, b_router, w_gate, w_up, w_down, final_g):
    bsz = x.shape[0]
    n_ctx = ctx.shape[1]
    cond = jax.nn.silu(c)
    cond_ctx = jax.nn.silu(c_ctx)
    xc = ctx
    for l in range(DEPTH):
        need_ctx = l < DEPTH - 1
        mod = (cond @ w_ada[l] + b_ada[l]).reshape(bsz, N_MOD, 1, D_MODEL)
        mod_c = (cond_ctx @ w_ada[l] + b_ada[l]).reshape(N_MOD, 1, D_MODEL)
        h = modulate(rms_norm(x, norm_mix_g[l]), mod[:, 0], mod[:, 1])
        hc = modulate(rms_norm(xc, norm_mix_g[l]), mod_c[0], mod_c[1])
        y, yc = hybrid_mixer(h, hc, w_in[l], lru_conv_w[l], lru_conv_b[l], rg_w[l], rg_b[l], rg_lam[l],
                             na_rpb[l], sc_conv_w[l], sc_conv_b[l], mix_out_g[l], w_out[l], need_ctx)
        x = x + mod[:, 2] * y
        h = modulate(rms_norm(x, norm_ffn_g[l]), mod[:, 3], mod[:, 4])
        if need_ctx:
            xc = xc + mod_c[2] * yc
            hc = modulate(rms_norm(xc, norm_ffn_g[l]), mod_c[3], mod_c[4])
            f = moe_ffn(jnp.concatenate([hc, h], axis=1), w_router, b_router, w_gate[l], w_up[l], w_down[l])
            xc = xc + mod_c[5] * f[:, :n_ctx]
            x = x + mod[:, 5] * f[:, n_ctx:]
        else:
            x = x + mod[:, 5] * moe_ffn(h, w_router, b_router, w_gate[l], w_up[l], w_down[l])
    return rms_norm(x, final_g)
```

```python
import numpy as np
from contextlib import ExitStack
import concourse.bass as bass
import concourse.mybir as mybir
from concourse.bass_utils import run_bass_kernel_spmd

F32 = mybir.dt.float32
BF16 = mybir.dt.bfloat16
AF = mybir.ActivationFunctionType
ALU = mybir.AluOpType

N_CORES = 8
D = 1024
KC = 8
SEQ = 2048
CTX = 256
T = CTX + SEQ
DEPTH = 2
IN_W = 2816
COL_Q, COL_K, COL_V, COL_RX, COL_RG, COL_SB, COL_SC, COL_SX = 0, 512, 1024, 1536, 1792, 2048, 2304, 2560
N_EXP = 16
D_EXP = 512
EPS = 1e-6
MASK = -30000.0
GRID_W = 64
ROWS = 32
WIN_R = 8
WIN_C = 16

GROUPS_ALL = [(0, 256), (256, 768), (768, 1280), (1280, 1792), (1792, 2304)]
GROUPS_LAT = GROUPS_ALL[1:]


def _r0(r):
    return min(max(r - WIN_R // 2, 0), ROWS - WIN_R)


def _na_structure():
    pats = {}
    plist = []
    per_tile = []
    for i in range(16):
        rows = set()
        for r in (2 * i, 2 * i + 1):
            rows.update(range(_r0(r), _r0(r) + WIN_R))
        tiles = sorted({kr // 2 for kr in rows})
        ent = []
        for j in tiles:
            key = []
            for a in (0, 1):
                for cq in (0, 1):
                    kr, r = 2 * j + a, 2 * i + cq
                    key.append(kr - r if _r0(r) <= kr < _r0(r) + WIN_R else None)
            key = tuple(key)
            if key not in pats:
                pats[key] = len(plist)
                plist.append(key)
            ent.append((j, pats[key]))
        per_tile.append(ent)
    return per_tile, plist


NA_TILES, NA_PATS = _na_structure()
NPAT = len(NA_PATS)


def _na_slots():
    import itertools
    seqs = []
    for ent in NA_TILES:
        sq = tuple(p for (_, p) in ent)
        if sq not in seqs:
            seqs.append(sq)

    def find(hay, needle):
        for i in range(len(hay) - len(needle) + 1):
            if tuple(hay[i:i + len(needle)]) == tuple(needle):
                return i
        return -1

    best = None
    for perm in itertools.permutations(seqs):
        cur = []
        for sq in perm:
            if find(cur, sq) >= 0:
                continue
            ov = 0
            for k in range(min(len(cur), len(sq)), 0, -1):
                if tuple(cur[-k:]) == tuple(sq[:k]):
                    ov = k
                    break
            cur = cur + list(sq[ov:])
        if best is None or len(cur) < len(best):
            best = cur
    starts = [find(best, tuple(p for (_, p) in ent)) for ent in NA_TILES]
    return best, starts


NA_SLOTS, NA_START = _na_slots()
NSLOT = len(NA_SLOTS)


def _build_bias_patterns(rpb):
    L, H = rpb.shape[0], rpb.shape[1]
    kc = np.arange(GRID_W)[:, None]
    qc = np.arange(GRID_W)[None, :]
    c_start = np.clip(qc - WIN_C // 2, 0, GRID_W - WIN_C)
    ok = (kc >= c_start) & (kc < c_start + WIN_C)
    rel_c = np.clip(kc - qc + WIN_C - 1, 0, 2 * WIN_C - 2)
    out = np.full((L, H, 128, NSLOT, 128), MASK, np.float32)
    for p, pid in enumerate(NA_SLOTS):
        key = NA_PATS[pid]
        idx = 0
        for a in (0, 1):
            for cq in (0, 1):
                dr = key[idx]
                idx += 1
                if dr is None:
                    continue
                blk = np.where(ok[None, None], rpb[:, :, dr + WIN_R - 1][:, :, rel_c], np.float32(MASK))
                out[:, :, 64 * a:64 * a + 64, p, 64 * cq:64 * cq + 64] = blk
    return out


class PP:
    def __init__(self):
        self.off = {}
        self.n = 0
        self.parts = []

    def add(self, name, arr):
        arr = np.ascontiguousarray(arr, dtype=np.float32).reshape(128, -1)
        self.off[name] = (self.n, arr.shape[1])
        self.n += arr.shape[1]
        self.parts.append(arr)

    def pack(self):
        return np.ascontiguousarray(np.concatenate(self.parts, axis=1))


def _chunked(v):
    v = np.asarray(v, np.float32)
    lead = v.shape[:-1]
    n = v.shape[-1] // 128
    v = v.reshape(*lead, n, 128)
    return np.moveaxis(v, -1, 0)


def _pack_params(inp):
    pp = PP()
    pp.add("b_ada", _chunked(inp["b_ada"]))
    pp.add("g_mix", _chunked(inp["norm_mix_g"]))
    pp.add("g_out", _chunked(inp["mix_out_g"]))
    pp.add("g_ffn", _chunked(inp["norm_ffn_g"]))
    pp.add("g_fin", _chunked(inp["final_g"]))
    pp.add("lconv_w", _chunked(inp["lru_conv_w"]))
    pp.add("lconv_b", _chunked(inp["lru_conv_b"]))
    pp.add("rg_b", _chunked(inp["rg_b"]))
    pp.add("rg_lam", _chunked(inp["rg_lam"]))
    pp.add("sconv_w", _chunked(inp["sc_conv_w"]))
    pp.add("sconv_b", _chunked(inp["sc_conv_b"]))
    return pp


def _wbd(rg_w):
    L = rg_w.shape[0]
    out = np.zeros((L, 2, 2, 2, 128, 128), np.float32)
    for c in range(2):
        for a in range(2):
            out[:, :, :, c, 64 * a:64 * a + 64, 64 * a:64 * a + 64] = rg_w[:, :, :, 2 * c + a]
    return out


class Buf:
    def __init__(self, name, t):
        self.name = name
        self.t = t
        self.all_w = None
        self.all_r = {}
        self.reg = {}
        self.sem = None
        self.semv = 0

    def __getitem__(self, idx):
        return self.t[idx]


class Sched:
    def __init__(self, nc, es):
        self.nc = nc
        self.es = es
        self.eng = {"pe": nc.tensor, "act": nc.scalar, "dve": nc.vector, "pool": nc.gpsimd, "sp": nc.sync}
        self.sem = {e: es.enter_context(nc.semaphore("s_" + e)) for e in self.eng}
        self.cnt = {e: 0 for e in self.eng}
        self.waited = {e: {} for e in self.eng}
        self.dsem = {}
        self.nbuf = 0

    def sb(self, es, name, shape, dt=F32):
        self.nbuf += 1
        return Buf(name, es.enter_context(self.nc.sbuf_tensor(f"{name}_{self.nbuf}", list(shape), dt)))

    def ps(self, es, name, shape, dt=F32):
        self.nbuf += 1
        return Buf(name, es.enter_context(self.nc.psum_tensor(f"{name}_{self.nbuf}", list(shape), dt)))

    def wrap(self, name, t):
        return Buf(name, t)

    @staticmethod
    def _norm(items):
        out = []
        for it in items:
            if isinstance(it, Buf):
                out.append((it, None))
            else:
                out.append(it)
        return out

    def _deps(self, reads, writes):
        raw, deps = [], []
        for b, k in reads:
            if b.all_w:
                raw.append(b.all_w)
            if k is None:
                for st in b.reg.values():
                    if st[0]:
                        raw.append(st[0])
            elif k in b.reg and b.reg[k][0]:
                raw.append(b.reg[k][0])
        for b, k in writes:
            if b.all_w:
                deps.append(b.all_w)
            deps.extend(b.all_r.values())
            if k is None:
                for st in b.reg.values():
                    if st[0]:
                        deps.append(st[0])
                    deps.extend(st[1].values())
            elif k in b.reg:
                st = b.reg[k]
                if st[0]:
                    deps.append(st[0])
                deps.extend(st[1].values())
        return raw, deps

    def _record(self, reads, writes, dep):
        key = dep[0]
        for b, k in reads:
            if k is None:
                b.all_r[key] = dep
            else:
                b.reg.setdefault(k, [None, {}])[1][key] = dep
        for b, k in writes:
            if k is None:
                b.all_w = dep
                b.all_r = {}
                b.reg = {}
            else:
                b.reg[k] = [dep, {}]

    def _wait(self, e, deps):
        raw, other = deps
        for lst, same_ok in ((raw, e in ("act", "dve", "pool")), (other, False)):
            for key, sem, val in lst:
                if key == e and not same_ok:
                    continue
                if self.waited[e].get(key, 0) < val:
                    self.eng[e].wait_ge(sem, val)
                    self.waited[e][key] = val

    def op(self, e, emit, R=(), W=(), inc=True):
        R = self._norm(R)
        W = self._norm(W)
        self._wait(e, self._deps(R, W))
        ins = emit()
        if inc:
            ins.then_inc(self.sem[e], 1)
            self.cnt[e] += 1
            tick = self.cnt[e]
        else:
            tick = self.cnt[e] + 1
        self._record(R, W, (e, self.sem[e], tick))

    def dma(self, q, out_ap, in_ap, semb, R=(), W=()):
        R = self._norm(R)
        W = self._norm(W)
        self._wait(q, self._deps(R, W))
        if semb.name not in self.dsem:
            self.dsem[semb.name] = [self.es.enter_context(self.nc.semaphore("d_" + semb.name)), 0]
        ds = self.dsem[semb.name]
        self.eng[q].dma_start(out=out_ap, in_=in_ap).then_inc(ds[0], 16)
        ds[1] += 16
        self._record(R, W, ("dma_" + semb.name, ds[0], ds[1]))

    def barrier(self):
        for e in self.eng:
            deps = [(f, self.sem[f], self.cnt[f]) for f in self.eng if f != e and self.cnt[f] > 0]
            deps += [("dma_" + n, ds[0], ds[1]) for n, ds in self.dsem.items() if ds[1] > 0]
            self._wait(e, ([], deps))

    def act(self, out, in_, func, R, W, **kw):
        self.op("act", lambda: self.nc.scalar.activation(out=out, in_=in_, func=func, **kw), R, W)

    def mm(self, out, lhsT, rhs, start, stop, R, W, inc=None):
        self.op("pe", lambda: self.nc.tensor.matmul(out, lhsT, rhs, start=start, stop=stop), R, W,
                inc=True)


def build_program(pp_off, npp, nb=2, depth=DEPTH, debug=None):
    nc = bass.Bass("TRN2", target_bir_lowering=False)

    def din(name, shape):
        return nc.dram_tensor(name, list(shape), F32, kind="ExternalInput").ap()

    x_d = din("x", [nb, SEQ, D])
    ctx_d = din("ctx", [nb, CTX, D])
    condT_d = din("condT", [128, KC, 3])
    pp_d = din("pp", [128, npp])
    w_ada_d = din("w_ada", [DEPTH, D, 6 * D])
    w_in_d = din("w_in", [DEPTH, D, IN_W])
    wbd_d = din("wbd", [DEPTH, 2, 2, 2, 128, 128])
    bpat_d = din("bpat", [DEPTH, 8, 128, NSLOT, 128])
    w_out_d = din("w_out", [DEPTH, D, D])
    w_router_d = din("w_router", [D, N_EXP])
    b_router_d = din("b_router", [1, N_EXP])
    w_gate_d = din("w_gate", [DEPTH, N_EXP, D, D_EXP])
    w_up_d = din("w_up", [DEPTH, N_EXP, D, D_EXP])
    w_down_d = din("w_down", [DEPTH, N_EXP, D_EXP, D])
    ident_d = din("ident", [128, 128])
    out_d = nc.dram_tensor("out", [nb, SEQ, D], F32, kind="ExternalOutput").ap()
    dbg_d = None
    if debug is not None:
        dbg_d = nc.dram_tensor("dbg", [128, KC, T], F32, kind="ExternalOutput").ap()

    V, A, P, G = nc.vector, nc.scalar, nc.tensor, nc.gpsimd

    with ExitStack() as es:
        S = Sched(nc, es)

        xT = S.sb(es, "xT", [128, KC, T], F32)
        hT = S.sb(es, "hT", [128, KC, T], BF16)
        ppt = S.sb(es, "ppt", [128, npp], F32)
        mod = S.sb(es, "mod", [128, DEPTH, 48, 3], F32)
        modv = S.sb(es, "modv", [128, DEPTH, 3, 6, KC], F32)
        ident = S.sb(es, "ident", [128, 128], F32)
        identb = S.sb(es, "identb", [128, 128], BF16)
        onesb = S.sb(es, "onesb", [128, 128], BF16)
        onesf = S.sb(es, "onesf", [128, 128], F32)
        condT = S.sb(es, "condT", [128, KC, 3], F32)
        wr_sb = S.sb(es, "wr_sb", [128, KC, N_EXP], F32)
        br_sb = S.sb(es, "br_sb", [1, N_EXP], F32)
        cl = S.sb(es, "cl", [128, DEPTH, 2, 2], F32)
        outsem = S.wrap("outsem", None)
        wbuf = S.sb(es, "wbuf", [128, KC, 384], BF16)
        wspecs = []
        for _b in range(nb):
            for _l in range(depth):
                for _c in range(2):
                    wspecs.append((_l, [COL_SB + _c * 128, COL_SC + _c * 128, COL_SX + _c * 128]))
                for _c in range(2):
                    wspecs.append((_l, [COL_RX + _c * 128, COL_RG + _c * 128]))
                for _c in range(4):
                    wspecs.append((_l, [COL_Q + _c * 128, COL_K + _c * 128, COL_V + _c * 128]))
        wnext = [0]

        def issue_w():
            if wnext[0] >= len(wspecs):
                return
            l_, cols = wspecs[wnext[0]]
            wnext[0] += 1
            for j, col in enumerate(cols):
                S.dma("pool", wbuf[:, :, j * 128:(j + 1) * 128],
                      w_in_d[l_].rearrange("(k p) n -> p k n", p=128)[:, :, col:col + 128], wbuf, W=[wbuf])

        def ppv(name, *idx):
            return ppv_shape[name](*idx)

        ppv_shape = {}

        def reg_pp(name, dims):
            o, w = pp_off[name]
            strides = []
            s = 1
            for d in reversed(dims):
                strides.insert(0, s)
                s *= d
            assert s == w, (name, dims, w)

            def f(*idx):
                col = o + sum(i * st for i, st in zip(idx, strides))
                return ppt[:, col:col + 1]
            ppv_shape[name] = f

        reg_pp("b_ada", [DEPTH, 48])
        reg_pp("g_mix", [DEPTH, KC])
        reg_pp("g_out", [DEPTH, KC])
        reg_pp("g_ffn", [DEPTH, KC])
        reg_pp("g_fin", [KC])
        reg_pp("lconv_w", [DEPTH, 4, 2])
        reg_pp("lconv_b", [DEPTH, 2])
        reg_pp("rg_b", [DEPTH, 2, 2, 2])
        reg_pp("rg_lam", [DEPTH, 2, 2])
        reg_pp("sconv_w", [DEPTH, 3, 2])
        reg_pp("sconv_b", [DEPTH, 2])

        S.dma("sp", ppt[:], pp_d[:, :], ppt, W=[ppt])
        S.dma("sp", ident[:], ident_d[:, :], ident, W=[ident])
        S.dma("sp", condT[:], condT_d[:, :, :], condT, W=[condT])
        S.dma("sp", wr_sb[:], w_router_d.rearrange("(k p) e -> p k e", p=128), wr_sb, W=[wr_sb])
        S.dma("sp", br_sb[:], b_router_d[:, :], br_sb, W=[br_sb])
        S.op("dve", lambda: V.tensor_copy(out=identb[:], in_=ident[:]), R=[ident], W=[identb])
        S.op("dve", lambda: V.memset(onesb[:], 1.0), W=[onesb])
        S.op("dve", lambda: V.memset(onesf[:], 1.0), W=[onesf])
        S.act(condT[:], condT[:], AF.Silu, R=[condT], W=[condT])
        o_lam, w_lam = pp_off["rg_lam"]
        clv = cl[:].rearrange("p a b c -> p (a b c)")
        S.act(clv, ppt[:, o_lam:o_lam + w_lam], AF.Exp, R=[ppt], W=[cl], scale=-1.0)
        S.act(clv, clv, AF.Ln, R=[cl], W=[cl], bias=1.0)
        S.op("dve", lambda: V.tensor_scalar(out=clv, in0=clv, scalar1=-8.0, scalar2=None, op0=ALU.mult), R=[cl], W=[cl])

        o_rgb, w_rgb = pp_off["rg_b"]
        nrgb = S.sb(es, "nrgb", [128, w_rgb], F32)
        S.op("dve", lambda: V.tensor_scalar(out=nrgb[:], in0=ppt[:, o_rgb:o_rgb + w_rgb], scalar1=-1.0, scalar2=None,
                                            op0=ALU.mult), R=[ppt], W=[nrgb])

        def nrgbv(l_, d_, g_, c_):
            col = ((l_ * 2 + d_) * 2 + g_) * 2 + c_
            return nrgb[:, col:col + 1]

        if debug == ("const", 0, 0):
            S.barrier()
            S.dma("sp", dbg_d[:, 0, 0:128], ident[:], ident, R=[ident])
            S.dma("sp", dbg_d[:, 1, 0:8], cl[:].rearrange("p a b c -> p (a b c)"), cl, R=[cl])
            S.dma("sp", dbg_d[:, 2, 0:24], condT[:].rearrange("p a b -> p (a b)"), condT, R=[condT])
            S.barrier()
            return nc
        with ExitStack() as ph:
            wa = [S.sb(ph, f"wa{i}", [128, KC, 768], F32) for i in range(2)]
            pmod = S.ps(ph, "pmod", [128, 512], F32)
            it = 0
            for l in range(depth):
                for cg in range(8):
                    wb = wa[it % 2]
                    it += 1
                    S.dma("sp" if it % 2 else "act", wb[:],
                          w_ada_d[l].rearrange("(k p) n -> p k n", p=128)[:, :, cg * 768:(cg + 1) * 768], wb, W=[wb])
                    for jj in range(6):
                        j = cg * 6 + jj
                        for k in range(KC):
                            S.mm(pmod[:, jj * 3:jj * 3 + 3], wb[:, k, jj * 128:(jj + 1) * 128], condT[:, k, :],
                                 start=(k == 0), stop=(k == KC - 1), R=[wb, condT], W=[pmod])
                    for jj in range(6):
                        j = cg * 6 + jj
                        S.op("dve", lambda j=j, jj=jj: V.tensor_scalar(
                            out=mod[:, l, j, :], in0=pmod[:, jj * 3:jj * 3 + 3], scalar1=ppv("b_ada", l, j),
                            scalar2=None, op0=ALU.add), R=[pmod, ppt], W=[mod])
            for l in range(depth):
                for r in range(3):
                    for (dst, mi, gname) in ((0, 1, "g_mix"), (3, 4, "g_ffn")):
                        og, _ = pp_off[gname]
                        S.op("dve", lambda l=l, r=r, dst=dst, mi=mi, og=og: V.scalar_tensor_tensor(
                            out=modv[:, l, r, dst, :], in0=mod[:, l, mi * 8:(mi + 1) * 8, r], scalar=1.0,
                            in1=ppt[:, og + l * KC:og + (l + 1) * KC], op0=ALU.add, op1=ALU.mult),
                            R=[mod, ppt], W=[modv])
                    for (dst, mi) in ((1, 0), (2, 2), (4, 3), (5, 5)):
                        S.op("dve", lambda l=l, r=r, dst=dst, mi=mi: V.tensor_copy(
                            out=modv[:, l, r, dst, :], in_=mod[:, l, mi * 8:(mi + 1) * 8, r]), R=[mod], W=[modv])
            S.barrier()

        def mv(l, r, which, k):
            return modv[:, l, r, which, k:k + 1]

        if debug == ("mod", 0, 0):
            S.dma("sp", dbg_d[:, 0, 0:DEPTH * 48 * 3], mod[:].rearrange("p a b c -> p (a b c)"), mod, R=[mod])
            S.dma("sp", dbg_d[:, 1, 0:DEPTH * 3 * 6 * KC], modv[:].rearrange("p a b c d -> p (a b c d)"), modv, R=[modv])
            S.barrier()
            return nc

        def rms_rstd(ph, src, chunks, groups, scale, name, psum, sq, rstd_buf, rkey=lambda gi: gi, gis=None):
            for gi_, (t0, t1) in enumerate(groups):
                gi = gi_ if gis is None else gis[gi_]
                n = t1 - t0
                for ci, k in enumerate(chunks):
                    S.act(sq[:, ci % 2, 0:n], src[:, k, t0:t1], AF.Square, R=[(src, rkey(gi))], W=[(sq, ci % 2)])
                    S.mm(psum[:, 0:n], onesb[:], sq[:, ci % 2, 0:n], start=(ci == 0), stop=(ci == len(chunks) - 1),
                         R=[(sq, ci % 2), onesb], W=[psum])
                S.act(rstd_buf[:, t0:t1], psum[:, 0:n], AF.Sqrt, R=[psum], W=[(rstd_buf, gi)], scale=scale, bias=EPS)
                S.op("dve", lambda t0=t0, t1=t1: V.reciprocal(out=rstd_buf[:, t0:t1], in_=rstd_buf[:, t0:t1]),
                     R=[(rstd_buf, gi)], W=[(rstd_buf, gi)])

        def gkey(groups, gi):
            return GROUPS_ALL.index(groups[gi])

        def dbgdump(slot, ap, n, Rbufs, parts=128):
            with ExitStack() as dph:
                n = min(n, 2048)
                st = S.sb(dph, "dbgst", [128, 2048], F32)
                S.op("dve", lambda: V.tensor_copy(out=st[0:parts, 0:n], in_=ap[:, 0:n]), R=Rbufs, W=[st])
                S.dma("sp", dbg_d[0:parts, slot, 0:n], st[0:parts, 0:n], st, R=[st])
                S.barrier()

        issue_w()
        for b in range(nb):
            with ExitStack() as ph:
                stg = [S.sb(ph, f"stg{i}", [128, D], F32) for i in range(2)]
                ptr = [S.ps(ph, f"ptr{i}", [128, 512], F32) for i in range(4)]
                for tt in range(T // 128):
                    st = stg[tt % 2]
                    src = ctx_d[b, tt * 128:(tt + 1) * 128, :] if tt < 2 else x_d[b, (tt - 2) * 128:(tt - 1) * 128, :]
                    S.dma("sp", st[:], src, st, W=[st])
                    for hf in range(2):
                        pt = ptr[(tt * 2 + hf) % 4]
                        for kk in range(4):
                            k = hf * 4 + kk
                            S.op("pe", lambda pt=pt, kk=kk, k=k, st=st: P.transpose(
                                pt[:, kk * 128:(kk + 1) * 128], st[:, k * 128:(k + 1) * 128], ident[:]),
                                R=[st, ident], W=[pt])
                        eng = "act" if hf == 0 else "dve"
                        dst = xT[:, hf * 4:hf * 4 + 4, tt * 128:(tt + 1) * 128]
                        srcp = pt[:].rearrange("p (k t) -> p k t", t=128)
                        if eng == "act":
                            S.act(dst, srcp, AF.Identity, R=[pt], W=[(xT, tt)])
                        else:
                            S.op("dve", lambda dst=dst, srcp=srcp: V.tensor_copy(out=dst, in_=srcp), R=[pt], W=[(xT, tt)])
                S.barrier()

            if debug == ("load", b, 0):
                _dump(S, nc, dbg_d, xT, es)
                return nc
            for l in range(depth):
                last = (l == DEPTH - 1)
                groups_out = GROUPS_LAT if last else GROUPS_ALL

                def norm_mod(ph, which_g, which_s, groups):
                    sq = S.sb(ph, "sq", [128, 2, 512], BF16)
                    rstd = S.sb(ph, "rstd", [128, T], F32)
                    tmp = S.sb(ph, "ntmp", [128, 2, 512], F32)
                    psn = S.ps(ph, "psn", [128, 512], F32)
                    def stats(gi):
                        rms_rstd(ph, xT, list(range(KC)), [groups[gi]], 1.0 / D, "n", psn, sq, rstd, gis=[gi])
                    stats(0)
                    for gi, (t0, t1) in enumerate(groups):
                        n = t1 - t0
                        r = 2 if t0 < CTX else b
                        if gi + 1 < len(groups):
                            stats(gi + 1)
                        for k in range(KC):
                            tb = k % 2
                            S.op("dve", lambda k=k, t0=t0, t1=t1, n=n, r=r, tb=tb: V.scalar_tensor_tensor(
                                out=tmp[:, tb, 0:n], in0=xT[:, k, t0:t1], scalar=mv(l, r, which_g, k),
                                in1=rstd[:, t0:t1], op0=ALU.mult, op1=ALU.mult),
                                R=[(xT, None), (rstd, gi), modv], W=[(tmp, tb)])
                            S.act(hT[:, k, t0:t1], tmp[:, tb, 0:n], AF.Identity, R=[(tmp, tb), modv],
                                  W=[(hT, gkey(groups, gi))], bias=mv(l, r, which_s, k))

                with ExitStack() as ph:
                    norm_mod(ph, 0, 1, GROUPS_ALL)
                    S.barrier()

                if debug == ("h1", b, l):
                    _dump(S, nc, dbg_d, hT, es)
                    return nc

                with ExitStack() as mx:
                    yT = S.sb(mx, "yT", [128, KC, T], BF16)

                    def proj_fm(ph, wsb, wcol0, m, dst_fn, groups, ps_list, base=0):
                        for gi, (t0, t1) in enumerate(groups):
                            n = t1 - t0
                            pt = ps_list[gi % len(ps_list)]
                            for k in range(KC):
                                S.mm(pt[0:m, 0:n], wsb[:, k, wcol0:wcol0 + m], hT[:, k, t0:t1], start=(k == 0),
                                     stop=(k == KC - 1), R=[wsb, (hT, gkey(groups, gi))], W=[pt])
                            dst_fn(gi, t0, t1, pt)

                    for c in range(2):
                        with ExitStack() as ph:
                            wsb = wbuf
                            sbv = S.sb(ph, "sbv", [128, T], F32)
                            pv = S.sb(ph, "pv", [128, T], F32)
                            acc = S.sb(ph, "acc", [128, T], F32)
                            pss = [S.ps(ph, f"pss{i}", [128, 512], F32) for i in range(4)]

                            def ev_sb(gi, t0, t1, pt):
                                S.act(sbv[:, t0:t1], pt[:, 0:t1 - t0], AF.Identity, R=[pt], W=[(sbv, gi)])

                            def ev_sc(gi, t0, t1, pt):
                                S.act(pv[:, t0:t1], pt[:, 0:t1 - t0], AF.Identity, R=[pt], W=[(pv, gi)])

                            def ev_sx(gi, t0, t1, pt):
                                S.op("dve", lambda: V.tensor_tensor(out=pv[:, t0:t1], in0=pt[:, 0:t1 - t0],
                                                                    in1=pv[:, t0:t1], op=ALU.mult),
                                     R=[pt, (pv, gi)], W=[(pv, gi)])

                            proj_fm(ph, wsb, 0, 128, ev_sb, GROUPS_ALL, pss)
                            proj_fm(ph, wsb, 128, 128, ev_sc, GROUPS_ALL, pss)
                            proj_fm(ph, wsb, 256, 128, ev_sx, GROUPS_ALL, pss)
                            issue_w()
                            for (s0, s1) in ((0, CTX), (CTX, T)):
                                S.op("dve", lambda s0=s0, s1=s1: V.tensor_scalar(
                                    out=acc[:, s0:s1], in0=pv[:, s0:s1], scalar1=ppv("sconv_w", l, 1, c),
                                    scalar2=ppv("sconv_b", l, c), op0=ALU.mult, op1=ALU.add), R=[pv, ppt], W=[acc])
                                S.op("dve", lambda s0=s0, s1=s1: V.scalar_tensor_tensor(
                                    out=acc[:, s0 + 1:s1], in0=pv[:, s0:s1 - 1], scalar=ppv("sconv_w", l, 0, c),
                                    in1=acc[:, s0 + 1:s1], op0=ALU.mult, op1=ALU.add), R=[pv, ppt, acc], W=[acc])
                                S.op("dve", lambda s0=s0, s1=s1: V.scalar_tensor_tensor(
                                    out=acc[:, s0:s1 - 1], in0=pv[:, s0 + 1:s1], scalar=ppv("sconv_w", l, 2, c),
                                    in1=acc[:, s0:s1 - 1], op0=ALU.mult, op1=ALU.add), R=[pv, ppt, acc], W=[acc])
                            S.op("dve", lambda: V.tensor_tensor(out=yT[:, 6 + c, :], in0=sbv[:], in1=acc[:], op=ALU.mult),
                                 R=[sbv, acc], W=[yT])
                            S.barrier()

                    for c in range(2):
                        with ExitStack() as ph:
                            wsb = wbuf
                            wg = S.sb(ph, "wbd", [128, 2, 2, 128], F32)
                            for d in range(2):
                                for g in range(2):
                                    S.dma("sp", wg[:, d, g, :], wbd_d[l, d, g, c, :, :], wg, W=[wg])
                            rxc = S.sb(ph, "rxc", [128, T], F32)
                            a_f = S.sb(ph, "a_f", [128, T], F32)
                            a_r = S.sb(ph, "a_r", [128, T], F32)
                            bb = S.sb(ph, "bb", [128, T], F32)
                            gt = S.sb(ph, "gt", [128, 4, 512], F32)
                            psl = [S.ps(ph, f"psl{i}", [128, 512], F32) for i in range(4)]
                            rx_raw = a_f

                            def ev_rx(gi, t0, t1, pt):
                                S.act(rx_raw[:, t0:t1], pt[:, 0:t1 - t0], AF.Identity, R=[pt], W=[(rx_raw, gi)])

                            def ev_rg(gi, t0, t1, pt):
                                S.act(yT[:, 4 + c, t0:t1], pt[:, 0:t1 - t0], AF.Gelu, R=[pt], W=[(yT, gi)])

                            proj_fm(ph, wsb, 0, 128, ev_rx, GROUPS_ALL, psl)
                            proj_fm(ph, wsb, 128, 128, ev_rg, GROUPS_ALL, psl)
                            issue_w()
                            for (s0, s1) in ((0, CTX), (CTX, T)):
                                S.op("dve", lambda s0=s0, s1=s1: V.tensor_scalar(
                                    out=rxc[:, s0:s1], in0=rx_raw[:, s0:s1], scalar1=ppv("lconv_w", l, 2, c),
                                    scalar2=ppv("lconv_b", l, c), op0=ALU.mult, op1=ALU.add), R=[rx_raw, ppt], W=[rxc])
                                for (kk, sh) in ((0, 2), (1, 1)):
                                    S.op("dve", lambda s0=s0, s1=s1, kk=kk, sh=sh: V.scalar_tensor_tensor(
                                        out=rxc[:, s0 + sh:s1], in0=rx_raw[:, s0:s1 - sh], scalar=ppv("lconv_w", l, kk, c),
                                        in1=rxc[:, s0 + sh:s1], op0=ALU.mult, op1=ALU.add), R=[rx_raw, ppt, rxc], W=[rxc])
                                S.op("dve", lambda s0=s0, s1=s1: V.scalar_tensor_tensor(
                                    out=rxc[:, s0:s1 - 1], in0=rx_raw[:, s0 + 1:s1], scalar=ppv("lconv_w", l, 3, c),
                                    in1=rxc[:, s0:s1 - 1], op0=ALU.mult, op1=ALU.add), R=[rx_raw, ppt, rxc], W=[rxc])
                            for d in range(2):
                                adst = a_f if d == 0 else a_r
                                for gi, (t0, t1) in enumerate(GROUPS_ALL):
                                    n = t1 - t0
                                    if d == 0:
                                        q0, q1 = t0, t1
                                    elif t0 < CTX:
                                        q0, q1 = 0, CTX
                                    else:
                                        q0 = CTX + (T - t1)
                                        q1 = q0 + n
                                    pr, pi = psl[0 + 2 * (gi % 2)], psl[1 + 2 * (gi % 2)]
                                    S.mm(pr[:, 0:n], wg[:, d, 0, :], rxc[:, t0:t1], True, True, R=[wg, rxc], W=[pr])
                                    S.mm(pi[:, 0:n], wg[:, d, 1, :], rxc[:, t0:t1], True, True, R=[wg, rxc], W=[pi])
                                    rr, ii, mm_, tt_ = gt[:, 0, 0:n], gt[:, 1, 0:n], gt[:, 2, 0:n], gt[:, 3, 0:n]
                                    S.act(rr, pr[:, 0:n], AF.Sigmoid, R=[pr, ppt], W=[(gt, 0)], bias=ppv("rg_b", l, d, 0, c))
                                    S.act(ii, pi[:, 0:n], AF.Sigmoid, R=[pi, ppt], W=[(gt, 1)], bias=ppv("rg_b", l, d, 1, c))

                                    def rv(ap):
                                        return ap[:, ::-1] if d == 1 else ap
                                    S.act(adst[:, q0:q1], rv(rr), AF.Exp, R=[(gt, 0), cl], W=[(adst, gi)],
                                          scale=cl[:, l, d, c:c + 1])
                                    S.op("pool", lambda mm_=mm_, q0=q0, q1=q1, adst=adst: G.tensor_tensor(
                                        out=mm_, in0=adst[:, q0:q1], in1=adst[:, q0:q1], op=ALU.mult),
                                        R=[(adst, gi)], W=[(gt, 2)])
                                    S.act(mm_, mm_, AF.Sqrt, R=[(gt, 2)], W=[(gt, 2)], scale=-1.0, bias=1.0)
                                    S.op("dve", lambda tt_=tt_, ii=ii, t0=t0, t1=t1: V.tensor_tensor(
                                        out=tt_, in0=ii, in1=rxc[:, t0:t1], op=ALU.mult), R=[(gt, 1), rxc], W=[(gt, 3)])
                                    S.op("dve", lambda tt_=tt_, mm_=mm_, q0=q0, q1=q1, rv=rv: V.tensor_tensor(
                                        out=bb[:, q0:q1], in0=mm_, in1=rv(tt_), op=ALU.mult),
                                        R=[(gt, 2), (gt, 3)], W=[(bb, gi)])
                                if debug == ("lru", b, l) and c == 0 and d == 0:
                                    dbgdump(0, rxc[:, :], T, [rxc])
                                    dbgdump(1, a_f[:, :], T, [a_f])
                                    dbgdump(2, bb[:, :], T, [bb])
                                    dbgdump(3, gt[:, :, :].rearrange("p a b -> p (a b)"), 2048, [gt])
                                    dbgdump(4, cl[:].rearrange("p a b c -> p (a b c)"), 8, [cl])
                                S.op("dve", lambda adst=adst: V.tensor_tensor_scan(
                                    out=adst[:, :], data0=adst[:, :], data1=bb[:, :], initial=0.0,
                                    op0=ALU.mult, op1=ALU.add), R=[adst, bb], W=[adst])
                            if debug == ("lru", b, l) and c == 0:
                                dbgdump(5, a_f[:, :], T, [a_f])
                                dbgdump(6, a_r[:, :], T, [a_r])
                                dbgdump(7, yT[:, 4 + c, :], T, [yT])
                                return nc
                            for (s0, s1) in ((0, CTX), (CTX, T)):
                                S.op("dve", lambda s0=s0, s1=s1: V.tensor_tensor(
                                    out=bb[:, s0:s1], in0=a_f[:, s0:s1], in1=a_r[:, s0:s1][:, ::-1], op=ALU.add),
                                    R=[a_f, a_r], W=[bb])
                            S.op("dve", lambda: V.tensor_tensor(out=yT[:, 4 + c, :], in0=bb[:], in1=yT[:, 4 + c, :],
                                                                op=ALU.mult), R=[bb, yT], W=[yT])
                            S.barrier()

                    qtiles = list(range(0 if not last else 2, T // 128))
                    with ExitStack() as nas:
                        bpb = [S.sb(nas, f"bp{i}", [128, 2, NSLOT, 128], BF16) for i in range(2)]

                        def load_bp(c_):
                            for hh_ in range(2):
                                S.dma("pool", bpb[c_ % 2][:, hh_, :, :], bpat_d[l, 2 * c_ + hh_, :, :, :], bpb[c_ % 2],
                                      W=[bpb[c_ % 2]])

                        load_bp(0)
                        for c in range(4):
                            with ExitStack() as ph:
                                wsb = wbuf
                                bp = bpb[c % 2]
                                QT = S.sb(ph, "QT", [128, T], BF16)
                                KT = S.sb(ph, "KT", [128, T], BF16)
                                Vp = S.sb(ph, "Vp", [128, T // 128, 2, 65], BF16)
                                PT = [S.sb(ph, f"PT{i}", [128, 7 * 128], BF16) for i in range(2)]
                                otok = [S.sb(ph, f"otok{i}", [128, 2, 64], BF16) for i in range(2)]
                                rec = [S.sb(ph, f"rec{i}", [128, 2, 1], F32) for i in range(2)]
                                psq = [S.ps(ph, f"psq{i}", [128, 512], F32) for i in range(2)]
                                pst = [S.ps(ph, f"pst{i}", [128, 1024], F32) for i in range(2)]
                                pso = S.ps(ph, "pso", [128, 2, 2, 65], F32)
                                pstr = S.ps(ph, "pstr", [128, 128], BF16)
                                S.op("dve", lambda: V.memset(Vp[:, :, :, 64:65], 1.0), W=[Vp])

                                def ev_q(gi, t0, t1, pt):
                                    S.act(QT[:, t0:t1], pt[:, 0:t1 - t0], AF.Identity, R=[pt], W=[(QT, gi)], scale=0.125)

                                def ev_k(gi, t0, t1, pt):
                                    S.op("dve", lambda: V.tensor_copy(out=KT[:, t0:t1], in_=pt[:, 0:t1 - t0]),
                                         R=[pt], W=[(KT, gi)])
                                proj_fm(ph, wsb, 0, 128, ev_q, GROUPS_LAT if last else GROUPS_ALL, psq)
                                proj_fm(ph, wsb, 128, 128, ev_k, GROUPS_ALL, psq)
                                for tt in range(T // 128):
                                    pt = psq[tt % 2]
                                    for k in range(KC):
                                        S.mm(pt[:, 0:128], hT[:, k, tt * 128:(tt + 1) * 128], wsb[:, k, 256:384], start=(k == 0),
                                             stop=(k == KC - 1), R=[wsb, (hT, None)], W=[pt])
                                    S.act(Vp[:, tt, :, 0:64], pt[:, 0:128].rearrange("p (h d) -> p h d", d=64), AF.Identity,
                                          R=[pt], W=[(Vp, tt)])
                                issue_w()
                                if c + 1 < 4:
                                    load_bp(c + 1)
                                bpf = bp[:].rearrange("p a b c -> p (a b c)")
                                S.act(bpf, bpf, AF.Exp, R=[bp], W=[bp])
                                for qi, qt in enumerate(qtiles):
                                    if qt < 2:
                                        blocks = [0, 1]
                                        nl, s0 = 0, 0
                                    else:
                                        blocks = [0, 1] + [2 + j for (j, p) in NA_TILES[qt - 2]]
                                        nl, s0 = len(NA_TILES[qt - 2]), NA_START[qt - 2]
                                    nbk = len(blocks)
                                    po = pso[:, qi % 2]
                                    pok = (pso, qi % 2)
                                    for hh in range(2):
                                        ps_s = pst[(qi * 2 + hh) % 2]
                                        ptb = PT[(qi * 2 + hh) % 2]
                                        for bi, kt in enumerate(blocks):
                                            S.mm(ps_s[:, bi * 128:(bi + 1) * 128], KT[hh * 64:(hh + 1) * 64, kt * 128:(kt + 1) * 128],
                                                 QT[hh * 64:(hh + 1) * 64, qt * 128:(qt + 1) * 128], start=True, stop=True,
                                                 R=[(KT, None), (QT, None)], W=[ps_s])
                                        for (c0, c1) in ((0, min(nbk, 4)), (4, nbk)):
                                            if c1 > c0:
                                                S.act(ptb[:, c0 * 128:c1 * 128], ps_s[:, c0 * 128:c1 * 128], AF.Exp, R=[ps_s], W=[ptb])
                                        if nl:
                                            S.op("dve", lambda ptb=ptb, hh=hh, nl=nl, s0=s0: V.tensor_tensor(
                                                out=ptb[:, 256:256 + nl * 128].rearrange("p (a b) -> p a b", b=128),
                                                in0=ptb[:, 256:256 + nl * 128].rearrange("p (a b) -> p a b", b=128),
                                                in1=bp[:, hh, s0:s0 + nl, :], op=ALU.mult), R=[ptb, bp], W=[ptb])
                                        for bi, kt in enumerate(blocks):
                                            S.mm(po[:, hh, :], ptb[:, bi * 128:(bi + 1) * 128], Vp[:, kt, hh, :], start=(bi == 0),
                                                 stop=(bi == nbk - 1), R=[ptb, (Vp, None)], W=[pok])
                                    rc, ot = rec[qi % 2], otok[qi % 2]
                                    S.op("dve", lambda rc=rc, po=po: V.reciprocal(out=rc[:], in_=po[:, :, 64:65]), R=[pok], W=[rc])
                                    S.op("dve", lambda rc=rc, po=po, ot=ot: V.tensor_tensor(
                                        out=ot[:], in0=po[:, :, 0:64], in1=rc[:].to_broadcast([128, 2, 64]), op=ALU.mult),
                                        R=[pok, rc], W=[ot])
                                    S.op("pe", lambda ot=ot: P.transpose(pstr[:], ot[:].rearrange("p h d -> p (h d)"), identb[:]),
                                         R=[ot, identb], W=[pstr])
                                    S.act(yT[:, c, qt * 128:(qt + 1) * 128], pstr[:], AF.Identity, R=[pstr], W=[(yT, ("na", qt))])
                                S.barrier()

                    if debug == ("y", b, l):
                        _dump(S, nc, dbg_d, yT, es)
                        return nc

                    with ExitStack() as ph:
                        wo = S.sb(ph, "wo", [128, KC, D], BF16)
                        for hf in range(2):
                            S.dma("pool", wo[:, :, hf * 512:(hf + 1) * 512],
                                  w_out_d[l].rearrange("(k p) n -> p k n", p=128)[:, :, hf * 512:(hf + 1) * 512], wo, W=[wo])
                        sq = S.sb(ph, "sq", [128, 2, 512], BF16)
                        rstds = [S.sb(ph, f"rstd{i}", [128, T], F32) for i in range(3)]
                        psn = S.ps(ph, "psn", [128, 512], F32)
                        pso2 = [S.ps(ph, f"pso2{i}", [128, 512], F32) for i in range(4)]
                        grp_chunks = ([0, 1, 2, 3], [4, 5], [6, 7])
                        def ostats(gi):
                            for gidx, chunks in enumerate(grp_chunks):
                                rms_rstd(ph, yT, chunks, [groups_out[gi]], 1.0 / (128 * len(chunks)), "g", psn, sq, rstds[gidx],
                                         rkey=lambda gi_: ("o", gi_), gis=[gi])
                        ostats(0)
                        for gi, (t0, t1) in enumerate(groups_out):
                            n = t1 - t0
                            r = 2 if t0 < CTX else b
                            if gi + 1 < len(groups_out):
                                ostats(gi + 1)
                            for gidx, chunks in enumerate(grp_chunks):
                                for k in chunks:
                                    S.op("dve", lambda k=k, t0=t0, t1=t1, gidx=gidx: V.scalar_tensor_tensor(
                                        out=yT[:, k, t0:t1], in0=yT[:, k, t0:t1], scalar=ppv("g_out", l, k),
                                        in1=rstds[gidx][:, t0:t1], op0=ALU.mult, op1=ALU.mult),
                                        R=[(yT, ("o", gi)), (rstds[gidx], gi), ppt], W=[(yT, ("o", gi))])
                            for oc in range(KC):
                                pt = pso2[oc % 4]
                                for k in range(KC):
                                    S.mm(pt[:, 0:n], wo[:, k, oc * 128:(oc + 1) * 128], yT[:, k, t0:t1], start=(k == 0),
                                         stop=(k == KC - 1), R=[wo, (yT, ("o", gi))], W=[pt])
                                S.op("dve", lambda oc=oc, t0=t0, t1=t1, n=n, r=r, pt=pt: V.scalar_tensor_tensor(
                                    out=xT[:, oc, t0:t1], in0=pt[:, 0:n], scalar=mv(l, r, 2, oc), in1=xT[:, oc, t0:t1],
                                    op0=ALU.mult, op1=ALU.add), R=[pt, (xT, None), modv], W=[(xT, None)])
                        S.barrier()

                if debug == ("xmix", b, l):
                    _dump(S, nc, dbg_d, xT, es)
                    return nc

                with ExitStack() as ph:
                    ng = len(groups_out)
                    tok0 = groups_out[0][0]
                    comb = S.sb(ph, "comb", [N_EXP, T], F32)
                    with ExitStack() as ph2:
                        sq = S.sb(ph2, "sq", [128, 2, 512], BF16)
                        rstd = S.sb(ph2, "rstd", [128, T], F32)
                        tmp = S.sb(ph2, "ntmp", [128, 2, 512], F32)
                        hfb = S.sb(ph2, "hfb", [128, KC, 512], F32)
                        psn = S.ps(ph2, "psn", [128, 512], F32)
                        psr = [S.ps(ph2, f"psr{i}", [128, 512], F32) for i in range(2)]
                        pscT = S.ps(ph2, "pscT", [N_EXP, 512], F32)
                        rts = [S.sb(ph2, f"rt{i}", [128, 16, 16], F32) for i in range(2)]
                        rms_rstd(ph2, xT, list(range(KC)), groups_out, 1.0 / D, "n2", psn, sq, rstd)
                        tile_i = 0
                        for gi, (t0, t1) in enumerate(groups_out):
                            n = t1 - t0
                            r = 2 if t0 < CTX else b
                            for k in range(KC):
                                tb = k % 2
                                S.op("dve", lambda k=k, t0=t0, t1=t1, n=n, r=r, tb=tb: V.scalar_tensor_tensor(
                                    out=tmp[:, tb, 0:n], in0=xT[:, k, t0:t1], scalar=mv(l, r, 3, k),
                                    in1=rstd[:, t0:t1], op0=ALU.mult, op1=ALU.mult),
                                    R=[(xT, None), (rstd, gi), modv], W=[(tmp, tb)])
                                S.act(hfb[:, k, 0:n], tmp[:, tb, 0:n], AF.Identity, R=[(tmp, tb), modv],
                                      W=[(hfb, k)], bias=mv(l, r, 4, k))
                                S.op("pool", lambda k=k, t0=t0, t1=t1, n=n: G.tensor_copy(
                                    out=hT[:, k, t0:t1], in_=hfb[:, k, 0:n]), R=[(hfb, k)], W=[(hT, None)])
                            for ti in range(n // 128):
                                pr = psr[tile_i % 2]
                                rt = rts[tile_i % 2]
                                tile_i += 1
                                for k in range(KC):
                                    S.mm(pr[:, 0:N_EXP], hfb[:, k, ti * 128:(ti + 1) * 128], wr_sb[:, k, :], start=(k == 0),
                                         stop=False, R=[(hfb, None), wr_sb], W=[pr])
                                S.mm(pr[:, 0:N_EXP], onesf[0:1, :], br_sb[:, :], start=False, stop=True, R=[onesf, br_sb], W=[pr])

                                def dv(fn, rt=rt):
                                    S.op("dve", fn, R=[rt], W=[rt])

                                def row(i, w=4, rt=rt):
                                    return rt[:, i, 0:w]
                                lg, ev, cw = row(0, 16), row(2, 16), row(15, 16)
                                nmx, gmax = rt[:, 1, 0:1], rt[:, 1, 1:2]
                                S.op("dve", lambda lg=lg, pr=pr: V.tensor_copy(out=lg, in_=pr[:, 0:N_EXP]), R=[pr], W=[rt])
                                dv(lambda: V.tensor_reduce(out=nmx, in_=lg, axis=mybir.AxisListType.X, op=ALU.max))
                                dv(lambda: V.tensor_scalar(out=nmx, in0=nmx, scalar1=-1.0, scalar2=None, op0=ALU.mult))
                                S.act(ev, lg, AF.Exp, R=[rt], W=[rt], bias=nmx)
                                e3 = ev.rearrange("p (g j) -> p g j", j=4)
                                a_, b_, c_, d_ = (e3[:, :, j] for j in range(4))

                                def tt2(out, i0, i1, op):
                                    dv(lambda: V.tensor_tensor(out=out, in0=i0, in1=i1, op=op))
                                tt2(row(3), a_, b_, ALU.max)
                                tt2(row(4), c_, d_, ALU.max)
                                tt2(row(5), a_, b_, ALU.min)
                                tt2(row(6), c_, d_, ALU.min)
                                tt2(row(7), row(3), row(4), ALU.max)
                                tt2(row(8), row(3), row(4), ALU.min)
                                tt2(row(9), row(5), row(6), ALU.max)
                                tt2(row(10), row(8), row(9), ALU.max)
                                tt2(row(11), row(7), row(10), ALU.add)
                                dv(lambda: V.tensor_reduce(out=gmax, in_=row(11), axis=mybir.AxisListType.X, op=ALU.max))
                                dv(lambda: V.tensor_scalar(out=row(12), in0=row(11), scalar1=gmax, scalar2=None, op0=ALU.is_equal))
                                dv(lambda: V.reciprocal(out=row(13), in_=row(11)))
                                tt2(row(14), row(12), row(13), ALU.mult)
                                cw3 = cw.rearrange("p (g j) -> p g j", j=4)
                                top2b = row(10).rearrange("p (g o) -> p g o", o=1).to_broadcast([128, 4, 4])
                                facb = row(14).rearrange("p (g o) -> p g o", o=1).to_broadcast([128, 4, 4])
                                tt2(cw3, e3, top2b, ALU.is_ge)
                                tt2(cw3, cw3, e3, ALU.mult)
                                tt2(cw3, cw3, facb, ALU.mult)
                                S.op("pe", lambda cw=cw, ti=ti: P.transpose(pscT[:, ti * 128:(ti + 1) * 128], cw, ident[:]),
                                     R=[rt, ident], W=[pscT])
                            S.act(comb[:, t0:t1], pscT[:, 0:n], AF.Identity, R=[pscT], W=[(comb, gi)])
                        S.barrier()

                    if debug == ("comb", b, l):
                        S.dma("sp", dbg_d[0:N_EXP, 0, :], comb[:, :], comb, R=[comb])
                        S.barrier()
                        return nc

                    wts = [[S.sb(ph, f"wg{i}", [128, KC, D_EXP], BF16), S.sb(ph, f"wu{i}", [128, KC, D_EXP], BF16),
                            S.sb(ph, f"wd{i}", [128, 4, D], BF16)] for i in range(2)]
                    cm = [S.sb(ph, f"cm{i}", [N_EXP, 512], F32) for i in range(2)]
                    cbc = [S.sb(ph, f"cbc{i}", [128, 512], F32) for i in range(2)]
                    sg = [S.sb(ph, f"sg{i}", [128, 512], F32) for i in range(2)]
                    hid = [S.sb(ph, f"hid{i}", [128, 4, 512], BF16) for i in range(2)]
                    psg = [S.ps(ph, f"psg{i}", [128, 512], F32) for i in range(2)]
                    psu = [S.ps(ph, f"psu{i}", [128, 512], F32) for i in range(2)]
                    psd = [S.ps(ph, f"psd{i}", [128, 512], F32) for i in range(3)]
                    psc = S.ps(ph, "psc", [128, 512], F32)

                    def load_expert(e):
                        wgt, wut, wdt = wts[e % 2]
                        S.dma("pool", wgt[:], w_gate_d[l, e].rearrange("(k p) n -> p k n", p=128), wgt, W=[wgt])
                        S.dma("pool", wut[:], w_up_d[l, e].rearrange("(k p) n -> p k n", p=128), wut, W=[wut])
                        S.dma("pool", wdt[:], w_down_d[l, e].rearrange("(k p) n -> p k n", p=128), wdt, W=[wdt])

                    it = 0
                    load_expert(0)
                    for e in range(N_EXP):
                        wgt, wut, wdt = wts[e % 2]
                        if e + 1 < N_EXP:
                            load_expert(e + 1)
                        for gi, (t0, t1) in enumerate(groups_out):
                            n = t1 - t0
                            r = 2 if t0 < CTX else b
                            cb, hd, cmt = cbc[it % 2], hid[it % 2], cm[it % 2]
                            it += 1
                            S.op("dve", lambda cmt=cmt, t0=t0, t1=t1, n=n, e=e: V.tensor_scalar(
                                out=cmt[:, 0:n], in0=comb[:, t0:t1], scalar1=ident[0:N_EXP, e:e + 1], scalar2=None,
                                op0=ALU.mult), R=[(comb, gi), ident], W=[cmt])
                            S.mm(psc[:, 0:n], onesf[0:N_EXP, :], cmt[:, 0:n], True, True, R=[onesf, cmt], W=[psc])
                            S.act(cb[:, 0:n], psc[:, 0:n], AF.Identity, R=[psc], W=[cb])
                            for hc in range(4):
                                pg, pu, sgt = psg[hc % 2], psu[hc % 2], sg[hc % 2]
                                for k in range(KC):
                                    S.mm(pg[:, 0:n], wgt[:, k, hc * 128:(hc + 1) * 128], hT[:, k, t0:t1], start=(k == 0),
                                         stop=(k == KC - 1), R=[wgt, (hT, None)], W=[pg])
                                for k in range(KC):
                                    S.mm(pu[:, 0:n], wut[:, k, hc * 128:(hc + 1) * 128], hT[:, k, t0:t1], start=(k == 0),
                                         stop=(k == KC - 1), R=[wut, (hT, None)], W=[pu])
                                S.act(sgt[:, 0:n], pg[:, 0:n], AF.Silu, R=[pg], W=[sgt])
                                S.op("dve", lambda sgt=sgt, cb=cb, n=n: V.tensor_tensor(
                                    out=sgt[:, 0:n], in0=sgt[:, 0:n], in1=cb[:, 0:n], op=ALU.mult), R=[sgt, cb], W=[sgt])
                                S.op("dve", lambda hd=hd, hc=hc, pu=pu, sgt=sgt, n=n: V.tensor_tensor(
                                    out=hd[:, hc, 0:n], in0=pu[:, 0:n], in1=sgt[:, 0:n], op=ALU.mult),
                                    R=[pu, sgt], W=[(hd, hc)])
                            for oc in range(KC):
                                pd = psd[oc % 3]
                                for hc in range(4):
                                    S.mm(pd[:, 0:n], wdt[:, hc, oc * 128:(oc + 1) * 128], hd[:, hc, 0:n], start=(hc == 0),
                                         stop=(hc == 3), R=[wdt, (hd, hc)], W=[pd])
                                S.op("dve", lambda oc=oc, t0=t0, t1=t1, n=n, r=r, pd=pd: V.scalar_tensor_tensor(
                                    out=xT[:, oc, t0:t1], in0=pd[:, 0:n], scalar=mv(l, r, 5, oc), in1=xT[:, oc, t0:t1],
                                    op0=ALU.mult, op1=ALU.add), R=[pd, (xT, gi), modv], W=[(xT, gi)])
                    S.barrier()

                if debug == ("xmoe", b, l):
                    _dump(S, nc, dbg_d, xT, es)
                    return nc

            with ExitStack() as ph:
                sq = S.sb(ph, "sq", [128, 2, 512], BF16)
                rstd = S.sb(ph, "rstd", [128, T], F32)
                xn = [S.sb(ph, f"xn{i}", [128, KC, 512], F32) for i in range(1)]
                ost = [S.sb(ph, f"ost{i}", [128, D], F32) for i in range(2)]
                psn = S.ps(ph, "psn", [128, 512], F32)
                ptr = [S.ps(ph, f"ptr{i}", [128, 512], F32) for i in range(4)]
                def fstats(gi):
                    rms_rstd(ph, xT, list(range(KC)), [GROUPS_LAT[gi]], 1.0 / D, "f", psn, sq, rstd, gis=[gi])
                fstats(0)
                oi = 0
                for gi, (t0, t1) in enumerate(GROUPS_LAT):
                    n = t1 - t0
                    xnb = xn[0]
                    if gi + 1 < len(GROUPS_LAT):
                        fstats(gi + 1)
                    for k in range(KC):
                        S.op("dve", lambda k=k, t0=t0, t1=t1, n=n, xnb=xnb: V.scalar_tensor_tensor(
                            out=xnb[:, k, 0:n], in0=xT[:, k, t0:t1], scalar=ppv("g_fin", k), in1=rstd[:, t0:t1],
                            op0=ALU.mult, op1=ALU.mult), R=[(xT, None), (rstd, gi), ppt], W=[(xnb, None)])
                    for ti in range(n // 128):
                        ot = ost[oi % 2]
                        for hf in range(2):
                            pt = ptr[(oi * 2 + hf) % 4]
                            for kk in range(4):
                                k = hf * 4 + kk
                                S.op("pe", lambda pt=pt, kk=kk, k=k, ti=ti, xnb=xnb: P.transpose(
                                    pt[:, kk * 128:(kk + 1) * 128], xnb[:, k, ti * 128:(ti + 1) * 128], ident[:]),
                                    R=[(xnb, None), ident], W=[pt])
                            if hf == 0:
                                S.act(ot[:, 0:512], pt[:], AF.Identity, R=[pt], W=[(ot, 0)])
                            else:
                                S.op("dve", lambda ot=ot, pt=pt: V.tensor_copy(out=ot[:, 512:1024], in_=pt[:]), R=[pt], W=[(ot, 1)])
                        row0 = t0 - CTX + ti * 128
                        S.dma("sp", out_d[b, row0:row0 + 128, :], ot[:], ot, R=[ot])
                        oi += 1
                S.barrier()

    return nc


def _finish(S, nc, bufs):
    S.barrier()


def _dump(S, nc, dbg_d, buf, es):
    with ExitStack() as ph:
        st = S.sb(ph, "dbgst", [128, T], F32)
        for k in range(KC):
            S.op("dve", lambda k=k: nc.vector.tensor_copy(out=st[:], in_=buf[:, k, :]), R=[buf, st], W=[st])
            S.dma("sp", dbg_d[:, k, :], st[:], st, R=[st])
        S.barrier()


_CACHE = {}


def _prep_shared(inp):
    pp = _pack_params(inp)
    shared = {
        "pp": pp.pack(),
        "w_ada": np.ascontiguousarray(inp["w_ada"], np.float32),
        "w_in": np.ascontiguousarray(inp["w_in"], np.float32),
        "wbd": _wbd(np.asarray(inp["rg_w"], np.float32)),
        "bpat": _build_bias_patterns(np.asarray(inp["na_rpb"], np.float32)),
        "w_out": np.ascontiguousarray(inp["w_out"], np.float32),
        "w_router": np.ascontiguousarray(inp["w_router"], np.float32),
        "b_router": np.ascontiguousarray(inp["b_router"], np.float32).reshape(1, N_EXP),
        "w_gate": np.ascontiguousarray(inp["w_gate"], np.float32),
        "w_up": np.ascontiguousarray(inp["w_up"], np.float32),
        "w_down": np.ascontiguousarray(inp["w_down"], np.float32),
        "ident": np.eye(128, dtype=np.float32),
    }
    return pp, shared


def _core_inputs(inp, shared, core, nb):
    b0 = core * nb
    c = np.asarray(inp["c"], np.float32)[b0:b0 + nb]
    rows = np.concatenate([c, np.asarray(inp["c_ctx"], np.float32)[None]], axis=0)
    if nb == 1:
        rows = np.concatenate([rows[:1], rows[:1], rows[1:]], axis=0)
    condT = np.ascontiguousarray(rows.reshape(3, KC, 128).transpose(2, 1, 0))
    m = dict(shared)
    m["x"] = np.ascontiguousarray(np.asarray(inp["x"], np.float32)[b0:b0 + nb])
    m["ctx"] = np.ascontiguousarray(np.asarray(inp["ctx"], np.float32)[b0:b0 + nb])
    m["condT"] = condT
    return m


def kernel(**inputs):
    inp = {k: np.asarray(v) for k, v in inputs.items()}
    nb = inp["x"].shape[0] // N_CORES
    pp, shared = _prep_shared(inp)
    nc = build_program(pp.off, pp.n, nb=nb)
    in_maps = [_core_inputs(inp, shared, core, nb) for core in range(N_CORES)]
    res = run_bass_kernel_spmd(nc, in_maps, core_ids=list(range(N_CORES)))
    out = np.concatenate([np.asarray(r["out"]) for r in res.results], axis=0)
    return out.astype(np.float32)
```

```python
import numpy as np
from contextlib import ExitStack
import concourse.bass as bass
import concourse.mybir as mybir
from concourse.bass_utils import run_bass_kernel_spmd

F32 = mybir.dt.float32
BF16 = mybir.dt.bfloat16
AF = mybir.ActivationFunctionType
ALU = mybir.AluOpType

N_CORES = 8
D = 1024
KC = 8
SEQ = 2048
CTX = 256
T = CTX + SEQ
DEPTH = 2
IN_W = 2816
COL_Q, COL_K, COL_V, COL_RX, COL_RG, COL_SB, COL_SC, COL_SX = 0, 512, 1024, 1536, 1792, 2048, 2304, 2560
N_EXP = 16
D_EXP = 512
EPS = 1e-6
MASK = -30000.0
GRID_W = 64
ROWS = 32
WIN_R = 8
WIN_C = 16

GROUPS_ALL = [(0, 256), (256, 768), (768, 1280), (1280, 1792), (1792, 2304)]
GROUPS_LAT = GROUPS_ALL[1:]


def _r0(r):
    return min(max(r - WIN_R // 2, 0), ROWS - WIN_R)


def _na_structure():
    pats = {}
    plist = []
    per_tile = []
    for i in range(16):
        rows = set()
        for r in (2 * i, 2 * i + 1):
            rows.update(range(_r0(r), _r0(r) + WIN_R))
        tiles = sorted({kr // 2 for kr in rows})
        ent = []
        for j in tiles:
            key = []
            for a in (0, 1):
                for cq in (0, 1):
                    kr, r = 2 * j + a, 2 * i + cq
                    key.append(kr - r if _r0(r) <= kr < _r0(r) + WIN_R else None)
            key = tuple(key)
            if key not in pats:
                pats[key] = len(plist)
                plist.append(key)
            ent.append((j, pats[key]))
        per_tile.append(ent)
    return per_tile, plist


NA_TILES, NA_PATS = _na_structure()
NPAT = len(NA_PATS)


def _na_slots():
    import itertools
    seqs = []
    for ent in NA_TILES:
        sq = tuple(p for (_, p) in ent)
        if sq not in seqs:
            seqs.append(sq)

    def find(hay, needle):
        for i in range(len(hay) - len(needle) + 1):
            if tuple(hay[i:i + len(needle)]) == tuple(needle):
                return i
        return -1

    best = None
    for perm in itertools.permutations(seqs):
        cur = []
        for sq in perm:
            if find(cur, sq) >= 0:
                continue
            ov = 0
            for k in range(min(len(cur), len(sq)), 0, -1):
                if tuple(cur[-k:]) == tuple(sq[:k]):
                    ov = k
                    break
            cur = cur + list(sq[ov:])
        if best is None or len(cur) < len(best):
            best = cur
    starts = [find(best, tuple(p for (_, p) in ent)) for ent in NA_TILES]
    return best, starts


NA_SLOTS, NA_START = _na_slots()
NSLOT = len(NA_SLOTS)


def _build_bias_patterns(rpb):
    L, H = rpb.shape[0], rpb.shape[1]
    kc = np.arange(GRID_W)[:, None]
    qc = np.arange(GRID_W)[None, :]
    c_start = np.clip(qc - WIN_C // 2, 0, GRID_W - WIN_C)
    ok = (kc >= c_start) & (kc < c_start + WIN_C)
    rel_c = np.clip(kc - qc + WIN_C - 1, 0, 2 * WIN_C - 2)
    out = np.full((L, H, 128, NSLOT, 128), MASK, np.float32)
    for p, pid in enumerate(NA_SLOTS):
        key = NA_PATS[pid]
        idx = 0
        for a in (0, 1):
            for cq in (0, 1):
                dr = key[idx]
                idx += 1
                if dr is None:
                    continue
                blk = np.where(ok[None, None], rpb[:, :, dr + WIN_R - 1][:, :, rel_c], np.float32(MASK))
                out[:, :, 64 * a:64 * a + 64, p, 64 * cq:64 * cq + 64] = blk
    return out


class PP:
    def __init__(self):
        self.off = {}
        self.n = 0
        self.parts = []

    def add(self, name, arr):
        arr = np.ascontiguousarray(arr, dtype=np.float32).reshape(128, -1)
        self.off[name] = (self.n, arr.shape[1])
        self.n += arr.shape[1]
        self.parts.append(arr)

    def pack(self):
        return np.ascontiguousarray(np.concatenate(self.parts, axis=1))


def _chunked(v):
    v = np.asarray(v, np.float32)
    lead = v.shape[:-1]
    n = v.shape[-1] // 128
    v = v.reshape(*lead, n, 128)
    return np.moveaxis(v, -1, 0)


def _pack_params(inp):
    pp = PP()
    pp.add("b_ada", _chunked(inp["b_ada"]))
    pp.add("g_mix", _chunked(inp["norm_mix_g"]))
    pp.add("g_out", _chunked(inp["mix_out_g"]))
    pp.add("g_ffn", _chunked(inp["norm_ffn_g"]))
    pp.add("g_fin", _chunked(inp["final_g"]))
    pp.add("lconv_w", _chunked(inp["lru_conv_w"]))
    pp.add("lconv_b", _chunked(inp["lru_conv_b"]))
    pp.add("rg_b", _chunked(inp["rg_b"]))
    pp.add("rg_lam", _chunked(inp["rg_lam"]))
    pp.add("sconv_w", _chunked(inp["sc_conv_w"]))
    pp.add("sconv_b", _chunked(inp["sc_conv_b"]))
    return pp


def _wbd(rg_w):
    L = rg_w.shape[0]
    out = np.zeros((L, 2, 2, 2, 128, 128), np.float32)
    for c in range(2):
        for a in range(2):
            out[:, :, :, c, 64 * a:64 * a + 64, 64 * a:64 * a + 64] = rg_w[:, :, :, 2 * c + a]
    return out


class Buf:
    def __init__(self, name, t):
        self.name = name
        self.t = t
        self.all_w = None
        self.all_r = {}
        self.reg = {}
        self.sem = None
        self.semv = 0

    def __getitem__(self, idx):
        return self.t[idx]


class Sched:
    def __init__(self, nc, es):
        self.nc = nc
        self.es = es
        self.eng = {"pe": nc.tensor, "act": nc.scalar, "dve": nc.vector, "pool": nc.gpsimd, "sp": nc.sync}
        self.sem = {e: es.enter_context(nc.semaphore("s_" + e)) for e in self.eng}
        self.cnt = {e: 0 for e in self.eng}
        self.waited = {e: {} for e in self.eng}
        self.dsem = {}
        self.nbuf = 0

    def sb(self, es, name, shape, dt=F32):
        self.nbuf += 1
        return Buf(name, es.enter_context(self.nc.sbuf_tensor(f"{name}_{self.nbuf}", list(shape), dt)))

    def ps(self, es, name, shape, dt=F32):
        self.nbuf += 1
        return Buf(name, es.enter_context(self.nc.psum_tensor(f"{name}_{self.nbuf}", list(shape), dt)))

    def wrap(self, name, t):
        return Buf(name, t)

    @staticmethod
    def _norm(items):
        out = []
        for it in items:
            if isinstance(it, Buf):
                out.append((it, None))
            else:
                out.append(it)
        return out

    def _deps(self, reads, writes):
        raw, deps = [], []
        for b, k in reads:
            if b.all_w:
                raw.append(b.all_w)
            if k is None:
                for st in b.reg.values():
                    if st[0]:
                        raw.append(st[0])
            elif k in b.reg and b.reg[k][0]:
                raw.append(b.reg[k][0])
        for b, k in writes:
            if b.all_w:
                deps.append(b.all_w)
            deps.extend(b.all_r.values())
            if k is None:
                for st in b.reg.values():
                    if st[0]:
                        deps.append(st[0])
                    deps.extend(st[1].values())
            elif k in b.reg:
                st = b.reg[k]
                if st[0]:
                    deps.append(st[0])
                deps.extend(st[1].values())
        return raw, deps

    def _record(self, reads, writes, dep):
        key = dep[0]
        for b, k in reads:
            if k is None:
                b.all_r[key] = dep
            else:
                b.reg.setdefault(k, [None, {}])[1][key] = dep
        for b, k in writes:
            if k is None:
                b.all_w = dep
                b.all_r = {}
                b.reg = {}
            else:
                b.reg[k] = [dep, {}]

    def _wait(self, e, deps):
        raw, other = deps
        for lst, same_ok in ((raw, e in ("act", "dve", "pool")), (other, False)):
            for key, sem, val in lst:
                if key == e and not same_ok:
                    continue
                if self.waited[e].get(key, 0) < val:
                    self.eng[e].wait_ge(sem, val)
                    self.waited[e][key] = val

    def op(self, e, emit, R=(), W=(), inc=True):
        R = self._norm(R)
        W = self._norm(W)
        self._wait(e, self._deps(R, W))
        ins = emit()
        if inc:
            ins.then_inc(self.sem[e], 1)
            self.cnt[e] += 1
            tick = self.cnt[e]
        else:
            tick = self.cnt[e] + 1
        self._record(R, W, (e, self.sem[e], tick))

    def dma(self, q, out_ap, in_ap, semb, R=(), W=()):
        R = self._norm(R)
        W = self._norm(W)
        self._wait(q, self._deps(R, W))
        if semb.name not in self.dsem:
            self.dsem[semb.name] = [self.es.enter_context(self.nc.semaphore("d_" + semb.name)), 0]
        ds = self.dsem[semb.name]
        self.eng[q].dma_start(out=out_ap, in_=in_ap).then_inc(ds[0], 16)
        ds[1] += 16
        self._record(R, W, ("dma_" + semb.name, ds[0], ds[1]))

    def barrier(self):
        for e in self.eng:
            deps = [(f, self.sem[f], self.cnt[f]) for f in self.eng if f != e and self.cnt[f] > 0]
            deps += [("dma_" + n, ds[0], ds[1]) for n, ds in self.dsem.items() if ds[1] > 0]
            self._wait(e, ([], deps))

    def act(self, out, in_, func, R, W, **kw):
        self.op("act", lambda: self.nc.scalar.activation(out=out, in_=in_, func=func, **kw), R, W)

    def mm(self, out, lhsT, rhs, start, stop, R, W, inc=None):
        self.op("pe", lambda: self.nc.tensor.matmul(out, lhsT, rhs, start=start, stop=stop), R, W,
                inc=True)


def build_program(pp_off, npp, nb=2, depth=DEPTH, debug=None):
    nc = bass.Bass("TRN2", target_bir_lowering=False)

    def din(name, shape):
        return nc.dram_tensor(name, list(shape), F32, kind="ExternalInput").ap()

    x_d = din("x", [nb, SEQ, D])
    ctx_d = din("ctx", [nb, CTX, D])
    condT_d = din("condT", [128, KC, 3])
    pp_d = din("pp", [128, npp])
    w_ada_d = din("w_ada", [DEPTH, D, 6 * D])
    w_in_d = din("w_in", [DEPTH, D, IN_W])
    wbd_d = din("wbd", [DEPTH, 2, 2, 2, 128, 128])
    bpat_d = din("bpat", [DEPTH, 8, 128, NSLOT, 128])
    w_out_d = din("w_out", [DEPTH, D, D])
    w_router_d = din("w_router", [D, N_EXP])
    b_router_d = din("b_router", [1, N_EXP])
    w_gate_d = din("w_gate", [DEPTH, N_EXP, D, D_EXP])
    w_up_d = din("w_up", [DEPTH, N_EXP, D, D_EXP])
    w_down_d = din("w_down", [DEPTH, N_EXP, D_EXP, D])
    ident_d = din("ident", [128, 128])
    out_d = nc.dram_tensor("out", [nb, SEQ, D], F32, kind="ExternalOutput").ap()
    dbg_d = None
    if debug is not None:
        dbg_d = nc.dram_tensor("dbg", [128, KC, T], F32, kind="ExternalOutput").ap()

    V, A, P, G = nc.vector, nc.scalar, nc.tensor, nc.gpsimd

    with ExitStack() as es:
        S = Sched(nc, es)

        xT = S.sb(es, "xT", [128, KC, T], F32)
        hT = S.sb(es, "hT", [128, KC, T], BF16)
        ppt = S.sb(es, "ppt", [128, npp], F32)
        mod = S.sb(es, "mod", [128, DEPTH, 48, 3], F32)
        modv = S.sb(es, "modv", [128, DEPTH, 3, 6, KC], F32)
        ident = S.sb(es, "ident", [128, 128], F32)
        identb = S.sb(es, "identb", [128, 128], BF16)
        onesb = S.sb(es, "onesb", [128, 128], BF16)
        onesf = S.sb(es, "onesf", [128, 128], F32)
        condT = S.sb(es, "condT", [128, KC, 3], F32)
        wr_sb = S.sb(es, "wr_sb", [128, KC, N_EXP], F32)
        br_sb = S.sb(es, "br_sb", [1, N_EXP], F32)
        cl = S.sb(es, "cl", [128, DEPTH, 2, 2], F32)
        outsem = S.wrap("outsem", None)
        wbuf = S.sb(es, "wbuf", [128, KC, 384], BF16)
        wspecs = []
        for _b in range(nb):
            for _l in range(depth):
                for _c in range(2):
                    wspecs.append((_l, [COL_SB + _c * 128, COL_SC + _c * 128, COL_SX + _c * 128]))
                for _c in range(2):
                    wspecs.append((_l, [COL_RX + _c * 128, COL_RG + _c * 128]))
                for _c in range(4):
                    wspecs.append((_l, [COL_Q + _c * 128, COL_K + _c * 128, COL_V + _c * 128]))
        wnext = [0]

        def issue_w():
            if wnext[0] >= len(wspecs):
                return
            l_, cols = wspecs[wnext[0]]
            wnext[0] += 1
            for j, col in enumerate(cols):
                S.dma("pool", wbuf[:, :, j * 128:(j + 1) * 128],
                      w_in_d[l_].rearrange("(k p) n -> p k n", p=128)[:, :, col:col + 128], wbuf, W=[wbuf])

        def ppv(name, *idx):
            return ppv_shape[name](*idx)

        ppv_shape = {}

        def reg_pp(name, dims):
            o, w = pp_off[name]
            strides = []
            s = 1
            for d in reversed(dims):
                strides.insert(0, s)
                s *= d
            assert s == w, (name, dims, w)

            def f(*idx):
                col = o + sum(i * st for i, st in zip(idx, strides))
                return ppt[:, col:col + 1]
            ppv_shape[name] = f

        reg_pp("b_ada", [DEPTH, 48])
        reg_pp("g_mix", [DEPTH, KC])
        reg_pp("g_out", [DEPTH, KC])
        reg_pp("g_ffn", [DEPTH, KC])
        reg_pp("g_fin", [KC])
        reg_pp("lconv_w", [DEPTH, 4, 2])
        reg_pp("lconv_b", [DEPTH, 2])
        reg_pp("rg_b", [DEPTH, 2, 2, 2])
        reg_pp("rg_lam", [DEPTH, 2, 2])
        reg_pp("sconv_w", [DEPTH, 3, 2])
        reg_pp("sconv_b", [DEPTH, 2])

        S.dma("sp", ppt[:], pp_d[:, :], ppt, W=[ppt])
        S.dma("sp", ident[:], ident_d[:, :], ident, W=[ident])
        S.dma("sp", condT[:], condT_d[:, :, :], condT, W=[condT])
        S.dma("sp", wr_sb[:], w_router_d.rearrange("(k p) e -> p k e", p=128), wr_sb, W=[wr_sb])
        S.dma("sp", br_sb[:], b_router_d[:, :], br_sb, W=[br_sb])
        S.op("dve", lambda: V.tensor_copy(out=identb[:], in_=ident[:]), R=[ident], W=[identb])
        S.op("dve", lambda: V.memset(onesb[:], 1.0), W=[onesb])
        S.op("dve", lambda: V.memset(onesf[:], 1.0), W=[onesf])
        S.act(condT[:], condT[:], AF.Silu, R=[condT], W=[condT])
        o_lam, w_lam = pp_off["rg_lam"]
        clv = cl[:].rearrange("p a b c -> p (a b c)")
        S.act(clv, ppt[:, o_lam:o_lam + w_lam], AF.Exp, R=[ppt], W=[cl], scale=-1.0)
        S.act(clv, clv, AF.Ln, R=[cl], W=[cl], bias=1.0)
        S.op("dve", lambda: V.tensor_scalar(out=clv, in0=clv, scalar1=-8.0, scalar2=None, op0=ALU.mult), R=[cl], W=[cl])

        o_rgb, w_rgb = pp_off["rg_b"]
        nrgb = S.sb(es, "nrgb", [128, w_rgb], F32)
        S.op("dve", lambda: V.tensor_scalar(out=nrgb[:], in0=ppt[:, o_rgb:o_rgb + w_rgb], scalar1=-1.0, scalar2=None,
                                            op0=ALU.mult), R=[ppt], W=[nrgb])

        def nrgbv(l_, d_, g_, c_):
            col = ((l_ * 2 + d_) * 2 + g_) * 2 + c_
            return nrgb[:, col:col + 1]

        if debug == ("const", 0, 0):
            S.barrier()
            S.dma("sp", dbg_d[:, 0, 0:128], ident[:], ident, R=[ident])
            S.dma("sp", dbg_d[:, 1, 0:8], cl[:].rearrange("p a b c -> p (a b c)"), cl, R=[cl])
            S.dma("sp", dbg_d[:, 2, 0:24], condT[:].rearrange("p a b -> p (a b)"), condT, R=[condT])
            S.barrier()
            return nc
        with ExitStack() as ph:
            wa = [S.sb(ph, f"wa{i}", [128, KC, 768], F32) for i in range(2)]
            pmod = S.ps(ph, "pmod", [128, 512], F32)
            it = 0
            for l in range(depth):
                for cg in range(8):
                    wb = wa[it % 2]
                    it += 1
                    S.dma("sp" if it % 2 else "act", wb[:],
                          w_ada_d[l].rearrange("(k p) n -> p k n", p=128)[:, :, cg * 768:(cg + 1) * 768], wb, W=[wb])
                    for jj in range(6):
                        j = cg * 6 + jj
                        for k in range(KC):
                            S.mm(pmod[:, jj * 3:jj * 3 + 3], wb[:, k, jj * 128:(jj + 1) * 128], condT[:, k, :],
                                 start=(k == 0), stop=(k == KC - 1), R=[wb, condT], W=[pmod])
                    for jj in range(6):
                        j = cg * 6 + jj
                        S.op("dve", lambda j=j, jj=jj: V.tensor_scalar(
                            out=mod[:, l, j, :], in0=pmod[:, jj * 3:jj * 3 + 3], scalar1=ppv("b_ada", l, j),
                            scalar2=None, op0=ALU.add), R=[pmod, ppt], W=[mod])
            for l in range(depth):
                for r in range(3):
                    for (dst, mi, gname) in ((0, 1, "g_mix"), (3, 4, "g_ffn")):
                        og, _ = pp_off[gname]
                        S.op("dve", lambda l=l, r=r, dst=dst, mi=mi, og=og: V.scalar_tensor_tensor(
                            out=modv[:, l, r, dst, :], in0=mod[:, l, mi * 8:(mi + 1) * 8, r], scalar=1.0,
                            in1=ppt[:, og + l * KC:og + (l + 1) * KC], op0=ALU.add, op1=ALU.mult),
                            R=[mod, ppt], W=[modv])
                    for (dst, mi) in ((1, 0), (2, 2), (4, 3), (5, 5)):
                        S.op("dve", lambda l=l, r=r, dst=dst, mi=mi: V.tensor_copy(
                            out=modv[:, l, r, dst, :], in_=mod[:, l, mi * 8:(mi + 1) * 8, r]), R=[mod], W=[modv])
            S.barrier()

        def mv(l, r, which, k):
            return modv[:, l, r, which, k:k + 1]

        if debug == ("mod", 0, 0):
            S.dma("sp", dbg_d[:, 0, 0:DEPTH * 48 * 3], mod[:].rearrange("p a b c -> p (a b c)"), mod, R=[mod])
            S.dma("sp", dbg_d[:, 1, 0:DEPTH * 3 * 6 * KC], modv[:].rearrange("p a b c d -> p (a b c d)"), modv, R=[modv])
            S.barrier()
            return nc

        def rms_rstd(ph, src, chunks, groups, scale, name, psum, sq, rstd_buf, rkey=lambda gi: gi, gis=None):
            for gi_, (t0, t1) in enumerate(groups):
                gi = gi_ if gis is None else gis[gi_]
                n = t1 - t0
                for ci, k in enumerate(chunks):
                    S.act(sq[:, ci % 2, 0:n], src[:, k, t0:t1], AF.Square, R=[(src, rkey(gi))], W=[(sq, ci % 2)])
                    S.mm(psum[:, 0:n], onesb[:], sq[:, ci % 2, 0:n], start=(ci == 0), stop=(ci == len(chunks) - 1),
                         R=[(sq, ci % 2), onesb], W=[psum])
                S.act(rstd_buf[:, t0:t1], psum[:, 0:n], AF.Sqrt, R=[psum], W=[(rstd_buf, gi)], scale=scale, bias=EPS)
                S.op("dve", lambda t0=t0, t1=t1: V.reciprocal(out=rstd_buf[:, t0:t1], in_=rstd_buf[:, t0:t1]),
                     R=[(rstd_buf, gi)], W=[(rstd_buf, gi)])

        def gkey(groups, gi):
            return GROUPS_ALL.index(groups[gi])

        def dbgdump(slot, ap, n, Rbufs, parts=128):
            with ExitStack() as dph:
                n = min(n, 2048)
                st = S.sb(dph, "dbgst", [128, 2048], F32)
                S.op("dve", lambda: V.tensor_copy(out=st[0:parts, 0:n], in_=ap[:, 0:n]), R=Rbufs, W=[st])
                S.dma("sp", dbg_d[0:parts, slot, 0:n], st[0:parts, 0:n], st, R=[st])
                S.barrier()

        issue_w()
        for b in range(nb):
            with ExitStack() as ph:
                stg = [S.sb(ph, f"stg{i}", [128, D], F32) for i in range(2)]
                ptr = [S.ps(ph, f"ptr{i}", [128, 512], F32) for i in range(4)]
                for tt in range(T // 128):
                    st = stg[tt % 2]
                    src = ctx_d[b, tt * 128:(tt + 1) * 128, :] if tt < 2 else x_d[b, (tt - 2) * 128:(tt - 1) * 128, :]
                    S.dma("sp", st[:], src, st, W=[st])
                    for hf in range(2):
                        pt = ptr[(tt * 2 + hf) % 4]
                        for kk in range(4):
                            k = hf * 4 + kk
                            S.op("pe", lambda pt=pt, kk=kk, k=k, st=st: P.transpose(
                                pt[:, kk * 128:(kk + 1) * 128], st[:, k * 128:(k + 1) * 128], ident[:]),
                                R=[st, ident], W=[pt])
                        eng = "act" if hf == 0 else "dve"
                        dst = xT[:, hf * 4:hf * 4 + 4, tt * 128:(tt + 1) * 128]
                        srcp = pt[:].rearrange("p (k t) -> p k t", t=128)
                        if eng == "act":
                            S.act(dst, srcp, AF.Identity, R=[pt], W=[(xT, tt)])
                        else:
                            S.op("dve", lambda dst=dst, srcp=srcp: V.tensor_copy(out=dst, in_=srcp), R=[pt], W=[(xT, tt)])
                S.barrier()

            if debug == ("load", b, 0):
                _dump(S, nc, dbg_d, xT, es)
                return nc
            for l in range(depth):
                last = (l == DEPTH - 1)
                groups_out = GROUPS_LAT if last else GROUPS_ALL

                def norm_mod(ph, which_g, which_s, groups):
                    sq = S.sb(ph, "sq", [128, 2, 512], BF16)
                    rstd = S.sb(ph, "rstd", [128, T], F32)
                    tmp = S.sb(ph, "ntmp", [128, 2, 512], F32)
                    psn = S.ps(ph, "psn", [128, 512], F32)
                    def stats(gi):
                        rms_rstd(ph, xT, list(range(KC)), [groups[gi]], 1.0 / D, "n", psn, sq, rstd, gis=[gi])
                    stats(0)
                    for gi, (t0, t1) in enumerate(groups):
                        n = t1 - t0
                        r = 2 if t0 < CTX else b
                        if gi + 1 < len(groups):
                            stats(gi + 1)
                        for k in range(KC):
                            tb = k % 2
                            S.op("dve", lambda k=k, t0=t0, t1=t1, n=n, r=r, tb=tb: V.scalar_tensor_tensor(
                                out=tmp[:, tb, 0:n], in0=xT[:, k, t0:t1], scalar=mv(l, r, which_g, k),
                                in1=rstd[:, t0:t1], op0=ALU.mult, op1=ALU.mult),
                                R=[(xT, None), (rstd, gi), modv], W=[(tmp, tb)])
                            S.act(hT[:, k, t0:t1], tmp[:, tb, 0:n], AF.Identity, R=[(tmp, tb), modv],
                                  W=[(hT, gkey(groups, gi))], bias=mv(l, r, which_s, k))

                with ExitStack() as ph:
                    norm_mod(ph, 0, 1, GROUPS_ALL)
                    S.barrier()

                if debug == ("h1", b, l):
                    _dump(S, nc, dbg_d, hT, es)
                    return nc

                with ExitStack() as mx:
                    yT = S.sb(mx, "yT", [128, KC, T], BF16)

                    def proj_fm(ph, wsb, wcol0, m, dst_fn, groups, ps_list, base=0):
                        for gi, (t0, t1) in enumerate(groups):
                            n = t1 - t0
                            pt = ps_list[gi % len(ps_list)]
                            for k in range(KC):
                                S.mm(pt[0:m, 0:n], wsb[:, k, wcol0:wcol0 + m], hT[:, k, t0:t1], start=(k == 0),
                                     stop=(k == KC - 1), R=[wsb, (hT, gkey(groups, gi))], W=[pt])
                            dst_fn(gi, t0, t1, pt)

                    for c in range(2):
                        with ExitStack() as ph:
                            wsb = wbuf
                            sbv = S.sb(ph, "sbv", [128, T], F32)
                            pv = S.sb(ph, "pv", [128, T], F32)
                            acc = S.sb(ph, "acc", [128, T], F32)
                            pss = [S.ps(ph, f"pss{i}", [128, 512], F32) for i in range(4)]

                            def ev_sb(gi, t0, t1, pt):
                                S.act(sbv[:, t0:t1], pt[:, 0:t1 - t0], AF.Identity, R=[pt], W=[(sbv, gi)])

                            def ev_sc(gi, t0, t1, pt):
                                S.act(pv[:, t0:t1], pt[:, 0:t1 - t0], AF.Identity, R=[pt], W=[(pv, gi)])

                            def ev_sx(gi, t0, t1, pt):
                                S.op("dve", lambda: V.tensor_tensor(out=pv[:, t0:t1], in0=pt[:, 0:t1 - t0],
                                                                    in1=pv[:, t0:t1], op=ALU.mult),
                                     R=[pt, (pv, gi)], W=[(pv, gi)])

                            proj_fm(ph, wsb, 0, 128, ev_sb, GROUPS_ALL, pss)
                            proj_fm(ph, wsb, 128, 128, ev_sc, GROUPS_ALL, pss)
                            proj_fm(ph, wsb, 256, 128, ev_sx, GROUPS_ALL, pss)
                            issue_w()
                            for (s0, s1) in ((0, CTX), (CTX, T)):
                                S.op("dve", lambda s0=s0, s1=s1: V.tensor_scalar(
                                    out=acc[:, s0:s1], in0=pv[:, s0:s1], scalar1=ppv("sconv_w", l, 1, c),
                                    scalar2=ppv("sconv_b", l, c), op0=ALU.mult, op1=ALU.add), R=[pv, ppt], W=[acc])
                                S.op("dve", lambda s0=s0, s1=s1: V.scalar_tensor_tensor(
                                    out=acc[:, s0 + 1:s1], in0=pv[:, s0:s1 - 1], scalar=ppv("sconv_w", l, 0, c),
                                    in1=acc[:, s0 + 1:s1], op0=ALU.mult, op1=ALU.add), R=[pv, ppt, acc], W=[acc])
                                S.op("dve", lambda s0=s0, s1=s1: V.scalar_tensor_tensor(
                                    out=acc[:, s0:s1 - 1], in0=pv[:, s0 + 1:s1], scalar=ppv("sconv_w", l, 2, c),
                                    in1=acc[:, s0:s1 - 1], op0=ALU.mult, op1=ALU.add), R=[pv, ppt, acc], W=[acc])
                            S.op("dve", lambda: V.tensor_tensor(out=yT[:, 6 + c, :], in0=sbv[:], in1=acc[:], op=ALU.mult),
                                 R=[sbv, acc], W=[yT])
                            S.barrier()

                    for c in range(2):
                        with ExitStack() as ph:
                            wsb = wbuf
                            wg = S.sb(ph, "wbd", [128, 2, 2, 128], F32)
                            for d in range(2):
                                for g in range(2):
                                    S.dma("sp", wg[:, d, g, :], wbd_d[l, d, g, c, :, :], wg, W=[wg])
                            rxc = S.sb(ph, "rxc", [128, T], F32)
                            a_f = S.sb(ph, "a_f", [128, T], F32)
                            a_r = S.sb(ph, "a_r", [128, T], F32)
                            bb = S.sb(ph, "bb", [128, T], F32)
                            gt = S.sb(ph, "gt", [128, 4, 512], F32)
                            psl = [S.ps(ph, f"psl{i}", [128, 512], F32) for i in range(4)]
                            rx_raw = a_f

                            def ev_rx(gi, t0, t1, pt):
                                S.act(rx_raw[:, t0:t1], pt[:, 0:t1 - t0], AF.Identity, R=[pt], W=[(rx_raw, gi)])

                            def ev_rg(gi, t0, t1, pt):
                                S.act(yT[:, 4 + c, t0:t1], pt[:, 0:t1 - t0], AF.Gelu, R=[pt], W=[(yT, gi)])

                            proj_fm(ph, wsb, 0, 128, ev_rx, GROUPS_ALL, psl)
                            proj_fm(ph, wsb, 128, 128, ev_rg, GROUPS_ALL, psl)
                            issue_w()
                            for (s0, s1) in ((0, CTX), (CTX, T)):
                                S.op("dve", lambda s0=s0, s1=s1: V.tensor_scalar(
                                    out=rxc[:, s0:s1], in0=rx_raw[:, s0:s1], scalar1=ppv("lconv_w", l, 2, c),
                                    scalar2=ppv("lconv_b", l, c), op0=ALU.mult, op1=ALU.add), R=[rx_raw, ppt], W=[rxc])
                                for (kk, sh) in ((0, 2), (1, 1)):
                                    S.op("dve", lambda s0=s0, s1=s1, kk=kk, sh=sh: V.scalar_tensor_tensor(
                                        out=rxc[:, s0 + sh:s1], in0=rx_raw[:, s0:s1 - sh], scalar=ppv("lconv_w", l, kk, c),
                                        in1=rxc[:, s0 + sh:s1], op0=ALU.mult, op1=ALU.add), R=[rx_raw, ppt, rxc], W=[rxc])
                                S.op("dve", lambda s0=s0, s1=s1: V.scalar_tensor_tensor(
                                    out=rxc[:, s0:s1 - 1], in0=rx_raw[:, s0 + 1:s1], scalar=ppv("lconv_w", l, 3, c),
                                    in1=rxc[:, s0:s1 - 1], op0=ALU.mult, op1=ALU.add), R=[rx_raw, ppt, rxc], W=[rxc])
                            for d in range(2):
                                adst = a_f if d == 0 else a_r
                                for gi, (t0, t1) in enumerate(GROUPS_ALL):
                                    n = t1 - t0
                                    if d == 0:
                                        q0, q1 = t0, t1
                                    elif t0 < CTX:
                                        q0, q1 = 0, CTX
                                    else:
                                        q0 = CTX + (T - t1)
                                        q1 = q0 + n
                                    pr, pi = psl[0 + 2 * (gi % 2)], psl[1 + 2 * (gi % 2)]
                                    S.mm(pr[:, 0:n], wg[:, d, 0, :], rxc[:, t0:t1], True, True, R=[wg, rxc], W=[pr])
                                    S.mm(pi[:, 0:n], wg[:, d, 1, :], rxc[:, t0:t1], True, True, R=[wg, rxc], W=[pi])
                                    rr, ii, mm_, tt_ = gt[:, 0, 0:n], gt[:, 1, 0:n], gt[:, 2, 0:n], gt[:, 3, 0:n]
                                    S.act(rr, pr[:, 0:n], AF.Sigmoid, R=[pr, ppt], W=[(gt, 0)], bias=ppv("rg_b", l, d, 0, c))
                                    S.act(ii, pi[:, 0:n], AF.Sigmoid, R=[pi, ppt], W=[(gt, 1)], bias=ppv("rg_b", l, d, 1, c))

                                    def rv(ap):
                                        return ap[:, ::-1] if d == 1 else ap
                                    S.act(adst[:, q0:q1], rv(rr), AF.Exp, R=[(gt, 0), cl], W=[(adst, gi)],
                                          scale=cl[:, l, d, c:c + 1])
                                    S.op("pool", lambda mm_=mm_, q0=q0, q1=q1, adst=adst: G.tensor_tensor(
                                        out=mm_, in0=adst[:, q0:q1], in1=adst[:, q0:q1], op=ALU.mult),
                                        R=[(adst, gi)], W=[(gt, 2)])
                                    S.act(mm_, mm_, AF.Sqrt, R=[(gt, 2)], W=[(gt, 2)], scale=-1.0, bias=1.0)
                                    S.op("dve", lambda tt_=tt_, ii=ii, t0=t0, t1=t1: V.tensor_tensor(
                                        out=tt_, in0=ii, in1=rxc[:, t0:t1], op=ALU.mult), R=[(gt, 1), rxc], W=[(gt, 3)])
                                    S.op("dve", lambda tt_=tt_, mm_=mm_, q0=q0, q1=q1, rv=rv: V.tensor_tensor(
                                        out=bb[:, q0:q1], in0=mm_, in1=rv(tt_), op=ALU.mult),
                                        R=[(gt, 2), (gt, 3)], W=[(bb, gi)])
                                if debug == ("lru", b, l) and c == 0 and d == 0:
                                    dbgdump(0, rxc[:, :], T, [rxc])
                                    dbgdump(1, a_f[:, :], T, [a_f])
                                    dbgdump(2, bb[:, :], T, [bb])
                                    dbgdump(3, gt[:, :, :].rearrange("p a b -> p (a b)"), 2048, [gt])
                                    dbgdump(4, cl[:].rearrange("p a b c -> p (a b c)"), 8, [cl])
                                S.op("dve", lambda adst=adst: V.tensor_tensor_scan(
                                    out=adst[:, :], data0=adst[:, :], data1=bb[:, :], initial=0.0,
                                    op0=ALU.mult, op1=ALU.add), R=[adst, bb], W=[adst])
                            if debug == ("lru", b, l) and c == 0:
                                dbgdump(5, a_f[:, :], T, [a_f])
                                dbgdump(6, a_r[:, :], T, [a_r])
                                dbgdump(7, yT[:, 4 + c, :], T, [yT])
                                return nc
                            for (s0, s1) in ((0, CTX), (CTX, T)):
                                S.op("dve", lambda s0=s0, s1=s1: V.tensor_tensor(
                                    out=bb[:, s0:s1], in0=a_f[:, s0:s1], in1=a_r[:, s0:s1][:, ::-1], op=ALU.add),
                                    R=[a_f, a_r], W=[bb])
                            S.op("dve", lambda: V.tensor_tensor(out=yT[:, 4 + c, :], in0=bb[:], in1=yT[:, 4 + c, :],
                                                                op=ALU.mult), R=[bb, yT], W=[yT])
                            S.barrier()

                    qtiles = list(range(0 if not last else 2, T // 128))
                    with ExitStack() as nas:
                        bpb = [S.sb(nas, f"bp{i}", [128, 2, NSLOT, 128], BF16) for i in range(2)]

                        def load_bp(c_):
                            for hh_ in range(2):
                                S.dma("pool", bpb[c_ % 2][:, hh_, :, :], bpat_d[l, 2 * c_ + hh_, :, :, :], bpb[c_ % 2],
                                      W=[bpb[c_ % 2]])

                        load_bp(0)
                        for c in range(4):
                            with ExitStack() as ph:
                                wsb = wbuf
                                bp = bpb[c % 2]
                                QT = S.sb(ph, "QT", [128, T], BF16)
                                KT = S.sb(ph, "KT", [128, T], BF16)
                                Vp = S.sb(ph, "Vp", [128, T // 128, 2, 65], BF16)
                                PT = [S.sb(ph, f"PT{i}", [128, 7 * 128], BF16) for i in range(2)]
                                otok = [S.sb(ph, f"otok{i}", [128, 2, 64], BF16) for i in range(2)]
                                rec = [S.sb(ph, f"rec{i}", [128, 2, 1], F32) for i in range(2)]
                                psq = [S.ps(ph, f"psq{i}", [128, 512], F32) for i in range(2)]
                                pst = [S.ps(ph, f"pst{i}", [128, 1024], F32) for i in range(2)]
                                pso = S.ps(ph, "pso", [128, 2, 2, 65], F32)
                                pstr = S.ps(ph, "pstr", [128, 128], BF16)
                                S.op("dve", lambda: V.memset(Vp[:, :, :, 64:65], 1.0), W=[Vp])

                                def ev_q(gi, t0, t1, pt):
                                    S.act(QT[:, t0:t1], pt[:, 0:t1 - t0], AF.Identity, R=[pt], W=[(QT, gi)], scale=0.125)

                                def ev_k(gi, t0, t1, pt):
                                    S.op("dve", lambda: V.tensor_copy(out=KT[:, t0:t1], in_=pt[:, 0:t1 - t0]),
                                         R=[pt], W=[(KT, gi)])
                                proj_fm(ph, wsb, 0, 128, ev_q, GROUPS_LAT if last else GROUPS_ALL, psq)
                                proj_fm(ph, wsb, 128, 128, ev_k, GROUPS_ALL, psq)
                                for tt in range(T // 128):
                                    pt = psq[tt % 2]
                                    for k in range(KC):
                                        S.mm(pt[:, 0:128], hT[:, k, tt * 128:(tt + 1) * 128], wsb[:, k, 256:384], start=(k == 0),
                                             stop=(k == KC - 1), R=[wsb, (hT, None)], W=[pt])
                                    S.act(Vp[:, tt, :, 0:64], pt[:, 0:128].rearrange("p (h d) -> p h d", d=64), AF.Identity,
                                          R=[pt], W=[(Vp, tt)])
                                issue_w()
                                if c + 1 < 4:
                                    load_bp(c + 1)
                                bpf = bp[:].rearrange("p a b c -> p (a b c)")
                                S.act(bpf, bpf, AF.Exp, R=[bp], W=[bp])
                                pairs = [(qi, qt, hh) for qi, qt in enumerate(qtiles) for hh in range(2)]

                                def blk(qt):
                                    if qt < 2:
                                        return [0, 1], 0, 0
                                    return ([0, 1] + [2 + j for (j, p) in NA_TILES[qt - 2]], len(NA_TILES[qt - 2]),
                                            NA_START[qt - 2])

                                def scores(i):
                                    qi, qt, hh = pairs[i]
                                    blocks, nl, s0 = blk(qt)
                                    nbk = len(blocks)
                                    ps_s = pst[i % 2]
                                    ptb = PT[i % 2]
                                    for bi, kt in enumerate(blocks):
                                        S.mm(ps_s[:, bi * 128:(bi + 1) * 128], KT[hh * 64:(hh + 1) * 64, kt * 128:(kt + 1) * 128],
                                             QT[hh * 64:(hh + 1) * 64, qt * 128:(qt + 1) * 128], start=True, stop=True,
                                             R=[(KT, None), (QT, None)], W=[ps_s])
                                    for (c0, c1) in ((0, min(nbk, 4)), (4, nbk)):
                                        if c1 > c0:
                                            S.act(ptb[:, c0 * 128:c1 * 128], ps_s[:, c0 * 128:c1 * 128], AF.Exp, R=[ps_s], W=[ptb])
                                    if nl:
                                        S.op("dve", lambda ptb=ptb, hh=hh, nl=nl, s0=s0: V.tensor_tensor(
                                            out=ptb[:, 256:256 + nl * 128].rearrange("p (a b) -> p a b", b=128),
                                            in0=ptb[:, 256:256 + nl * 128].rearrange("p (a b) -> p a b", b=128),
                                            in1=bp[:, hh, s0:s0 + nl, :], op=ALU.mult), R=[ptb, bp], W=[ptb])

                                def pv(i):
                                    qi, qt, hh = pairs[i]
                                    blocks, nl, s0 = blk(qt)
                                    nbk = len(blocks)
                                    ptb = PT[i % 2]
                                    po = pso[:, qi % 2]
                                    for bi, kt in enumerate(blocks):
                                        S.mm(po[:, hh, :], ptb[:, bi * 128:(bi + 1) * 128], Vp[:, kt, hh, :], start=(bi == 0),
                                             stop=(bi == nbk - 1), R=[ptb, (Vp, None)], W=[(pso, qi % 2)])

                                def normalise(qi, qt):
                                    po = pso[:, qi % 2]
                                    pok = (pso, qi % 2)
                                    rc, ot = rec[qi % 2], otok[qi % 2]
                                    S.op("dve", lambda rc=rc, po=po: V.reciprocal(out=rc[:], in_=po[:, :, 64:65]), R=[pok], W=[rc])
                                    S.op("dve", lambda rc=rc, po=po, ot=ot: V.tensor_tensor(
                                        out=ot[:], in0=po[:, :, 0:64], in1=rc[:].to_broadcast([128, 2, 64]), op=ALU.mult),
                                        R=[pok, rc], W=[ot])

                                def to_fm(qi, qt):
                                    ot = otok[qi % 2]
                                    S.op("pe", lambda ot=ot: P.transpose(pstr[:], ot[:].rearrange("p h d -> p (h d)"), identb[:]),
                                         R=[ot, identb], W=[pstr])
                                    S.act(yT[:, c, qt * 128:(qt + 1) * 128], pstr[:], AF.Identity, R=[pstr], W=[(yT, ("na", qt))])

                                scores(0)
                                deferred = None
                                for i in range(len(pairs)):
                                    if i + 1 < len(pairs):
                                        scores(i + 1)
                                    if deferred is not None:
                                        to_fm(*deferred)
                                        deferred = None
                                    pv(i)
                                    qi, qt, hh = pairs[i]
                                    if hh == 1:
                                        normalise(qi, qt)
                                        deferred = (qi, qt)
                                if deferred is not None:
                                    to_fm(*deferred)
                                S.barrier()

                    if debug == ("y", b, l):
                        _dump(S, nc, dbg_d, yT, es)
                        return nc

                    with ExitStack() as ph:
                        wo = S.sb(ph, "wo", [128, KC, D], BF16)
                        for hf in range(2):
                            S.dma("pool", wo[:, :, hf * 512:(hf + 1) * 512],
                                  w_out_d[l].rearrange("(k p) n -> p k n", p=128)[:, :, hf * 512:(hf + 1) * 512], wo, W=[wo])
                        sq = S.sb(ph, "sq", [128, 2, 512], BF16)
                        rstds = [S.sb(ph, f"rstd{i}", [128, T], F32) for i in range(3)]
                        psn = S.ps(ph, "psn", [128, 512], F32)
                        pso2 = [S.ps(ph, f"pso2{i}", [128, 512], F32) for i in range(4)]
                        grp_chunks = ([0, 1, 2, 3], [4, 5], [6, 7])
                        def ostats(gi):
                            for gidx, chunks in enumerate(grp_chunks):
                                rms_rstd(ph, yT, chunks, [groups_out[gi]], 1.0 / (128 * len(chunks)), "g", psn, sq, rstds[gidx],
                                         rkey=lambda gi_: ("o", gi_), gis=[gi])
                        ostats(0)
                        for gi, (t0, t1) in enumerate(groups_out):
                            n = t1 - t0
                            r = 2 if t0 < CTX else b
                            if gi + 1 < len(groups_out):
                                ostats(gi + 1)
                            for gidx, chunks in enumerate(grp_chunks):
                                for k in chunks:
                                    S.op("dve", lambda k=k, t0=t0, t1=t1, gidx=gidx: V.scalar_tensor_tensor(
                                        out=yT[:, k, t0:t1], in0=yT[:, k, t0:t1], scalar=ppv("g_out", l, k),
                                        in1=rstds[gidx][:, t0:t1], op0=ALU.mult, op1=ALU.mult),
                                        R=[(yT, ("o", gi)), (rstds[gidx], gi), ppt], W=[(yT, ("o", gi))])
                            for oc in range(KC):
                                pt = pso2[oc % 4]
                                for k in range(KC):
                                    S.mm(pt[:, 0:n], wo[:, k, oc * 128:(oc + 1) * 128], yT[:, k, t0:t1], start=(k == 0),
                                         stop=(k == KC - 1), R=[wo, (yT, ("o", gi))], W=[pt])
                                S.op("dve", lambda oc=oc, t0=t0, t1=t1, n=n, r=r, pt=pt: V.scalar_tensor_tensor(
                                    out=xT[:, oc, t0:t1], in0=pt[:, 0:n], scalar=mv(l, r, 2, oc), in1=xT[:, oc, t0:t1],
                                    op0=ALU.mult, op1=ALU.add), R=[pt, (xT, None), modv], W=[(xT, None)])
                        S.barrier()

                if debug == ("xmix", b, l):
                    _dump(S, nc, dbg_d, xT, es)
                    return nc

                with ExitStack() as ph:
                    ng = len(groups_out)
                    tok0 = groups_out[0][0]
                    comb = S.sb(ph, "comb", [N_EXP, T], F32)
                    with ExitStack() as ph2:
                        sq = S.sb(ph2, "sq", [128, 2, 512], BF16)
                        rstd = S.sb(ph2, "rstd", [128, T], F32)
                        tmp = S.sb(ph2, "ntmp", [128, 2, 512], F32)
                        hfb = S.sb(ph2, "hfb", [128, KC, 512], F32)
                        psn = S.ps(ph2, "psn", [128, 512], F32)
                        psr = [S.ps(ph2, f"psr{i}", [128, 512], F32) for i in range(2)]
                        pscT = S.ps(ph2, "pscT", [N_EXP, 512], F32)
                        rts = [S.sb(ph2, f"rt{i}", [128, 16, 16], F32) for i in range(2)]
                        rms_rstd(ph2, xT, list(range(KC)), groups_out, 1.0 / D, "n2", psn, sq, rstd)
                        tile_i = 0
                        for gi, (t0, t1) in enumerate(groups_out):
                            n = t1 - t0
                            r = 2 if t0 < CTX else b
                            for k in range(KC):
                                tb = k % 2
                                S.op("dve", lambda k=k, t0=t0, t1=t1, n=n, r=r, tb=tb: V.scalar_tensor_tensor(
                                    out=tmp[:, tb, 0:n], in0=xT[:, k, t0:t1], scalar=mv(l, r, 3, k),
                                    in1=rstd[:, t0:t1], op0=ALU.mult, op1=ALU.mult),
                                    R=[(xT, None), (rstd, gi), modv], W=[(tmp, tb)])
                                S.act(hfb[:, k, 0:n], tmp[:, tb, 0:n], AF.Identity, R=[(tmp, tb), modv],
                                      W=[(hfb, k)], bias=mv(l, r, 4, k))
                                S.op("pool", lambda k=k, t0=t0, t1=t1, n=n: G.tensor_copy(
                                    out=hT[:, k, t0:t1], in_=hfb[:, k, 0:n]), R=[(hfb, k)], W=[(hT, None)])
                            for ti in range(n // 128):
                                pr = psr[tile_i % 2]
                                rt = rts[tile_i % 2]
                                tile_i += 1
                                for k in range(KC):
                                    S.mm(pr[:, 0:N_EXP], hfb[:, k, ti * 128:(ti + 1) * 128], wr_sb[:, k, :], start=(k == 0),
                                         stop=False, R=[(hfb, None), wr_sb], W=[pr])
                                S.mm(pr[:, 0:N_EXP], onesf[0:1, :], br_sb[:, :], start=False, stop=True, R=[onesf, br_sb], W=[pr])

                                def dv(fn, rt=rt):
                                    S.op("dve", fn, R=[rt], W=[rt])

                                def row(i, w=4, rt=rt):
                                    return rt[:, i, 0:w]
                                lg, ev, cw = row(0, 16), row(2, 16), row(15, 16)
                                nmx, gmax = rt[:, 1, 0:1], rt[:, 1, 1:2]
                                S.op("dve", lambda lg=lg, pr=pr: V.tensor_copy(out=lg, in_=pr[:, 0:N_EXP]), R=[pr], W=[rt])
                                dv(lambda: V.tensor_reduce(out=nmx, in_=lg, axis=mybir.AxisListType.X, op=ALU.max))
                                dv(lambda: V.tensor_scalar(out=nmx, in0=nmx, scalar1=-1.0, scalar2=None, op0=ALU.mult))
                                S.act(ev, lg, AF.Exp, R=[rt], W=[rt], bias=nmx)
                                e3 = ev.rearrange("p (g j) -> p g j", j=4)
                                a_, b_, c_, d_ = (e3[:, :, j] for j in range(4))

                                def tt2(out, i0, i1, op):
                                    dv(lambda: V.tensor_tensor(out=out, in0=i0, in1=i1, op=op))
                                tt2(row(3), a_, b_, ALU.max)
                                tt2(row(4), c_, d_, ALU.max)
                                tt2(row(5), a_, b_, ALU.min)
                                tt2(row(6), c_, d_, ALU.min)
                                tt2(row(7), row(3), row(4), ALU.max)
                                tt2(row(8), row(3), row(4), ALU.min)
                                tt2(row(9), row(5), row(6), ALU.max)
                                tt2(row(10), row(8), row(9), ALU.max)
                                tt2(row(11), row(7), row(10), ALU.add)
                                dv(lambda: V.tensor_reduce(out=gmax, in_=row(11), axis=mybir.AxisListType.X, op=ALU.max))
                                dv(lambda: V.tensor_scalar(out=row(12), in0=row(11), scalar1=gmax, scalar2=None, op0=ALU.is_equal))
                                dv(lambda: V.reciprocal(out=row(13), in_=row(11)))
                                tt2(row(14), row(12), row(13), ALU.mult)
                                cw3 = cw.rearrange("p (g j) -> p g j", j=4)
                                top2b = row(10).rearrange("p (g o) -> p g o", o=1).to_broadcast([128, 4, 4])
                                facb = row(14).rearrange("p (g o) -> p g o", o=1).to_broadcast([128, 4, 4])
                                tt2(cw3, e3, top2b, ALU.is_ge)
                                tt2(cw3, cw3, e3, ALU.mult)
                                tt2(cw3, cw3, facb, ALU.mult)
                                S.op("pe", lambda cw=cw, ti=ti: P.transpose(pscT[:, ti * 128:(ti + 1) * 128], cw, ident[:]),
                                     R=[rt, ident], W=[pscT])
                            S.act(comb[:, t0:t1], pscT[:, 0:n], AF.Identity, R=[pscT], W=[(comb, gi)])
                        S.barrier()

                    if debug == ("comb", b, l):
                        S.dma("sp", dbg_d[0:N_EXP, 0, :], comb[:, :], comb, R=[comb])
                        S.barrier()
                        return nc

                    wts = [[S.sb(ph, f"wg{i}", [128, KC, D_EXP], BF16), S.sb(ph, f"wu{i}", [128, KC, D_EXP], BF16),
                            S.sb(ph, f"wd{i}", [128, 4, D], BF16)] for i in range(2)]
                    cm = [S.sb(ph, f"cm{i}", [N_EXP, 512], F32) for i in range(2)]
                    cbc = [S.sb(ph, f"cbc{i}", [128, 512], F32) for i in range(2)]
                    sg = [S.sb(ph, f"sg{i}", [128, 512], F32) for i in range(2)]
                    hid = [S.sb(ph, f"hid{i}", [128, 4, 512], BF16) for i in range(2)]
                    psg = [S.ps(ph, f"psg{i}", [128, 512], F32) for i in range(2)]
                    psu = [S.ps(ph, f"psu{i}", [128, 512], F32) for i in range(2)]
                    psd = [S.ps(ph, f"psd{i}", [128, 512], F32) for i in range(3)]
                    psc = S.ps(ph, "psc", [128, 512], F32)

                    def load_expert(e):
                        wgt, wut, wdt = wts[e % 2]
                        S.dma("pool", wgt[:], w_gate_d[l, e].rearrange("(k p) n -> p k n", p=128), wgt, W=[wgt])
                        S.dma("pool", wut[:], w_up_d[l, e].rearrange("(k p) n -> p k n", p=128), wut, W=[wut])
                        S.dma("pool", wdt[:], w_down_d[l, e].rearrange("(k p) n -> p k n", p=128), wdt, W=[wdt])

                    it = 0
                    load_expert(0)
                    for e in range(N_EXP):
                        wgt, wut, wdt = wts[e % 2]
                        if e + 1 < N_EXP:
                            load_expert(e + 1)
                        for gi, (t0, t1) in enumerate(groups_out):
                            n = t1 - t0
                            r = 2 if t0 < CTX else b
                            cb, hd, cmt = cbc[it % 2], hid[it % 2], cm[it % 2]
                            it += 1
                            S.op("dve", lambda cmt=cmt, t0=t0, t1=t1, n=n, e=e: V.tensor_scalar(
                                out=cmt[:, 0:n], in0=comb[:, t0:t1], scalar1=ident[0:N_EXP, e:e + 1], scalar2=None,
                                op0=ALU.mult), R=[(comb, gi), ident], W=[cmt])
                            S.mm(psc[:, 0:n], onesf[0:N_EXP, :], cmt[:, 0:n], True, True, R=[onesf, cmt], W=[psc])
                            S.act(cb[:, 0:n], psc[:, 0:n], AF.Identity, R=[psc], W=[cb])
                            for hc in range(4):
                                pg, pu, sgt = psg[hc % 2], psu[hc % 2], sg[hc % 2]
                                for k in range(KC):
                                    S.mm(pg[:, 0:n], wgt[:, k, hc * 128:(hc + 1) * 128], hT[:, k, t0:t1], start=(k == 0),
                                         stop=(k == KC - 1), R=[wgt, (hT, None)], W=[pg])
                                for k in range(KC):
                                    S.mm(pu[:, 0:n], wut[:, k, hc * 128:(hc + 1) * 128], hT[:, k, t0:t1], start=(k == 0),
                                         stop=(k == KC - 1), R=[wut, (hT, None)], W=[pu])
                                S.act(sgt[:, 0:n], pg[:, 0:n], AF.Silu, R=[pg], W=[sgt])
                                S.op("dve", lambda sgt=sgt, cb=cb, n=n: V.tensor_tensor(
                                    out=sgt[:, 0:n], in0=sgt[:, 0:n], in1=cb[:, 0:n], op=ALU.mult), R=[sgt, cb], W=[sgt])
                                S.op("dve", lambda hd=hd, hc=hc, pu=pu, sgt=sgt, n=n: V.tensor_tensor(
                                    out=hd[:, hc, 0:n], in0=pu[:, 0:n], in1=sgt[:, 0:n], op=ALU.mult),
                                    R=[pu, sgt], W=[(hd, hc)])
                            for oc in range(KC):
                                pd = psd[oc % 3]
                                for hc in range(4):
                                    S.mm(pd[:, 0:n], wdt[:, hc, oc * 128:(oc + 1) * 128], hd[:, hc, 0:n], start=(hc == 0),
                                         stop=(hc == 3), R=[wdt, (hd, hc)], W=[pd])
                                S.op("dve", lambda oc=oc, t0=t0, t1=t1, n=n, r=r, pd=pd: V.scalar_tensor_tensor(
                                    out=xT[:, oc, t0:t1], in0=pd[:, 0:n], scalar=mv(l, r, 5, oc), in1=xT[:, oc, t0:t1],
                                    op0=ALU.mult, op1=ALU.add), R=[pd, (xT, gi), modv], W=[(xT, gi)])
                    S.barrier()

                if debug == ("xmoe", b, l):
                    _dump(S, nc, dbg_d, xT, es)
                    return nc

            with ExitStack() as ph:
                sq = S.sb(ph, "sq", [128, 2, 512], BF16)
                rstd = S.sb(ph, "rstd", [128, T], F32)
                xn = [S.sb(ph, f"xn{i}", [128, KC, 512], F32) for i in range(1)]
                ost = [S.sb(ph, f"ost{i}", [128, D], F32) for i in range(2)]
                psn = S.ps(ph, "psn", [128, 512], F32)
                ptr = [S.ps(ph, f"ptr{i}", [128, 512], F32) for i in range(4)]
                def fstats(gi):
                    rms_rstd(ph, xT, list(range(KC)), [GROUPS_LAT[gi]], 1.0 / D, "f", psn, sq, rstd, gis=[gi])
                fstats(0)
                oi = 0
                for gi, (t0, t1) in enumerate(GROUPS_LAT):
                    n = t1 - t0
                    xnb = xn[0]
                    if gi + 1 < len(GROUPS_LAT):
                        fstats(gi + 1)
                    for k in range(KC):
                        S.op("dve", lambda k=k, t0=t0, t1=t1, n=n, xnb=xnb: V.scalar_tensor_tensor(
                            out=xnb[:, k, 0:n], in0=xT[:, k, t0:t1], scalar=ppv("g_fin", k), in1=rstd[:, t0:t1],
                            op0=ALU.mult, op1=ALU.mult), R=[(xT, None), (rstd, gi), ppt], W=[(xnb, None)])
                    for ti in range(n // 128):
                        ot = ost[oi % 2]
                        for hf in range(2):
                            pt = ptr[(oi * 2 + hf) % 4]
                            for kk in range(4):
                                k = hf * 4 + kk
                                S.op("pe", lambda pt=pt, kk=kk, k=k, ti=ti, xnb=xnb: P.transpose(
                                    pt[:, kk * 128:(kk + 1) * 128], xnb[:, k, ti * 128:(ti + 1) * 128], ident[:]),
                                    R=[(xnb, None), ident], W=[pt])
                            if hf == 0:
                                S.act(ot[:, 0:512], pt[:], AF.Identity, R=[pt], W=[(ot, 0)])
                            else:
                                S.op("dve", lambda ot=ot, pt=pt: V.tensor_copy(out=ot[:, 512:1024], in_=pt[:]), R=[pt], W=[(ot, 1)])
                        row0 = t0 - CTX + ti * 128
                        S.dma("sp", out_d[b, row0:row0 + 128, :], ot[:], ot, R=[ot])
                        oi += 1
                S.barrier()

    return nc


def _finish(S, nc, bufs):
    S.barrier()


def _dump(S, nc, dbg_d, buf, es):
    with ExitStack() as ph:
        st = S.sb(ph, "dbgst", [128, T], F32)
        for k in range(KC):
            S.op("dve", lambda k=k: nc.vector.tensor_copy(out=st[:], in_=buf[:, k, :]), R=[buf, st], W=[st])
            S.dma("sp", dbg_d[:, k, :], st[:], st, R=[st])
        S.barrier()


_CACHE = {}


def _prep_shared(inp):
    pp = _pack_params(inp)
    shared = {
        "pp": pp.pack(),
        "w_ada": np.ascontiguousarray(inp["w_ada"], np.float32),
        "w_in": np.ascontiguousarray(inp["w_in"], np.float32),
        "wbd": _wbd(np.asarray(inp["rg_w"], np.float32)),
        "bpat": _build_bias_patterns(np.asarray(inp["na_rpb"], np.float32)),
        "w_out": np.ascontiguousarray(inp["w_out"], np.float32),
        "w_router": np.ascontiguousarray(inp["w_router"], np.float32),
        "b_router": np.ascontiguousarray(inp["b_router"], np.float32).reshape(1, N_EXP),
        "w_gate": np.ascontiguousarray(inp["w_gate"], np.float32),
        "w_up": np.ascontiguousarray(inp["w_up"], np.float32),
        "w_down": np.ascontiguousarray(inp["w_down"], np.float32),
        "ident": np.eye(128, dtype=np.float32),
    }
    return pp, shared


def _core_inputs(inp, shared, core, nb):
    b0 = core * nb
    c = np.asarray(inp["c"], np.float32)[b0:b0 + nb]
    rows = np.concatenate([c, np.asarray(inp["c_ctx"], np.float32)[None]], axis=0)
    if nb == 1:
        rows = np.concatenate([rows[:1], rows[:1], rows[1:]], axis=0)
    condT = np.ascontiguousarray(rows.reshape(3, KC, 128).transpose(2, 1, 0))
    m = dict(shared)
    m["x"] = np.ascontiguousarray(np.asarray(inp["x"], np.float32)[b0:b0 + nb])
    m["ctx"] = np.ascontiguousarray(np.asarray(inp["ctx"], np.float32)[b0:b0 + nb])
    m["condT"] = condT
    return m


def kernel(**inputs):
    inp = {k: np.asarray(v) for k, v in inputs.items()}
    nb = inp["x"].shape[0] // N_CORES
    pp, shared = _prep_shared(inp)
    nc = build_program(pp.off, pp.n, nb=nb)
    in_maps = [_core_inputs(inp, shared, core, nb) for core in range(N_CORES)]
    res = run_bass_kernel_spmd(nc, in_maps, core_ids=list(range(N_CORES)))
    out = np.concatenate([np.asarray(r["out"]) for r in res.results], axis=0)
    return out.astype(np.float32)
```

```python
import numpy as np
from contextlib import ExitStack
import concourse.bass as bass
import concourse.mybir as mybir
from concourse.bass_utils import run_bass_kernel_spmd

F32 = mybir.dt.float32
BF16 = mybir.dt.bfloat16
AF = mybir.ActivationFunctionType
ALU = mybir.AluOpType

N_CORES = 8
D = 1024
KC = 8
SEQ = 2048
CTX = 256
T = CTX + SEQ
DEPTH = 2
IN_W = 2816
COL_Q, COL_K, COL_V, COL_RX, COL_RG, COL_SB, COL_SC, COL_SX = 0, 512, 1024, 1536, 1792, 2048, 2304, 2560
N_EXP = 16
D_EXP = 512
EPS = 1e-6
MASK = -30000.0
GRID_W = 64
ROWS = 32
WIN_R = 8
WIN_C = 16

GROUPS_ALL = [(0, 256), (256, 768), (768, 1280), (1280, 1792), (1792, 2304)]
GROUPS_LAT = GROUPS_ALL[1:]


def _r0(r):
    return min(max(r - WIN_R // 2, 0), ROWS - WIN_R)


def _na_structure():
    pats = {}
    plist = []
    per_tile = []
    for i in range(16):
        rows = set()
        for r in (2 * i, 2 * i + 1):
            rows.update(range(_r0(r), _r0(r) + WIN_R))
        tiles = sorted({kr // 2 for kr in rows})
        ent = []
        for j in tiles:
            key = []
            for a in (0, 1):
                for cq in (0, 1):
                    kr, r = 2 * j + a, 2 * i + cq
                    key.append(kr - r if _r0(r) <= kr < _r0(r) + WIN_R else None)
            key = tuple(key)
            if key not in pats:
                pats[key] = len(plist)
                plist.append(key)
            ent.append((j, pats[key]))
        per_tile.append(ent)
    return per_tile, plist


NA_TILES, NA_PATS = _na_structure()
NPAT = len(NA_PATS)


def _na_slots():
    import itertools
    seqs = []
    for ent in NA_TILES:
        sq = tuple(p for (_, p) in ent)
        if sq not in seqs:
            seqs.append(sq)

    def find(hay, needle):
        for i in range(len(hay) - len(needle) + 1):
            if tuple(hay[i:i + len(needle)]) == tuple(needle):
                return i
        return -1

    best = None
    for perm in itertools.permutations(seqs):
        cur = []
        for sq in perm:
            if find(cur, sq) >= 0:
                continue
            ov = 0
            for k in range(min(len(cur), len(sq)), 0, -1):
                if tuple(cur[-k:]) == tuple(sq[:k]):
                    ov = k
                    break
            cur = cur + list(sq[ov:])
        if best is None or len(cur) < len(best):
            best = cur
    starts = [find(best, tuple(p for (_, p) in ent)) for ent in NA_TILES]
    return best, starts


NA_SLOTS, NA_START = _na_slots()
NSLOT = len(NA_SLOTS)


def _build_bias_patterns(rpb):
    L, H = rpb.shape[0], rpb.shape[1]
    kc = np.arange(GRID_W)[:, None]
    qc = np.arange(GRID_W)[None, :]
    c_start = np.clip(qc - WIN_C // 2, 0, GRID_W - WIN_C)
    ok = (kc >= c_start) & (kc < c_start + WIN_C)
    rel_c = np.clip(kc - qc + WIN_C - 1, 0, 2 * WIN_C - 2)
    out = np.full((L, H, 128, NSLOT, 128), MASK, np.float32)
    for p, pid in enumerate(NA_SLOTS):
        key = NA_PATS[pid]
        idx = 0
        for a in (0, 1):
            for cq in (0, 1):
                dr = key[idx]
                idx += 1
                if dr is None:
                    continue
                blk = np.where(ok[None, None], rpb[:, :, dr + WIN_R - 1][:, :, rel_c], np.float32(MASK))
                out[:, :, 64 * a:64 * a + 64, p, 64 * cq:64 * cq + 64] = blk
    return out


class PP:
    def __init__(self):
        self.off = {}
        self.n = 0
        self.parts = []

    def add(self, name, arr):
        arr = np.ascontiguousarray(arr, dtype=np.float32).reshape(128, -1)
        self.off[name] = (self.n, arr.shape[1])
        self.n += arr.shape[1]
        self.parts.append(arr)

    def pack(self):
        return np.ascontiguousarray(np.concatenate(self.parts, axis=1))


def _chunked(v):
    v = np.asarray(v, np.float32)
    lead = v.shape[:-1]
    n = v.shape[-1] // 128
    v = v.reshape(*lead, n, 128)
    return np.moveaxis(v, -1, 0)


def _pack_params(inp):
    pp = PP()
    pp.add("b_ada", _chunked(inp["b_ada"]))
    pp.add("g_mix", _chunked(inp["norm_mix_g"]))
    pp.add("g_out", _chunked(inp["mix_out_g"]))
    pp.add("g_ffn", _chunked(inp["norm_ffn_g"]))
    pp.add("g_fin", _chunked(inp["final_g"]))
    pp.add("lconv_w", _chunked(inp["lru_conv_w"]))
    pp.add("lconv_b", _chunked(inp["lru_conv_b"]))
    pp.add("rg_b", _chunked(inp["rg_b"]))
    pp.add("rg_lam", _chunked(inp["rg_lam"]))
    pp.add("sconv_w", _chunked(inp["sc_conv_w"]))
    pp.add("sconv_b", _chunked(inp["sc_conv_b"]))
    return pp


def _wbd(rg_w):
    L = rg_w.shape[0]
    out = np.zeros((L, 2, 2, 2, 128, 128), np.float32)
    for c in range(2):
        for a in range(2):
            out[:, :, :, c, 64 * a:64 * a + 64, 64 * a:64 * a + 64] = rg_w[:, :, :, 2 * c + a]
    return out


class Buf:
    def __init__(self, name, t):
        self.name = name
        self.t = t
        self.all_w = None
        self.all_r = {}
        self.reg = {}
        self.sem = None
        self.semv = 0

    def __getitem__(self, idx):
        return self.t[idx]


class Sched:
    def __init__(self, nc, es):
        self.nc = nc
        self.es = es
        self.eng = {"pe": nc.tensor, "act": nc.scalar, "dve": nc.vector, "pool": nc.gpsimd, "sp": nc.sync}
        self.sem = {e: es.enter_context(nc.semaphore("s_" + e)) for e in self.eng}
        self.cnt = {e: 0 for e in self.eng}
        self.waited = {e: {} for e in self.eng}
        self.dsem = {}
        self.nbuf = 0

    def sb(self, es, name, shape, dt=F32):
        self.nbuf += 1
        return Buf(name, es.enter_context(self.nc.sbuf_tensor(f"{name}_{self.nbuf}", list(shape), dt)))

    def ps(self, es, name, shape, dt=F32):
        self.nbuf += 1
        return Buf(name, es.enter_context(self.nc.psum_tensor(f"{name}_{self.nbuf}", list(shape), dt)))

    def wrap(self, name, t):
        return Buf(name, t)

    @staticmethod
    def _norm(items):
        out = []
        for it in items:
            if isinstance(it, Buf):
                out.append((it, None))
            else:
                out.append(it)
        return out

    def _deps(self, reads, writes):
        raw, deps = [], []
        for b, k in reads:
            if b.all_w:
                raw.append(b.all_w)
            if k is None:
                for st in b.reg.values():
                    if st[0]:
                        raw.append(st[0])
            elif k in b.reg and b.reg[k][0]:
                raw.append(b.reg[k][0])
        for b, k in writes:
            if b.all_w:
                deps.append(b.all_w)
            deps.extend(b.all_r.values())
            if k is None:
                for st in b.reg.values():
                    if st[0]:
                        deps.append(st[0])
                    deps.extend(st[1].values())
            elif k in b.reg:
                st = b.reg[k]
                if st[0]:
                    deps.append(st[0])
                deps.extend(st[1].values())
        return raw, deps

    def _record(self, reads, writes, dep):
        key = dep[0]
        for b, k in reads:
            if k is None:
                b.all_r[key] = dep
            else:
                b.reg.setdefault(k, [None, {}])[1][key] = dep
        for b, k in writes:
            if k is None:
                b.all_w = dep
                b.all_r = {}
                b.reg = {}
            else:
                b.reg[k] = [dep, {}]

    def _wait(self, e, deps):
        raw, other = deps
        for lst, same_ok in ((raw, e in ("act", "dve", "pool")), (other, False)):
            for key, sem, val in lst:
                if key == e and not same_ok:
                    continue
                if self.waited[e].get(key, 0) < val:
                    self.eng[e].wait_ge(sem, val)
                    self.waited[e][key] = val

    def op(self, e, emit, R=(), W=(), inc=True):
        R = self._norm(R)
        W = self._norm(W)
        self._wait(e, self._deps(R, W))
        ins = emit()
        if inc:
            ins.then_inc(self.sem[e], 1)
            self.cnt[e] += 1
            tick = self.cnt[e]
        else:
            tick = self.cnt[e] + 1
        self._record(R, W, (e, self.sem[e], tick))

    def dma(self, q, out_ap, in_ap, semb, R=(), W=()):
        R = self._norm(R)
        W = self._norm(W)
        self._wait(q, self._deps(R, W))
        if semb.name not in self.dsem:
            self.dsem[semb.name] = [self.es.enter_context(self.nc.semaphore("d_" + semb.name)), 0]
        ds = self.dsem[semb.name]
        self.eng[q].dma_start(out=out_ap, in_=in_ap).then_inc(ds[0], 16)
        ds[1] += 16
        self._record(R, W, ("dma_" + semb.name, ds[0], ds[1]))

    def barrier(self):
        for e in self.eng:
            deps = [(f, self.sem[f], self.cnt[f]) for f in self.eng if f != e and self.cnt[f] > 0]
            deps += [("dma_" + n, ds[0], ds[1]) for n, ds in self.dsem.items() if ds[1] > 0]
            self._wait(e, ([], deps))

    def act(self, out, in_, func, R, W, **kw):
        self.op("act", lambda: self.nc.scalar.activation(out=out, in_=in_, func=func, **kw), R, W)

    def mm(self, out, lhsT, rhs, start, stop, R, W, inc=None):
        self.op("pe", lambda: self.nc.tensor.matmul(out, lhsT, rhs, start=start, stop=stop), R, W,
                inc=True)


def build_program(pp_off, npp, nb=2, depth=DEPTH, debug=None):
    nc = bass.Bass("TRN2", target_bir_lowering=False)

    def din(name, shape):
        return nc.dram_tensor(name, list(shape), F32, kind="ExternalInput").ap()

    x_d = din("x", [nb, SEQ, D])
    ctx_d = din("ctx", [nb, CTX, D])
    condT_d = din("condT", [128, KC, 3])
    pp_d = din("pp", [128, npp])
    w_ada_d = din("w_ada", [DEPTH, D, 6 * D])
    w_in_d = din("w_in", [DEPTH, D, IN_W])
    wbd_d = din("wbd", [DEPTH, 2, 2, 2, 128, 128])
    bpat_d = din("bpat", [DEPTH, 8, 128, NSLOT, 128])
    w_out_d = din("w_out", [DEPTH, D, D])
    w_router_d = din("w_router", [D, N_EXP])
    b_router_d = din("b_router", [1, N_EXP])
    w_gate_d = din("w_gate", [DEPTH, N_EXP, D, D_EXP])
    w_up_d = din("w_up", [DEPTH, N_EXP, D, D_EXP])
    w_down_d = din("w_down", [DEPTH, N_EXP, D_EXP, D])
    ident_d = din("ident", [128, 128])
    out_d = nc.dram_tensor("out", [nb, SEQ, D], F32, kind="ExternalOutput").ap()
    dbg_d = None
    if debug is not None:
        dbg_d = nc.dram_tensor("dbg", [128, KC, T], F32, kind="ExternalOutput").ap()

    V, A, P, G = nc.vector, nc.scalar, nc.tensor, nc.gpsimd

    with ExitStack() as es:
        S = Sched(nc, es)

        xT = S.sb(es, "xT", [128, KC, T], F32)
        hT = S.sb(es, "hT", [128, KC, T], BF16)
        ppt = S.sb(es, "ppt", [128, npp], F32)
        mod = S.sb(es, "mod", [128, DEPTH, 48, 3], F32)
        modv = S.sb(es, "modv", [128, DEPTH, 3, 6, KC], F32)
        ident = S.sb(es, "ident", [128, 128], F32)
        identb = S.sb(es, "identb", [128, 128], BF16)
        onesb = S.sb(es, "onesb", [128, 128], BF16)
        onesf = S.sb(es, "onesf", [128, 128], F32)
        condT = S.sb(es, "condT", [128, KC, 3], F32)
        wr_sb = S.sb(es, "wr_sb", [128, KC, N_EXP], F32)
        br_sb = S.sb(es, "br_sb", [1, N_EXP], F32)
        cl = S.sb(es, "cl", [128, DEPTH, 2, 2], F32)
        outsem = S.wrap("outsem", None)
        wbuf = S.sb(es, "wbuf", [128, KC, 384], BF16)
        wspecs = []
        for _b in range(nb):
            for _l in range(depth):
                for _c in range(2):
                    wspecs.append((_l, [COL_SB + _c * 128, COL_SC + _c * 128, COL_SX + _c * 128]))
                for _c in range(2):
                    wspecs.append((_l, [COL_RX + _c * 128, COL_RG + _c * 128]))
                for _c in range(4):
                    wspecs.append((_l, [COL_Q + _c * 128, COL_K + _c * 128, COL_V + _c * 128]))
        wnext = [0]

        def issue_w():
            if wnext[0] >= len(wspecs):
                return
            l_, cols = wspecs[wnext[0]]
            wnext[0] += 1
            for j, col in enumerate(cols):
                S.dma("pool", wbuf[:, :, j * 128:(j + 1) * 128],
                      w_in_d[l_].rearrange("(k p) n -> p k n", p=128)[:, :, col:col + 128], wbuf, W=[wbuf])

        def ppv(name, *idx):
            return ppv_shape[name](*idx)

        ppv_shape = {}

        def reg_pp(name, dims):
            o, w = pp_off[name]
            strides = []
            s = 1
            for d in reversed(dims):
                strides.insert(0, s)
                s *= d
            assert s == w, (name, dims, w)

            def f(*idx):
                col = o + sum(i * st for i, st in zip(idx, strides))
                return ppt[:, col:col + 1]
            ppv_shape[name] = f

        reg_pp("b_ada", [DEPTH, 48])
        reg_pp("g_mix", [DEPTH, KC])
        reg_pp("g_out", [DEPTH, KC])
        reg_pp("g_ffn", [DEPTH, KC])
        reg_pp("g_fin", [KC])
        reg_pp("lconv_w", [DEPTH, 4, 2])
        reg_pp("lconv_b", [DEPTH, 2])
        reg_pp("rg_b", [DEPTH, 2, 2, 2])
        reg_pp("rg_lam", [DEPTH, 2, 2])
        reg_pp("sconv_w", [DEPTH, 3, 2])
        reg_pp("sconv_b", [DEPTH, 2])

        S.dma("sp", ppt[:], pp_d[:, :], ppt, W=[ppt])
        S.dma("sp", ident[:], ident_d[:, :], ident, W=[ident])
        S.dma("sp", condT[:], condT_d[:, :, :], condT, W=[condT])
        S.dma("sp", wr_sb[:], w_router_d.rearrange("(k p) e -> p k e", p=128), wr_sb, W=[wr_sb])
        S.dma("sp", br_sb[:], b_router_d[:, :], br_sb, W=[br_sb])
        S.op("dve", lambda: V.tensor_copy(out=identb[:], in_=ident[:]), R=[ident], W=[identb])
        S.op("dve", lambda: V.memset(onesb[:], 1.0), W=[onesb])
        S.op("dve", lambda: V.memset(onesf[:], 1.0), W=[onesf])
        S.act(condT[:], condT[:], AF.Silu, R=[condT], W=[condT])
        o_lam, w_lam = pp_off["rg_lam"]
        clv = cl[:].rearrange("p a b c -> p (a b c)")
        S.act(clv, ppt[:, o_lam:o_lam + w_lam], AF.Exp, R=[ppt], W=[cl], scale=-1.0)
        S.act(clv, clv, AF.Ln, R=[cl], W=[cl], bias=1.0)
        S.op("dve", lambda: V.tensor_scalar(out=clv, in0=clv, scalar1=-8.0, scalar2=None, op0=ALU.mult), R=[cl], W=[cl])

        o_rgb, w_rgb = pp_off["rg_b"]
        nrgb = S.sb(es, "nrgb", [128, w_rgb], F32)
        S.op("dve", lambda: V.tensor_scalar(out=nrgb[:], in0=ppt[:, o_rgb:o_rgb + w_rgb], scalar1=-1.0, scalar2=None,
                                            op0=ALU.mult), R=[ppt], W=[nrgb])

        def nrgbv(l_, d_, g_, c_):
            col = ((l_ * 2 + d_) * 2 + g_) * 2 + c_
            return nrgb[:, col:col + 1]

        if debug == ("const", 0, 0):
            S.barrier()
            S.dma("sp", dbg_d[:, 0, 0:128], ident[:], ident, R=[ident])
            S.dma("sp", dbg_d[:, 1, 0:8], cl[:].rearrange("p a b c -> p (a b c)"), cl, R=[cl])
            S.dma("sp", dbg_d[:, 2, 0:24], condT[:].rearrange("p a b -> p (a b)"), condT, R=[condT])
            S.barrier()
            return nc
        with ExitStack() as ph:
            wa = [S.sb(ph, f"wa{i}", [128, KC, 768], F32) for i in range(2)]
            pmod = S.ps(ph, "pmod", [128, 512], F32)
            it = 0
            for l in range(depth):
                for cg in range(8):
                    wb = wa[it % 2]
                    it += 1
                    S.dma("sp" if it % 2 else "act", wb[:],
                          w_ada_d[l].rearrange("(k p) n -> p k n", p=128)[:, :, cg * 768:(cg + 1) * 768], wb, W=[wb])
                    for jj in range(6):
                        j = cg * 6 + jj
                        for k in range(KC):
                            S.mm(pmod[:, jj * 3:jj * 3 + 3], wb[:, k, jj * 128:(jj + 1) * 128], condT[:, k, :],
                                 start=(k == 0), stop=(k == KC - 1), R=[wb, condT], W=[pmod])
                    for jj in range(6):
                        j = cg * 6 + jj
                        S.op("dve", lambda j=j, jj=jj: V.tensor_scalar(
                            out=mod[:, l, j, :], in0=pmod[:, jj * 3:jj * 3 + 3], scalar1=ppv("b_ada", l, j),
                            scalar2=None, op0=ALU.add), R=[pmod, ppt], W=[mod])
            for l in range(depth):
                for r in range(3):
                    for (dst, mi, gname) in ((0, 1, "g_mix"), (3, 4, "g_ffn")):
                        og, _ = pp_off[gname]
                        S.op("dve", lambda l=l, r=r, dst=dst, mi=mi, og=og: V.scalar_tensor_tensor(
                            out=modv[:, l, r, dst, :], in0=mod[:, l, mi * 8:(mi + 1) * 8, r], scalar=1.0,
                            in1=ppt[:, og + l * KC:og + (l + 1) * KC], op0=ALU.add, op1=ALU.mult),
                            R=[mod, ppt], W=[modv])
                    for (dst, mi) in ((1, 0), (2, 2), (4, 3), (5, 5)):
                        S.op("dve", lambda l=l, r=r, dst=dst, mi=mi: V.tensor_copy(
                            out=modv[:, l, r, dst, :], in_=mod[:, l, mi * 8:(mi + 1) * 8, r]), R=[mod], W=[modv])
            S.barrier()

        def mv(l, r, which, k):
            return modv[:, l, r, which, k:k + 1]

        if debug == ("mod", 0, 0):
            S.dma("sp", dbg_d[:, 0, 0:DEPTH * 48 * 3], mod[:].rearrange("p a b c -> p (a b c)"), mod, R=[mod])
            S.dma("sp", dbg_d[:, 1, 0:DEPTH * 3 * 6 * KC], modv[:].rearrange("p a b c d -> p (a b c d)"), modv, R=[modv])
            S.barrier()
            return nc

        def rms_rstd(ph, src, chunks, groups, scale, name, psum, sq, rstd_buf, rkey=lambda gi: gi, gis=None):
            for gi_, (t0, t1) in enumerate(groups):
                gi = gi_ if gis is None else gis[gi_]
                n = t1 - t0
                for ci, k in enumerate(chunks):
                    S.act(sq[:, ci % 2, 0:n], src[:, k, t0:t1], AF.Square, R=[(src, rkey(gi))], W=[(sq, ci % 2)])
                    S.mm(psum[:, 0:n], onesb[:], sq[:, ci % 2, 0:n], start=(ci == 0), stop=(ci == len(chunks) - 1),
                         R=[(sq, ci % 2), onesb], W=[psum])
                S.act(rstd_buf[:, t0:t1], psum[:, 0:n], AF.Sqrt, R=[psum], W=[(rstd_buf, gi)], scale=scale, bias=EPS)
                S.op("dve", lambda t0=t0, t1=t1: V.reciprocal(out=rstd_buf[:, t0:t1], in_=rstd_buf[:, t0:t1]),
                     R=[(rstd_buf, gi)], W=[(rstd_buf, gi)])

        def gkey(groups, gi):
            return GROUPS_ALL.index(groups[gi])

        def dbgdump(slot, ap, n, Rbufs, parts=128):
            with ExitStack() as dph:
                n = min(n, 2048)
                st = S.sb(dph, "dbgst", [128, 2048], F32)
                S.op("dve", lambda: V.tensor_copy(out=st[0:parts, 0:n], in_=ap[:, 0:n]), R=Rbufs, W=[st])
                S.dma("sp", dbg_d[0:parts, slot, 0:n], st[0:parts, 0:n], st, R=[st])
                S.barrier()

        issue_w()
        for b in range(nb):
            with ExitStack() as ph:
                stg = [S.sb(ph, f"stg{i}", [128, D], F32) for i in range(2)]
                ptr = [S.ps(ph, f"ptr{i}", [128, 512], F32) for i in range(4)]
                for tt in range(T // 128):
                    st = stg[tt % 2]
                    src = ctx_d[b, tt * 128:(tt + 1) * 128, :] if tt < 2 else x_d[b, (tt - 2) * 128:(tt - 1) * 128, :]
                    S.dma("sp", st[:], src, st, W=[st])
                    for hf in range(2):
                        pt = ptr[(tt * 2 + hf) % 4]
                        for kk in range(4):
                            k = hf * 4 + kk
                            S.op("pe", lambda pt=pt, kk=kk, k=k, st=st: P.transpose(
                                pt[:, kk * 128:(kk + 1) * 128], st[:, k * 128:(k + 1) * 128], ident[:]),
                                R=[st, ident], W=[pt])
                        eng = "act" if hf == 0 else "dve"
                        dst = xT[:, hf * 4:hf * 4 + 4, tt * 128:(tt + 1) * 128]
                        srcp = pt[:].rearrange("p (k t) -> p k t", t=128)
                        if eng == "act":
                            S.act(dst, srcp, AF.Identity, R=[pt], W=[(xT, tt)])
                        else:
                            S.op("dve", lambda dst=dst, srcp=srcp: V.tensor_copy(out=dst, in_=srcp), R=[pt], W=[(xT, tt)])
                S.barrier()

            if debug == ("load", b, 0):
                _dump(S, nc, dbg_d, xT, es)
                return nc
            for l in range(depth):
                last = (l == DEPTH - 1)
                groups_out = GROUPS_LAT if last else GROUPS_ALL

                def norm_mod(ph, which_g, which_s, groups):
                    sq = S.sb(ph, "sq", [128, 2, 512], BF16)
                    rstd = S.sb(ph, "rstd", [128, T], F32)
                    tmp = S.sb(ph, "ntmp", [128, 2, 512], F32)
                    psn = S.ps(ph, "psn", [128, 512], F32)
                    def stats(gi):
                        rms_rstd(ph, xT, list(range(KC)), [groups[gi]], 1.0 / D, "n", psn, sq, rstd, gis=[gi])
                    stats(0)
                    for gi, (t0, t1) in enumerate(groups):
                        n = t1 - t0
                        r = 2 if t0 < CTX else b
                        if gi + 1 < len(groups):
                            stats(gi + 1)
                        for k in range(KC):
                            tb = k % 2
                            S.op("dve", lambda k=k, t0=t0, t1=t1, n=n, r=r, tb=tb: V.scalar_tensor_tensor(
                                out=tmp[:, tb, 0:n], in0=xT[:, k, t0:t1], scalar=mv(l, r, which_g, k),
                                in1=rstd[:, t0:t1], op0=ALU.mult, op1=ALU.mult),
                                R=[(xT, None), (rstd, gi), modv], W=[(tmp, tb)])
                            S.act(hT[:, k, t0:t1], tmp[:, tb, 0:n], AF.Identity, R=[(tmp, tb), modv],
                                  W=[(hT, gkey(groups, gi))], bias=mv(l, r, which_s, k))

                with ExitStack() as ph:
                    norm_mod(ph, 0, 1, GROUPS_ALL)
                    S.barrier()

                if debug == ("h1", b, l):
                    _dump(S, nc, dbg_d, hT, es)
                    return nc

                with ExitStack() as mx:
                    yT = S.sb(mx, "yT", [128, KC, T], BF16)

                    def proj_fm(ph, wsb, wcol0, m, dst_fn, groups, ps_list, base=0):
                        for gi, (t0, t1) in enumerate(groups):
                            n = t1 - t0
                            pt = ps_list[gi % len(ps_list)]
                            for k in range(KC):
                                S.mm(pt[0:m, 0:n], wsb[:, k, wcol0:wcol0 + m], hT[:, k, t0:t1], start=(k == 0),
                                     stop=(k == KC - 1), R=[wsb, (hT, gkey(groups, gi))], W=[pt])
                            dst_fn(gi, t0, t1, pt)

                    for c in range(2):
                        with ExitStack() as ph:
                            wsb = wbuf
                            sbv = S.sb(ph, "sbv", [128, T], F32)
                            pv = S.sb(ph, "pv", [128, T], F32)
                            acc = S.sb(ph, "acc", [128, T], F32)
                            pss = [S.ps(ph, f"pss{i}", [128, 512], F32) for i in range(4)]

                            def ev_sb(gi, t0, t1, pt):
                                S.act(sbv[:, t0:t1], pt[:, 0:t1 - t0], AF.Identity, R=[pt], W=[(sbv, gi)])

                            def ev_sc(gi, t0, t1, pt):
                                S.act(pv[:, t0:t1], pt[:, 0:t1 - t0], AF.Identity, R=[pt], W=[(pv, gi)])

                            def ev_sx(gi, t0, t1, pt):
                                S.op("dve", lambda: V.tensor_tensor(out=pv[:, t0:t1], in0=pt[:, 0:t1 - t0],
                                                                    in1=pv[:, t0:t1], op=ALU.mult),
                                     R=[pt, (pv, gi)], W=[(pv, gi)])

                            proj_fm(ph, wsb, 0, 128, ev_sb, GROUPS_ALL, pss)
                            proj_fm(ph, wsb, 128, 128, ev_sc, GROUPS_ALL, pss)
                            proj_fm(ph, wsb, 256, 128, ev_sx, GROUPS_ALL, pss)
                            issue_w()
                            for (s0, s1) in ((0, CTX), (CTX, T)):
                                S.op("dve", lambda s0=s0, s1=s1: V.tensor_scalar(
                                    out=acc[:, s0:s1], in0=pv[:, s0:s1], scalar1=ppv("sconv_w", l, 1, c),
                                    scalar2=ppv("sconv_b", l, c), op0=ALU.mult, op1=ALU.add), R=[pv, ppt], W=[acc])
                                S.op("dve", lambda s0=s0, s1=s1: V.scalar_tensor_tensor(
                                    out=acc[:, s0 + 1:s1], in0=pv[:, s0:s1 - 1], scalar=ppv("sconv_w", l, 0, c),
                                    in1=acc[:, s0 + 1:s1], op0=ALU.mult, op1=ALU.add), R=[pv, ppt, acc], W=[acc])
                                S.op("dve", lambda s0=s0, s1=s1: V.scalar_tensor_tensor(
                                    out=acc[:, s0:s1 - 1], in0=pv[:, s0 + 1:s1], scalar=ppv("sconv_w", l, 2, c),
                                    in1=acc[:, s0:s1 - 1], op0=ALU.mult, op1=ALU.add), R=[pv, ppt, acc], W=[acc])
                            S.op("dve", lambda: V.tensor_tensor(out=yT[:, 6 + c, :], in0=sbv[:], in1=acc[:], op=ALU.mult),
                                 R=[sbv, acc], W=[yT])
                            S.barrier()

                    for c in range(2):
                        with ExitStack() as ph:
                            wsb = wbuf
                            wg = S.sb(ph, "wbd", [128, 2, 2, 128], F32)
                            for d in range(2):
                                for g in range(2):
                                    S.dma("sp", wg[:, d, g, :], wbd_d[l, d, g, c, :, :], wg, W=[wg])
                            rxc = S.sb(ph, "rxc", [128, T], F32)
                            a_f = S.sb(ph, "a_f", [128, T], F32)
                            a_r = S.sb(ph, "a_r", [128, T], F32)
                            bb = S.sb(ph, "bb", [128, T], F32)
                            gt = S.sb(ph, "gt", [128, 4, 512], F32)
                            psl = [S.ps(ph, f"psl{i}", [128, 512], F32) for i in range(4)]
                            rx_raw = a_f

                            def ev_rx(gi, t0, t1, pt):
                                S.act(rx_raw[:, t0:t1], pt[:, 0:t1 - t0], AF.Identity, R=[pt], W=[(rx_raw, gi)])

                            def ev_rg(gi, t0, t1, pt):
                                S.act(yT[:, 4 + c, t0:t1], pt[:, 0:t1 - t0], AF.Gelu, R=[pt], W=[(yT, gi)])

                            proj_fm(ph, wsb, 0, 128, ev_rx, GROUPS_ALL, psl)
                            proj_fm(ph, wsb, 128, 128, ev_rg, GROUPS_ALL, psl)
                            issue_w()
                            for (s0, s1) in ((0, CTX), (CTX, T)):
                                S.op("dve", lambda s0=s0, s1=s1: V.tensor_scalar(
                                    out=rxc[:, s0:s1], in0=rx_raw[:, s0:s1], scalar1=ppv("lconv_w", l, 2, c),
                                    scalar2=ppv("lconv_b", l, c), op0=ALU.mult, op1=ALU.add), R=[rx_raw, ppt], W=[rxc])
                                for (kk, sh) in ((0, 2), (1, 1)):
                                    S.op("dve", lambda s0=s0, s1=s1, kk=kk, sh=sh: V.scalar_tensor_tensor(
                                        out=rxc[:, s0 + sh:s1], in0=rx_raw[:, s0:s1 - sh], scalar=ppv("lconv_w", l, kk, c),
                                        in1=rxc[:, s0 + sh:s1], op0=ALU.mult, op1=ALU.add), R=[rx_raw, ppt, rxc], W=[rxc])
                                S.op("dve", lambda s0=s0, s1=s1: V.scalar_tensor_tensor(
                                    out=rxc[:, s0:s1 - 1], in0=rx_raw[:, s0 + 1:s1], scalar=ppv("lconv_w", l, 3, c),
                                    in1=rxc[:, s0:s1 - 1], op0=ALU.mult, op1=ALU.add), R=[rx_raw, ppt, rxc], W=[rxc])
                            for d in range(2):
                                adst = a_f if d == 0 else a_r
                                for gi, (t0, t1) in enumerate(GROUPS_ALL):
                                    n = t1 - t0
                                    if d == 0:
                                        q0, q1 = t0, t1
                                    elif t0 < CTX:
                                        q0, q1 = 0, CTX
                                    else:
                                        q0 = CTX + (T - t1)
                                        q1 = q0 + n
                                    pr, pi = psl[0 + 2 * (gi % 2)], psl[1 + 2 * (gi % 2)]
                                    S.mm(pr[:, 0:n], wg[:, d, 0, :], rxc[:, t0:t1], True, True, R=[wg, rxc], W=[pr])
                                    S.mm(pi[:, 0:n], wg[:, d, 1, :], rxc[:, t0:t1], True, True, R=[wg, rxc], W=[pi])
                                    ii = gt[:, gi % 2, 0:n]

                                    def rv(ap):
                                        return ap[:, ::-1] if d == 1 else ap
                                    S.act(adst[:, q0:q1], rv(pr[:, 0:n]), AF.Sigmoid, R=[pr, ppt], W=[(adst, gi)],
                                          bias=ppv("rg_b", l, d, 0, c))
                                    S.act(ii, pi[:, 0:n], AF.Sigmoid, R=[pi, ppt], W=[(gt, gi % 2)], bias=ppv("rg_b", l, d, 1, c))
                                    S.op("dve", lambda ii=ii, t0=t0, t1=t1, q0=q0, q1=q1, rv=rv: V.tensor_tensor(
                                        out=bb[:, q0:q1], in0=rv(ii), in1=rv(rxc[:, t0:t1]), op=ALU.mult),
                                        R=[(gt, gi % 2), rxc], W=[(bb, gi)])
                                S.act(adst[:, :], adst[:, :], AF.Exp, R=[adst, cl], W=[adst], scale=cl[:, l, d, c:c + 1])
                                for gi, (t0, t1) in enumerate(GROUPS_ALL):
                                    n = t1 - t0
                                    mm_ = gt[:, 2 + gi % 2, 0:n]
                                    S.op("pool", lambda mm_=mm_, t0=t0, t1=t1, adst=adst: G.tensor_tensor(
                                        out=mm_, in0=adst[:, t0:t1], in1=adst[:, t0:t1], op=ALU.mult),
                                        R=[adst], W=[(gt, 2 + gi % 2)])
                                    S.act(mm_, mm_, AF.Sqrt, R=[(gt, 2 + gi % 2)], W=[(gt, 2 + gi % 2)], scale=-1.0, bias=1.0)
                                    S.op("dve", lambda mm_=mm_, t0=t0, t1=t1: V.tensor_tensor(
                                        out=bb[:, t0:t1], in0=bb[:, t0:t1], in1=mm_, op=ALU.mult),
                                        R=[(gt, 2 + gi % 2), bb], W=[bb])
                                if debug == ("lru", b, l) and c == 0 and d == 0:
                                    dbgdump(0, rxc[:, :], T, [rxc])
                                    dbgdump(1, a_f[:, :], T, [a_f])
                                    dbgdump(2, bb[:, :], T, [bb])
                                    dbgdump(3, gt[:, :, :].rearrange("p a b -> p (a b)"), 2048, [gt])
                                    dbgdump(4, cl[:].rearrange("p a b c -> p (a b c)"), 8, [cl])
                                S.op("dve", lambda adst=adst: V.tensor_tensor_scan(
                                    out=adst[:, :], data0=adst[:, :], data1=bb[:, :], initial=0.0,
                                    op0=ALU.mult, op1=ALU.add), R=[adst, bb], W=[adst])
                            if debug == ("lru", b, l) and c == 0:
                                dbgdump(5, a_f[:, :], T, [a_f])
                                dbgdump(6, a_r[:, :], T, [a_r])
                                dbgdump(7, yT[:, 4 + c, :], T, [yT])
                                return nc
                            for (s0, s1) in ((0, CTX), (CTX, T)):
                                S.op("dve", lambda s0=s0, s1=s1: V.tensor_tensor(
                                    out=bb[:, s0:s1], in0=a_f[:, s0:s1], in1=a_r[:, s0:s1][:, ::-1], op=ALU.add),
                                    R=[a_f, a_r], W=[bb])
                            S.op("dve", lambda: V.tensor_tensor(out=yT[:, 4 + c, :], in0=bb[:], in1=yT[:, 4 + c, :],
                                                                op=ALU.mult), R=[bb, yT], W=[yT])
                            S.barrier()

                    qtiles = list(range(0 if not last else 2, T // 128))
                    with ExitStack() as nas:
                        bpb = [S.sb(nas, f"bp{i}", [128, 2, NSLOT, 128], BF16) for i in range(2)]

                        def load_bp(c_):
                            for hh_ in range(2):
                                S.dma("pool", bpb[c_ % 2][:, hh_, :, :], bpat_d[l, 2 * c_ + hh_, :, :, :], bpb[c_ % 2],
                                      W=[bpb[c_ % 2]])

                        load_bp(0)
                        for c in range(4):
                            with ExitStack() as ph:
                                wsb = wbuf
                                bp = bpb[c % 2]
                                QT = S.sb(ph, "QT", [128, T], BF16)
                                KT = S.sb(ph, "KT", [128, T], BF16)
                                Vp = S.sb(ph, "Vp", [128, T // 128, 2, 65], BF16)
                                PT = [S.sb(ph, f"PT{i}", [128, 7 * 128], BF16) for i in range(2)]
                                otok = [S.sb(ph, f"otok{i}", [128, 2, 64], BF16) for i in range(2)]
                                rec = [S.sb(ph, f"rec{i}", [128, 2, 1], F32) for i in range(2)]
                                psq = [S.ps(ph, f"psq{i}", [128, 512], F32) for i in range(2)]
                                pst = [S.ps(ph, f"pst{i}", [128, 1024], F32) for i in range(2)]
                                pso = S.ps(ph, "pso", [128, 2, 2, 65], F32)
                                pstr = S.ps(ph, "pstr", [128, 128], BF16)
                                S.op("dve", lambda: V.memset(Vp[:, :, :, 64:65], 1.0), W=[Vp])

                                def ev_q(gi, t0, t1, pt):
                                    S.act(QT[:, t0:t1], pt[:, 0:t1 - t0], AF.Identity, R=[pt], W=[(QT, gi)], scale=0.125)

                                def ev_k(gi, t0, t1, pt):
                                    S.op("dve", lambda: V.tensor_copy(out=KT[:, t0:t1], in_=pt[:, 0:t1 - t0]),
                                         R=[pt], W=[(KT, gi)])
                                proj_fm(ph, wsb, 0, 128, ev_q, GROUPS_LAT if last else GROUPS_ALL, psq)
                                proj_fm(ph, wsb, 128, 128, ev_k, GROUPS_ALL, psq)
                                for tt in range(T // 128):
                                    pt = psq[tt % 2]
                                    for k in range(KC):
                                        S.mm(pt[:, 0:128], hT[:, k, tt * 128:(tt + 1) * 128], wsb[:, k, 256:384], start=(k == 0),
                                             stop=(k == KC - 1), R=[wsb, (hT, None)], W=[pt])
                                    S.act(Vp[:, tt, :, 0:64], pt[:, 0:128].rearrange("p (h d) -> p h d", d=64), AF.Identity,
                                          R=[pt], W=[(Vp, tt)])
                                issue_w()
                                if c + 1 < 4:
                                    load_bp(c + 1)
                                bpf = bp[:].rearrange("p a b c -> p (a b c)")
                                S.act(bpf, bpf, AF.Exp, R=[bp], W=[bp])
                                pairs = [(qi, qt, hh) for qi, qt in enumerate(qtiles) for hh in range(2)]

                                def blk(qt):
                                    if qt < 2:
                                        return [0, 1], 0, 0
                                    return ([0, 1] + [2 + j for (j, p) in NA_TILES[qt - 2]], len(NA_TILES[qt - 2]),
                                            NA_START[qt - 2])

                                def scores(i):
                                    qi, qt, hh = pairs[i]
                                    blocks, nl, s0 = blk(qt)
                                    nbk = len(blocks)
                                    ps_s = pst[i % 2]
                                    ptb = PT[i % 2]
                                    for bi, kt in enumerate(blocks):
                                        S.mm(ps_s[:, bi * 128:(bi + 1) * 128], KT[hh * 64:(hh + 1) * 64, kt * 128:(kt + 1) * 128],
                                             QT[hh * 64:(hh + 1) * 64, qt * 128:(qt + 1) * 128], start=True, stop=True,
                                             R=[(KT, None), (QT, None)], W=[ps_s])
                                    for (c0, c1) in ((0, min(nbk, 4)), (4, nbk)):
                                        if c1 > c0:
                                            S.act(ptb[:, c0 * 128:c1 * 128], ps_s[:, c0 * 128:c1 * 128], AF.Exp, R=[ps_s], W=[ptb])
                                    if nl:
                                        S.op("dve", lambda ptb=ptb, hh=hh, nl=nl, s0=s0: V.tensor_tensor(
                                            out=ptb[:, 256:256 + nl * 128].rearrange("p (a b) -> p a b", b=128),
                                            in0=ptb[:, 256:256 + nl * 128].rearrange("p (a b) -> p a b", b=128),
                                            in1=bp[:, hh, s0:s0 + nl, :], op=ALU.mult), R=[ptb, bp], W=[ptb])

                                def pv(i):
                                    qi, qt, hh = pairs[i]
                                    blocks, nl, s0 = blk(qt)
                                    nbk = len(blocks)
                                    ptb = PT[i % 2]
                                    po = pso[:, qi % 2]
                                    for bi, kt in enumerate(blocks):
                                        S.mm(po[:, hh, :], ptb[:, bi * 128:(bi + 1) * 128], Vp[:, kt, hh, :], start=(bi == 0),
                                             stop=(bi == nbk - 1), R=[ptb, (Vp, None)], W=[(pso, qi % 2)])

                                def normalise(qi, qt):
                                    po = pso[:, qi % 2]
                                    pok = (pso, qi % 2)
                                    rc, ot = rec[qi % 2], otok[qi % 2]
                                    S.op("dve", lambda rc=rc, po=po: V.reciprocal(out=rc[:], in_=po[:, :, 64:65]), R=[pok], W=[rc])
                                    S.op("dve", lambda rc=rc, po=po, ot=ot: V.tensor_tensor(
                                        out=ot[:], in0=po[:, :, 0:64], in1=rc[:].to_broadcast([128, 2, 64]), op=ALU.mult),
                                        R=[pok, rc], W=[ot])

                                def to_fm(qi, qt):
                                    ot = otok[qi % 2]
                                    S.op("pe", lambda ot=ot: P.transpose(pstr[:], ot[:].rearrange("p h d -> p (h d)"), identb[:]),
                                         R=[ot, identb], W=[pstr])
                                    S.act(yT[:, c, qt * 128:(qt + 1) * 128], pstr[:], AF.Identity, R=[pstr], W=[(yT, ("na", qt))])

                                scores(0)
                                deferred = None
                                for i in range(len(pairs)):
                                    if i + 1 < len(pairs):
                                        scores(i + 1)
                                    if deferred is not None:
                                        to_fm(*deferred)
                                        deferred = None
                                    pv(i)
                                    qi, qt, hh = pairs[i]
                                    if hh == 1:
                                        normalise(qi, qt)
                                        deferred = (qi, qt)
                                if deferred is not None:
                                    to_fm(*deferred)
                                S.barrier()

                    if debug == ("y", b, l):
                        _dump(S, nc, dbg_d, yT, es)
                        return nc

                    with ExitStack() as ph:
                        wo = S.sb(ph, "wo", [128, KC, D], BF16)
                        for hf in range(2):
                            S.dma("pool", wo[:, :, hf * 512:(hf + 1) * 512],
                                  w_out_d[l].rearrange("(k p) n -> p k n", p=128)[:, :, hf * 512:(hf + 1) * 512], wo, W=[wo])
                        sq = S.sb(ph, "sq", [128, 2, 512], BF16)
                        rstds = [S.sb(ph, f"rstd{i}", [128, T], F32) for i in range(3)]
                        psn = S.ps(ph, "psn", [128, 512], F32)
                        pso2 = [S.ps(ph, f"pso2{i}", [128, 512], F32) for i in range(4)]
                        grp_chunks = ([0, 1, 2, 3], [4, 5], [6, 7])
                        def ostats(gi):
                            for gidx, chunks in enumerate(grp_chunks):
                                rms_rstd(ph, yT, chunks, [groups_out[gi]], 1.0 / (128 * len(chunks)), "g", psn, sq, rstds[gidx],
                                         rkey=lambda gi_: ("o", gi_), gis=[gi])
                        ostats(0)
                        for gi, (t0, t1) in enumerate(groups_out):
                            n = t1 - t0
                            r = 2 if t0 < CTX else b
                            if gi + 1 < len(groups_out):
                                ostats(gi + 1)
                            for gidx, chunks in enumerate(grp_chunks):
                                for k in chunks:
                                    S.op("dve", lambda k=k, t0=t0, t1=t1, gidx=gidx: V.scalar_tensor_tensor(
                                        out=yT[:, k, t0:t1], in0=yT[:, k, t0:t1], scalar=ppv("g_out", l, k),
                                        in1=rstds[gidx][:, t0:t1], op0=ALU.mult, op1=ALU.mult),
                                        R=[(yT, ("o", gi)), (rstds[gidx], gi), ppt], W=[(yT, ("o", gi))])
                            for oc in range(KC):
                                pt = pso2[oc % 4]
                                for k in range(KC):
                                    S.mm(pt[:, 0:n], wo[:, k, oc * 128:(oc + 1) * 128], yT[:, k, t0:t1], start=(k == 0),
                                         stop=(k == KC - 1), R=[wo, (yT, ("o", gi))], W=[pt])
                                S.op("dve", lambda oc=oc, t0=t0, t1=t1, n=n, r=r, pt=pt: V.scalar_tensor_tensor(
                                    out=xT[:, oc, t0:t1], in0=pt[:, 0:n], scalar=mv(l, r, 2, oc), in1=xT[:, oc, t0:t1],
                                    op0=ALU.mult, op1=ALU.add), R=[pt, (xT, None), modv], W=[(xT, None)])
                        S.barrier()

                if debug == ("xmix", b, l):
                    _dump(S, nc, dbg_d, xT, es)
                    return nc

                with ExitStack() as ph:
                    ng = len(groups_out)
                    tok0 = groups_out[0][0]
                    comb = S.sb(ph, "comb", [N_EXP, T], F32)
                    with ExitStack() as ph2:
                        sq = S.sb(ph2, "sq", [128, 2, 512], BF16)
                        rstd = S.sb(ph2, "rstd", [128, T], F32)
                        tmp = S.sb(ph2, "ntmp", [128, 2, 512], F32)
                        hfb = S.sb(ph2, "hfb", [128, KC, 512], F32)
                        psn = S.ps(ph2, "psn", [128, 512], F32)
                        psr = [S.ps(ph2, f"psr{i}", [128, 512], F32) for i in range(2)]
                        pscT = S.ps(ph2, "pscT", [N_EXP, 512], F32)
                        rts = [S.sb(ph2, f"rt{i}", [128, 16, 16], F32) for i in range(2)]
                        rms_rstd(ph2, xT, list(range(KC)), groups_out, 1.0 / D, "n2", psn, sq, rstd)
                        tile_i = 0
                        for gi, (t0, t1) in enumerate(groups_out):
                            n = t1 - t0
                            r = 2 if t0 < CTX else b
                            for k in range(KC):
                                tb = k % 2
                                S.op("dve", lambda k=k, t0=t0, t1=t1, n=n, r=r, tb=tb: V.scalar_tensor_tensor(
                                    out=tmp[:, tb, 0:n], in0=xT[:, k, t0:t1], scalar=mv(l, r, 3, k),
                                    in1=rstd[:, t0:t1], op0=ALU.mult, op1=ALU.mult),
                                    R=[(xT, None), (rstd, gi), modv], W=[(tmp, tb)])
                                S.act(hfb[:, k, 0:n], tmp[:, tb, 0:n], AF.Identity, R=[(tmp, tb), modv],
                                      W=[(hfb, k)], bias=mv(l, r, 4, k))
                                S.op("pool", lambda k=k, t0=t0, t1=t1, n=n: G.tensor_copy(
                                    out=hT[:, k, t0:t1], in_=hfb[:, k, 0:n]), R=[(hfb, k)], W=[(hT, None)])
                            for ti in range(n // 128):
                                pr = psr[tile_i % 2]
                                rt = rts[tile_i % 2]
                                tile_i += 1
                                for k in range(KC):
                                    S.mm(pr[:, 0:N_EXP], hfb[:, k, ti * 128:(ti + 1) * 128], wr_sb[:, k, :], start=(k == 0),
                                         stop=False, R=[(hfb, None), wr_sb], W=[pr])
                                S.mm(pr[:, 0:N_EXP], onesf[0:1, :], br_sb[:, :], start=False, stop=True, R=[onesf, br_sb], W=[pr])

                                def dv(fn, rt=rt):
                                    S.op("dve", fn, R=[rt], W=[rt])

                                def row(i, w=4, rt=rt):
                                    return rt[:, i, 0:w]
                                lg, ev, cw = row(0, 16), row(2, 16), row(15, 16)
                                nmx, gmax = rt[:, 1, 0:1], rt[:, 1, 1:2]
                                S.op("dve", lambda lg=lg, pr=pr: V.tensor_copy(out=lg, in_=pr[:, 0:N_EXP]), R=[pr], W=[rt])
                                dv(lambda: V.tensor_reduce(out=nmx, in_=lg, axis=mybir.AxisListType.X, op=ALU.max))
                                dv(lambda: V.tensor_scalar(out=nmx, in0=nmx, scalar1=-1.0, scalar2=None, op0=ALU.mult))
                                S.act(ev, lg, AF.Exp, R=[rt], W=[rt], bias=nmx)
                                e3 = ev.rearrange("p (g j) -> p g j", j=4)
                                a_, b_, c_, d_ = (e3[:, :, j] for j in range(4))

                                def tt2(out, i0, i1, op):
                                    dv(lambda: V.tensor_tensor(out=out, in0=i0, in1=i1, op=op))
                                tt2(row(3), a_, b_, ALU.max)
                                tt2(row(4), c_, d_, ALU.max)
                                tt2(row(5), a_, b_, ALU.min)
                                tt2(row(6), c_, d_, ALU.min)
                                tt2(row(7), row(3), row(4), ALU.max)
                                tt2(row(8), row(3), row(4), ALU.min)
                                tt2(row(9), row(5), row(6), ALU.max)
                                tt2(row(10), row(8), row(9), ALU.max)
                                tt2(row(11), row(7), row(10), ALU.add)
                                dv(lambda: V.tensor_reduce(out=gmax, in_=row(11), axis=mybir.AxisListType.X, op=ALU.max))
                                dv(lambda: V.tensor_scalar(out=row(12), in0=row(11), scalar1=gmax, scalar2=None, op0=ALU.is_equal))
                                dv(lambda: V.reciprocal(out=row(13), in_=row(11)))
                                tt2(row(14), row(12), row(13), ALU.mult)
                                cw3 = cw.rearrange("p (g j) -> p g j", j=4)
                                top2b = row(10).rearrange("p (g o) -> p g o", o=1).to_broadcast([128, 4, 4])
                                facb = row(14).rearrange("p (g o) -> p g o", o=1).to_broadcast([128, 4, 4])
                                tt2(cw3, e3, top2b, ALU.is_ge)
                                tt2(cw3, cw3, e3, ALU.mult)
                                tt2(cw3, cw3, facb, ALU.mult)
                                S.op("pe", lambda cw=cw, ti=ti: P.transpose(pscT[:, ti * 128:(ti + 1) * 128], cw, ident[:]),
                                     R=[rt, ident], W=[pscT])
                            S.act(comb[:, t0:t1], pscT[:, 0:n], AF.Identity, R=[pscT], W=[(comb, gi)])
                        S.barrier()

                    if debug == ("comb", b, l):
                        S.dma("sp", dbg_d[0:N_EXP, 0, :], comb[:, :], comb, R=[comb])
                        S.barrier()
                        return nc

                    wts = [[S.sb(ph, f"wg{i}", [128, KC, D_EXP], BF16), S.sb(ph, f"wu{i}", [128, KC, D_EXP], BF16),
                            S.sb(ph, f"wd{i}", [128, 4, D], BF16)] for i in range(2)]
                    cm = [S.sb(ph, f"cm{i}", [N_EXP, 512], F32) for i in range(2)]
                    cbc = [S.sb(ph, f"cbc{i}", [128, 512], F32) for i in range(2)]
                    sg = [S.sb(ph, f"sg{i}", [128, 512], F32) for i in range(2)]
                    hid = [S.sb(ph, f"hid{i}", [128, 4, 512], BF16) for i in range(2)]
                    psg = [S.ps(ph, f"psg{i}", [128, 512], F32) for i in range(2)]
                    psu = [S.ps(ph, f"psu{i}", [128, 512], F32) for i in range(2)]
                    psd = [S.ps(ph, f"psd{i}", [128, 512], F32) for i in range(3)]
                    psc = S.ps(ph, "psc", [128, 512], F32)

                    def load_expert(e):
                        wgt, wut, wdt = wts[e % 2]
                        S.dma("pool", wgt[:], w_gate_d[l, e].rearrange("(k p) n -> p k n", p=128), wgt, W=[wgt])
                        S.dma("pool", wut[:], w_up_d[l, e].rearrange("(k p) n -> p k n", p=128), wut, W=[wut])
                        S.dma("pool", wdt[:], w_down_d[l, e].rearrange("(k p) n -> p k n", p=128), wdt, W=[wdt])

                    seq = [(e, gi) for e in range(N_EXP) for gi in range(len(groups_out))]

                    def prep(it):
                        e, gi = seq[it]
                        t0, t1 = groups_out[gi]
                        n = t1 - t0
                        cb, cmt = cbc[it % 2], cm[it % 2]
                        S.op("dve", lambda cmt=cmt, t0=t0, t1=t1, n=n, e=e: V.tensor_scalar(
                            out=cmt[:, 0:n], in0=comb[:, t0:t1], scalar1=ident[0:N_EXP, e:e + 1], scalar2=None,
                            op0=ALU.mult), R=[(comb, gi), ident], W=[cmt])
                        S.mm(psc[:, 0:n], onesf[0:N_EXP, :], cmt[:, 0:n], True, True, R=[onesf, cmt], W=[psc])
                        S.act(cb[:, 0:n], psc[:, 0:n], AF.Identity, R=[psc], W=[cb])

                    load_expert(0)
                    prep(0)
                    for it, (e, gi) in enumerate(seq):
                        wgt, wut, wdt = wts[e % 2]
                        if gi == 0 and e + 1 < N_EXP:
                            load_expert(e + 1)
                        if it + 1 < len(seq):
                            prep(it + 1)
                        t0, t1 = groups_out[gi]
                        n = t1 - t0
                        r = 2 if t0 < CTX else b
                        cb, hd = cbc[it % 2], hid[it % 2]
                        for hc in range(4):
                            pg, pu, sgt = psg[hc % 2], psu[hc % 2], sg[hc % 2]
                            for k in range(KC):
                                S.mm(pg[:, 0:n], wgt[:, k, hc * 128:(hc + 1) * 128], hT[:, k, t0:t1], start=(k == 0),
                                     stop=(k == KC - 1), R=[wgt, (hT, None)], W=[pg])
                            for k in range(KC):
                                S.mm(pu[:, 0:n], wut[:, k, hc * 128:(hc + 1) * 128], hT[:, k, t0:t1], start=(k == 0),
                                     stop=(k == KC - 1), R=[wut, (hT, None)], W=[pu])
                            S.act(sgt[:, 0:n], pg[:, 0:n], AF.Silu, R=[pg], W=[sgt])
                            S.op("pool", lambda sgt=sgt, cb=cb, n=n: G.tensor_tensor(
                                out=sgt[:, 0:n], in0=sgt[:, 0:n], in1=cb[:, 0:n], op=ALU.mult), R=[sgt, cb], W=[sgt])
                            S.op("dve", lambda hd=hd, hc=hc, pu=pu, sgt=sgt, n=n: V.tensor_tensor(
                                out=hd[:, hc, 0:n], in0=pu[:, 0:n], in1=sgt[:, 0:n], op=ALU.mult),
                                R=[pu, sgt], W=[(hd, hc)])
                        for oc in range(KC):
                            pd = psd[oc % 3]
                            for hc in range(4):
                                S.mm(pd[:, 0:n], wdt[:, hc, oc * 128:(oc + 1) * 128], hd[:, hc, 0:n], start=(hc == 0),
                                     stop=(hc == 3), R=[wdt, (hd, hc)], W=[pd])
                            S.op("dve", lambda oc=oc, t0=t0, t1=t1, n=n, r=r, pd=pd: V.scalar_tensor_tensor(
                                out=xT[:, oc, t0:t1], in0=pd[:, 0:n], scalar=mv(l, r, 5, oc), in1=xT[:, oc, t0:t1],
                                op0=ALU.mult, op1=ALU.add), R=[pd, (xT, gi), modv], W=[(xT, gi)])
                    S.barrier()

                if debug == ("xmoe", b, l):
                    _dump(S, nc, dbg_d, xT, es)
                    return nc

            with ExitStack() as ph:
                sq = S.sb(ph, "sq", [128, 2, 512], BF16)
                rstd = S.sb(ph, "rstd", [128, T], F32)
                xn = [S.sb(ph, f"xn{i}", [128, KC, 512], F32) for i in range(1)]
                ost = [S.sb(ph, f"ost{i}", [128, D], F32) for i in range(2)]
                psn = S.ps(ph, "psn", [128, 512], F32)
                ptr = [S.ps(ph, f"ptr{i}", [128, 512], F32) for i in range(4)]
                def fstats(gi):
                    rms_rstd(ph, xT, list(range(KC)), [GROUPS_LAT[gi]], 1.0 / D, "f", psn, sq, rstd, gis=[gi])
                fstats(0)
                oi = 0
                for gi, (t0, t1) in enumerate(GROUPS_LAT):
                    n = t1 - t0
                    xnb = xn[0]
                    if gi + 1 < len(GROUPS_LAT):
                        fstats(gi + 1)
                    for k in range(KC):
                        S.op("dve", lambda k=k, t0=t0, t1=t1, n=n, xnb=xnb: V.scalar_tensor_tensor(
                            out=xnb[:, k, 0:n], in0=xT[:, k, t0:t1], scalar=ppv("g_fin", k), in1=rstd[:, t0:t1],
                            op0=ALU.mult, op1=ALU.mult), R=[(xT, None), (rstd, gi), ppt], W=[(xnb, None)])
                    for ti in range(n // 128):
                        ot = ost[oi % 2]
                        for hf in range(2):
                            pt = ptr[(oi * 2 + hf) % 4]
                            for kk in range(4):
                                k = hf * 4 + kk
                                S.op("pe", lambda pt=pt, kk=kk, k=k, ti=ti, xnb=xnb: P.transpose(
                                    pt[:, kk * 128:(kk + 1) * 128], xnb[:, k, ti * 128:(ti + 1) * 128], ident[:]),
                                    R=[(xnb, None), ident], W=[pt])
                            if hf == 0:
                                S.act(ot[:, 0:512], pt[:], AF.Identity, R=[pt], W=[(ot, 0)])
                            else:
                                S.op("dve", lambda ot=ot, pt=pt: V.tensor_copy(out=ot[:, 512:1024], in_=pt[:]), R=[pt], W=[(ot, 1)])
                        row0 = t0 - CTX + ti * 128
                        S.dma("sp", out_d[b, row0:row0 + 128, :], ot[:], ot, R=[ot])
                        oi += 1
                S.barrier()

    return nc


def _finish(S, nc, bufs):
    S.barrier()


def _dump(S, nc, dbg_d, buf, es):
    with ExitStack() as ph:
        st = S.sb(ph, "dbgst", [128, T], F32)
        for k in range(KC):
            S.op("dve", lambda k=k: nc.vector.tensor_copy(out=st[:], in_=buf[:, k, :]), R=[buf, st], W=[st])
            S.dma("sp", dbg_d[:, k, :], st[:], st, R=[st])
        S.barrier()


_CACHE = {}


def _prep_shared(inp):
    pp = _pack_params(inp)
    shared = {
        "pp": pp.pack(),
        "w_ada": np.ascontiguousarray(inp["w_ada"], np.float32),
        "w_in": np.ascontiguousarray(inp["w_in"], np.float32),
        "wbd": _wbd(np.asarray(inp["rg_w"], np.float32)),
        "bpat": _build_bias_patterns(np.asarray(inp["na_rpb"], np.float32)),
        "w_out": np.ascontiguousarray(inp["w_out"], np.float32),
        "w_router": np.ascontiguousarray(inp["w_router"], np.float32),
        "b_router": np.ascontiguousarray(inp["b_router"], np.float32).reshape(1, N_EXP),
        "w_gate": np.ascontiguousarray(inp["w_gate"], np.float32),
        "w_up": np.ascontiguousarray(inp["w_up"], np.float32),
        "w_down": np.ascontiguousarray(inp["w_down"], np.float32),
        "ident": np.eye(128, dtype=np.float32),
    }
    return pp, shared


def _core_inputs(inp, shared, core, nb):
    b0 = core * nb
    c = np.asarray(inp["c"], np.float32)[b0:b0 + nb]
    rows = np.concatenate([c, np.asarray(inp["c_ctx"], np.float32)[None]], axis=0)
    if nb == 1:
        rows = np.concatenate([rows[:1], rows[:1], rows[1:]], axis=0)
    condT = np.ascontiguousarray(rows.reshape(3, KC, 128).transpose(2, 1, 0))
    m = dict(shared)
    m["x"] = np.ascontiguousarray(np.asarray(inp["x"], np.float32)[b0:b0 + nb])
    m["ctx"] = np.ascontiguousarray(np.asarray(inp["ctx"], np.float32)[b0:b0 + nb])
    m["condT"] = condT
    return m


def kernel(**inputs):
    inp = {k: np.asarray(v) for k, v in inputs.items()}
    nb = inp["x"].shape[0] // N_CORES
    pp, shared = _prep_shared(inp)
    nc = build_program(pp.off, pp.n, nb=nb)
    in_maps = [_core_inputs(inp, shared, core, nb) for core in range(N_CORES)]
    res = run_bass_kernel_spmd(nc, in_maps, core_ids=list(range(N_CORES)))
    out = np.concatenate([np.asarray(r["out"]) for r in res.results], axis=0)
    return out.astype(np.float32)
```

```python
import numpy as np
from contextlib import ExitStack
import concourse.bass as bass
import concourse.mybir as mybir
from concourse.bass_utils import run_bass_kernel_spmd

F32 = mybir.dt.float32
BF16 = mybir.dt.bfloat16
AF = mybir.ActivationFunctionType
ALU = mybir.AluOpType

N_CORES = 8
D = 1024
KC = 8
SEQ = 2048
CTX = 256
T = CTX + SEQ
DEPTH = 2
IN_W = 2816
COL_Q, COL_K, COL_V, COL_RX, COL_RG, COL_SB, COL_SC, COL_SX = 0, 512, 1024, 1536, 1792, 2048, 2304, 2560
N_EXP = 16
D_EXP = 512
EPS = 1e-6
MASK = -30000.0
GRID_W = 64
ROWS = 32
WIN_R = 8
WIN_C = 16

GROUPS_ALL = [(0, 256), (256, 768), (768, 1280), (1280, 1792), (1792, 2304)]
GROUPS_LAT = GROUPS_ALL[1:]


def _r0(r):
    return min(max(r - WIN_R // 2, 0), ROWS - WIN_R)


def _na_structure():
    pats = {}
    plist = []
    per_tile = []
    for i in range(16):
        rows = set()
        for r in (2 * i, 2 * i + 1):
            rows.update(range(_r0(r), _r0(r) + WIN_R))
        tiles = sorted({kr // 2 for kr in rows})
        ent = []
        for j in tiles:
            key = []
            for a in (0, 1):
                for cq in (0, 1):
                    kr, r = 2 * j + a, 2 * i + cq
                    key.append(kr - r if _r0(r) <= kr < _r0(r) + WIN_R else None)
            key = tuple(key)
            if key not in pats:
                pats[key] = len(plist)
                plist.append(key)
            ent.append((j, pats[key]))
        per_tile.append(ent)
    return per_tile, plist


NA_TILES, NA_PATS = _na_structure()
NPAT = len(NA_PATS)


def _na_slots():
    import itertools
    seqs = []
    for ent in NA_TILES:
        sq = tuple(p for (_, p) in ent)
        if sq not in seqs:
            seqs.append(sq)

    def find(hay, needle):
        for i in range(len(hay) - len(needle) + 1):
            if tuple(hay[i:i + len(needle)]) == tuple(needle):
                return i
        return -1

    best = None
    for perm in itertools.permutations(seqs):
        cur = []
        for sq in perm:
            if find(cur, sq) >= 0:
                continue
            ov = 0
            for k in range(min(len(cur), len(sq)), 0, -1):
                if tuple(cur[-k:]) == tuple(sq[:k]):
                    ov = k
                    break
            cur = cur + list(sq[ov:])
        if best is None or len(cur) < len(best):
            best = cur
    starts = [find(best, tuple(p for (_, p) in ent)) for ent in NA_TILES]
    return best, starts


NA_SLOTS, NA_START = _na_slots()
NSLOT = len(NA_SLOTS)


def _build_bias_patterns(rpb):
    L, H = rpb.shape[0], rpb.shape[1]
    kc = np.arange(GRID_W)[:, None]
    qc = np.arange(GRID_W)[None, :]
    c_start = np.clip(qc - WIN_C // 2, 0, GRID_W - WIN_C)
    ok = (kc >= c_start) & (kc < c_start + WIN_C)
    rel_c = np.clip(kc - qc + WIN_C - 1, 0, 2 * WIN_C - 2)
    out = np.full((L, H, 128, NSLOT, 128), MASK, np.float32)
    for p, pid in enumerate(NA_SLOTS):
        key = NA_PATS[pid]
        idx = 0
        for a in (0, 1):
            for cq in (0, 1):
                dr = key[idx]
                idx += 1
                if dr is None:
                    continue
                blk = np.where(ok[None, None], rpb[:, :, dr + WIN_R - 1][:, :, rel_c], np.float32(MASK))
                out[:, :, 64 * a:64 * a + 64, p, 64 * cq:64 * cq + 64] = blk
    return out


class PP:
    def __init__(self):
        self.off = {}
        self.n = 0
        self.parts = []

    def add(self, name, arr):
        arr = np.ascontiguousarray(arr, dtype=np.float32).reshape(128, -1)
        self.off[name] = (self.n, arr.shape[1])
        self.n += arr.shape[1]
        self.parts.append(arr)

    def pack(self):
        return np.ascontiguousarray(np.concatenate(self.parts, axis=1))


def _chunked(v):
    v = np.asarray(v, np.float32)
    lead = v.shape[:-1]
    n = v.shape[-1] // 128
    v = v.reshape(*lead, n, 128)
    return np.moveaxis(v, -1, 0)


def _pack_params(inp):
    pp = PP()
    pp.add("b_ada", _chunked(inp["b_ada"]))
    pp.add("g_mix", _chunked(inp["norm_mix_g"]))
    pp.add("g_out", _chunked(inp["mix_out_g"]))
    pp.add("g_ffn", _chunked(inp["norm_ffn_g"]))
    pp.add("g_fin", _chunked(inp["final_g"]))
    pp.add("lconv_w", _chunked(inp["lru_conv_w"]))
    pp.add("lconv_b", _chunked(inp["lru_conv_b"]))
    pp.add("rg_b", _chunked(inp["rg_b"]))
    pp.add("rg_lam", _chunked(inp["rg_lam"]))
    pp.add("sconv_w", _chunked(inp["sc_conv_w"]))
    pp.add("sconv_b", _chunked(inp["sc_conv_b"]))
    return pp


def _wbd(rg_w):
    L = rg_w.shape[0]
    out = np.zeros((L, 2, 2, 2, 128, 128), np.float32)
    for c in range(2):
        for a in range(2):
            out[:, :, :, c, 64 * a:64 * a + 64, 64 * a:64 * a + 64] = rg_w[:, :, :, 2 * c + a]
    return out


class Buf:
    def __init__(self, name, t):
        self.name = name
        self.t = t
        self.all_w = None
        self.all_r = {}
        self.reg = {}
        self.sem = None
        self.semv = 0

    def __getitem__(self, idx):
        return self.t[idx]


class Sched:
    def __init__(self, nc, es):
        self.nc = nc
        self.es = es
        self.eng = {"pe": nc.tensor, "act": nc.scalar, "dve": nc.vector, "pool": nc.gpsimd, "sp": nc.sync}
        self.sem = {e: es.enter_context(nc.semaphore("s_" + e)) for e in self.eng}
        self.cnt = {e: 0 for e in self.eng}
        self.waited = {e: {} for e in self.eng}
        self.dsem = {}
        self.nbuf = 0

    def sb(self, es, name, shape, dt=F32):
        self.nbuf += 1
        return Buf(name, es.enter_context(self.nc.sbuf_tensor(f"{name}_{self.nbuf}", list(shape), dt)))

    def ps(self, es, name, shape, dt=F32):
        self.nbuf += 1
        return Buf(name, es.enter_context(self.nc.psum_tensor(f"{name}_{self.nbuf}", list(shape), dt)))

    def wrap(self, name, t):
        return Buf(name, t)

    @staticmethod
    def _norm(items):
        out = []
        for it in items:
            if isinstance(it, Buf):
                out.append((it, None))
            else:
                out.append(it)
        return out

    def _deps(self, reads, writes):
        raw, deps = [], []
        for b, k in reads:
            if b.all_w:
                raw.append(b.all_w)
            if k is None:
                for st in b.reg.values():
                    if st[0]:
                        raw.append(st[0])
            elif k in b.reg and b.reg[k][0]:
                raw.append(b.reg[k][0])
        for b, k in writes:
            if b.all_w:
                deps.append(b.all_w)
            deps.extend(b.all_r.values())
            if k is None:
                for st in b.reg.values():
                    if st[0]:
                        deps.append(st[0])
                    deps.extend(st[1].values())
            elif k in b.reg:
                st = b.reg[k]
                if st[0]:
                    deps.append(st[0])
                deps.extend(st[1].values())
        return raw, deps

    def _record(self, reads, writes, dep):
        key = dep[0]
        for b, k in reads:
            if k is None:
                b.all_r[key] = dep
            else:
                b.reg.setdefault(k, [None, {}])[1][key] = dep
        for b, k in writes:
            if k is None:
                b.all_w = dep
                b.all_r = {}
                b.reg = {}
            else:
                b.reg[k] = [dep, {}]

    def _wait(self, e, deps):
        raw, other = deps
        for lst, same_ok in ((raw, e in ("act", "dve", "pool")), (other, False)):
            for key, sem, val in lst:
                if key == e and not same_ok:
                    continue
                if self.waited[e].get(key, 0) < val:
                    self.eng[e].wait_ge(sem, val)
                    self.waited[e][key] = val

    def op(self, e, emit, R=(), W=(), inc=True):
        R = self._norm(R)
        W = self._norm(W)
        self._wait(e, self._deps(R, W))
        ins = emit()
        if inc:
            ins.then_inc(self.sem[e], 1)
            self.cnt[e] += 1
            tick = self.cnt[e]
        else:
            tick = self.cnt[e] + 1
        self._record(R, W, (e, self.sem[e], tick))

    def dma(self, q, out_ap, in_ap, semb, R=(), W=()):
        R = self._norm(R)
        W = self._norm(W)
        self._wait(q, self._deps(R, W))
        if semb.name not in self.dsem:
            self.dsem[semb.name] = [self.es.enter_context(self.nc.semaphore("d_" + semb.name)), 0]
        ds = self.dsem[semb.name]
        self.eng[q].dma_start(out=out_ap, in_=in_ap).then_inc(ds[0], 16)
        ds[1] += 16
        self._record(R, W, ("dma_" + semb.name, ds[0], ds[1]))

    def barrier(self):
        for e in self.eng:
            deps = [(f, self.sem[f], self.cnt[f]) for f in self.eng if f != e and self.cnt[f] > 0]
            deps += [("dma_" + n, ds[0], ds[1]) for n, ds in self.dsem.items() if ds[1] > 0]
            self._wait(e, ([], deps))

    def act(self, out, in_, func, R, W, **kw):
        self.op("act", lambda: self.nc.scalar.activation(out=out, in_=in_, func=func, **kw), R, W)

    def mm(self, out, lhsT, rhs, start, stop, R, W, inc=None):
        self.op("pe", lambda: self.nc.tensor.matmul(out, lhsT, rhs, start=start, stop=stop), R, W,
                inc=True)


def build_program(pp_off, npp, nb=2, depth=DEPTH, debug=None):
    nc = bass.Bass("TRN2", target_bir_lowering=False)

    def din(name, shape):
        return nc.dram_tensor(name, list(shape), F32, kind="ExternalInput").ap()

    x_d = din("x", [nb, SEQ, D])
    ctx_d = din("ctx", [nb, CTX, D])
    condT_d = din("condT", [128, KC, 3])
    pp_d = din("pp", [128, npp])
    w_ada_d = din("w_ada", [DEPTH, D, 6 * D])
    w_in_d = din("w_in", [DEPTH, D, IN_W])
    wbd_d = din("wbd", [DEPTH, 2, 2, 2, 128, 128])
    bpat_d = din("bpat", [DEPTH, 8, 128, NSLOT, 128])
    w_out_d = din("w_out", [DEPTH, D, D])
    w_router_d = din("w_router", [D, N_EXP])
    b_router_d = din("b_router", [1, N_EXP])
    w_gate_d = din("w_gate", [DEPTH, N_EXP, D, D_EXP])
    w_up_d = din("w_up", [DEPTH, N_EXP, D, D_EXP])
    w_down_d = din("w_down", [DEPTH, N_EXP, D_EXP, D])
    ident_d = din("ident", [128, 128])
    out_d = nc.dram_tensor("out", [nb, SEQ, D], F32, kind="ExternalOutput").ap()
    dbg_d = None
    if debug is not None:
        dbg_d = nc.dram_tensor("dbg", [128, KC, T], F32, kind="ExternalOutput").ap()

    V, A, P, G = nc.vector, nc.scalar, nc.tensor, nc.gpsimd

    with ExitStack() as es:
        S = Sched(nc, es)

        xT = S.sb(es, "xT", [128, KC, T], F32)
        hT = S.sb(es, "hT", [128, KC, T], BF16)
        ppt = S.sb(es, "ppt", [128, npp], F32)
        mod = S.sb(es, "mod", [128, DEPTH, 48, 3], F32)
        modv = S.sb(es, "modv", [128, DEPTH, 3, 6, KC], F32)
        ident = S.sb(es, "ident", [128, 128], F32)
        identb = S.sb(es, "identb", [128, 128], BF16)
        onesb = S.sb(es, "onesb", [128, 128], BF16)
        onesf = S.sb(es, "onesf", [128, 128], F32)
        condT = S.sb(es, "condT", [128, KC, 3], F32)
        wr_sb = S.sb(es, "wr_sb", [128, KC, N_EXP], F32)
        br_sb = S.sb(es, "br_sb", [1, N_EXP], F32)
        cl = S.sb(es, "cl", [128, DEPTH, 2, 2], F32)
        outsem = S.wrap("outsem", None)
        wbuf = S.sb(es, "wbuf", [128, KC, 384], BF16)
        wspecs = []
        for _b in range(nb):
            for _l in range(depth):
                for _c in range(2):
                    wspecs.append((_l, [COL_SB + _c * 128, COL_SC + _c * 128, COL_SX + _c * 128]))
                for _c in range(2):
                    wspecs.append((_l, [COL_RX + _c * 128, COL_RG + _c * 128]))
                for _c in range(4):
                    wspecs.append((_l, [COL_Q + _c * 128, COL_K + _c * 128, COL_V + _c * 128]))
        wnext = [0]

        def issue_w():
            if wnext[0] >= len(wspecs):
                return
            l_, cols = wspecs[wnext[0]]
            wnext[0] += 1
            for j, col in enumerate(cols):
                S.dma("pool", wbuf[:, :, j * 128:(j + 1) * 128],
                      w_in_d[l_].rearrange("(k p) n -> p k n", p=128)[:, :, col:col + 128], wbuf, W=[wbuf])

        def ppv(name, *idx):
            return ppv_shape[name](*idx)

        ppv_shape = {}

        def reg_pp(name, dims):
            o, w = pp_off[name]
            strides = []
            s = 1
            for d in reversed(dims):
                strides.insert(0, s)
                s *= d
            assert s == w, (name, dims, w)

            def f(*idx):
                col = o + sum(i * st for i, st in zip(idx, strides))
                return ppt[:, col:col + 1]
            ppv_shape[name] = f

        reg_pp("b_ada", [DEPTH, 48])
        reg_pp("g_mix", [DEPTH, KC])
        reg_pp("g_out", [DEPTH, KC])
        reg_pp("g_ffn", [DEPTH, KC])
        reg_pp("g_fin", [KC])
        reg_pp("lconv_w", [DEPTH, 4, 2])
        reg_pp("lconv_b", [DEPTH, 2])
        reg_pp("rg_b", [DEPTH, 2, 2, 2])
        reg_pp("rg_lam", [DEPTH, 2, 2])
        reg_pp("sconv_w", [DEPTH, 3, 2])
        reg_pp("sconv_b", [DEPTH, 2])

        S.dma("sp", ppt[:], pp_d[:, :], ppt, W=[ppt])
        S.dma("sp", ident[:], ident_d[:, :], ident, W=[ident])
        S.dma("sp", condT[:], condT_d[:, :, :], condT, W=[condT])
        S.dma("sp", wr_sb[:], w_router_d.rearrange("(k p) e -> p k e", p=128), wr_sb, W=[wr_sb])
        S.dma("sp", br_sb[:], b_router_d[:, :], br_sb, W=[br_sb])
        S.op("dve", lambda: V.tensor_copy(out=identb[:], in_=ident[:]), R=[ident], W=[identb])
        S.op("dve", lambda: V.memset(onesb[:], 1.0), W=[onesb])
        S.op("dve", lambda: V.memset(onesf[:], 1.0), W=[onesf])
        S.act(condT[:], condT[:], AF.Silu, R=[condT], W=[condT])
        o_lam, w_lam = pp_off["rg_lam"]
        clv = cl[:].rearrange("p a b c -> p (a b c)")
        S.act(clv, ppt[:, o_lam:o_lam + w_lam], AF.Exp, R=[ppt], W=[cl], scale=-1.0)
        S.act(clv, clv, AF.Ln, R=[cl], W=[cl], bias=1.0)
        S.op("dve", lambda: V.tensor_scalar(out=clv, in0=clv, scalar1=-8.0, scalar2=None, op0=ALU.mult), R=[cl], W=[cl])

        o_rgb, w_rgb = pp_off["rg_b"]
        nrgb = S.sb(es, "nrgb", [128, w_rgb], F32)
        S.op("dve", lambda: V.tensor_scalar(out=nrgb[:], in0=ppt[:, o_rgb:o_rgb + w_rgb], scalar1=-1.0, scalar2=None,
                                            op0=ALU.mult), R=[ppt], W=[nrgb])

        def nrgbv(l_, d_, g_, c_):
            col = ((l_ * 2 + d_) * 2 + g_) * 2 + c_
            return nrgb[:, col:col + 1]

        if debug == ("const", 0, 0):
            S.barrier()
            S.dma("sp", dbg_d[:, 0, 0:128], ident[:], ident, R=[ident])
            S.dma("sp", dbg_d[:, 1, 0:8], cl[:].rearrange("p a b c -> p (a b c)"), cl, R=[cl])
            S.dma("sp", dbg_d[:, 2, 0:24], condT[:].rearrange("p a b -> p (a b)"), condT, R=[condT])
            S.barrier()
            return nc
        with ExitStack() as ph:
            wa = [S.sb(ph, f"wa{i}", [128, KC, 768], F32) for i in range(2)]
            pmod = S.ps(ph, "pmod", [128, 512], F32)
            it = 0
            for l in range(depth):
                for cg in range(8):
                    wb = wa[it % 2]
                    it += 1
                    S.dma("sp" if it % 2 else "act", wb[:],
                          w_ada_d[l].rearrange("(k p) n -> p k n", p=128)[:, :, cg * 768:(cg + 1) * 768], wb, W=[wb])
                    for jj in range(6):
                        j = cg * 6 + jj
                        for k in range(KC):
                            S.mm(pmod[:, jj * 3:jj * 3 + 3], wb[:, k, jj * 128:(jj + 1) * 128], condT[:, k, :],
                                 start=(k == 0), stop=(k == KC - 1), R=[wb, condT], W=[pmod])
                    for jj in range(6):
                        j = cg * 6 + jj
                        S.op("dve", lambda j=j, jj=jj: V.tensor_scalar(
                            out=mod[:, l, j, :], in0=pmod[:, jj * 3:jj * 3 + 3], scalar1=ppv("b_ada", l, j),
                            scalar2=None, op0=ALU.add), R=[pmod, ppt], W=[mod])
            for l in range(depth):
                for r in range(3):
                    for (dst, mi, gname) in ((0, 1, "g_mix"), (3, 4, "g_ffn")):
                        og, _ = pp_off[gname]
                        S.op("dve", lambda l=l, r=r, dst=dst, mi=mi, og=og: V.scalar_tensor_tensor(
                            out=modv[:, l, r, dst, :], in0=mod[:, l, mi * 8:(mi + 1) * 8, r], scalar=1.0,
                            in1=ppt[:, og + l * KC:og + (l + 1) * KC], op0=ALU.add, op1=ALU.mult),
                            R=[mod, ppt], W=[modv])
                    for (dst, mi) in ((1, 0), (2, 2), (4, 3), (5, 5)):
                        S.op("dve", lambda l=l, r=r, dst=dst, mi=mi: V.tensor_copy(
                            out=modv[:, l, r, dst, :], in_=mod[:, l, mi * 8:(mi + 1) * 8, r]), R=[mod], W=[modv])
            S.barrier()

        def mv(l, r, which, k):
            return modv[:, l, r, which, k:k + 1]

        if debug == ("mod", 0, 0):
            S.dma("sp", dbg_d[:, 0, 0:DEPTH * 48 * 3], mod[:].rearrange("p a b c -> p (a b c)"), mod, R=[mod])
            S.dma("sp", dbg_d[:, 1, 0:DEPTH * 3 * 6 * KC], modv[:].rearrange("p a b c d -> p (a b c d)"), modv, R=[modv])
            S.barrier()
            return nc

        def rms_rstd(ph, src, chunks, groups, scale, name, psum, sq, rstd_buf, rkey=lambda gi: gi, gis=None):
            for gi_, (t0, t1) in enumerate(groups):
                gi = gi_ if gis is None else gis[gi_]
                n = t1 - t0
                for ci, k in enumerate(chunks):
                    S.act(sq[:, ci % 2, 0:n], src[:, k, t0:t1], AF.Square, R=[(src, rkey(gi))], W=[(sq, ci % 2)])
                    S.mm(psum[:, 0:n], onesb[:], sq[:, ci % 2, 0:n], start=(ci == 0), stop=(ci == len(chunks) - 1),
                         R=[(sq, ci % 2), onesb], W=[psum])
                S.act(rstd_buf[:, t0:t1], psum[:, 0:n], AF.Sqrt, R=[psum], W=[(rstd_buf, gi)], scale=scale, bias=EPS)
                S.op("dve", lambda t0=t0, t1=t1: V.reciprocal(out=rstd_buf[:, t0:t1], in_=rstd_buf[:, t0:t1]),
                     R=[(rstd_buf, gi)], W=[(rstd_buf, gi)])

        def gkey(groups, gi):
            return GROUPS_ALL.index(groups[gi])

        def dbgdump(slot, ap, n, Rbufs, parts=128):
            with ExitStack() as dph:
                n = min(n, 2048)
                st = S.sb(dph, "dbgst", [128, 2048], F32)
                S.op("dve", lambda: V.tensor_copy(out=st[0:parts, 0:n], in_=ap[:, 0:n]), R=Rbufs, W=[st])
                S.dma("sp", dbg_d[0:parts, slot, 0:n], st[0:parts, 0:n], st, R=[st])
                S.barrier()

        issue_w()
        for b in range(nb):
            with ExitStack() as ph:
                stg = [S.sb(ph, f"stg{i}", [128, D], F32) for i in range(2)]
                ptr = [S.ps(ph, f"ptr{i}", [128, 512], F32) for i in range(4)]
                for tt in range(T // 128):
                    st = stg[tt % 2]
                    src = ctx_d[b, tt * 128:(tt + 1) * 128, :] if tt < 2 else x_d[b, (tt - 2) * 128:(tt - 1) * 128, :]
                    S.dma("sp", st[:], src, st, W=[st])
                    for hf in range(2):
                        pt = ptr[(tt * 2 + hf) % 4]
                        for kk in range(4):
                            k = hf * 4 + kk
                            S.op("pe", lambda pt=pt, kk=kk, k=k, st=st: P.transpose(
                                pt[:, kk * 128:(kk + 1) * 128], st[:, k * 128:(k + 1) * 128], ident[:]),
                                R=[st, ident], W=[pt])
                        eng = "act" if hf == 0 else "dve"
                        dst = xT[:, hf * 4:hf * 4 + 4, tt * 128:(tt + 1) * 128]
                        srcp = pt[:].rearrange("p (k t) -> p k t", t=128)
                        if eng == "act":
                            S.act(dst, srcp, AF.Identity, R=[pt], W=[(xT, tt)])
                        else:
                            S.op("dve", lambda dst=dst, srcp=srcp: V.tensor_copy(out=dst, in_=srcp), R=[pt], W=[(xT, tt)])
                S.barrier()

            if debug == ("load", b, 0):
                _dump(S, nc, dbg_d, xT, es)
                return nc
            for l in range(depth):
                last = (l == DEPTH - 1)
                groups_out = GROUPS_LAT if last else GROUPS_ALL

                def norm_mod(ph, which_g, which_s, groups):
                    sq = S.sb(ph, "sq", [128, 2, 512], BF16)
                    rstd = S.sb(ph, "rstd", [128, T], F32)
                    tmp = S.sb(ph, "ntmp", [128, 2, 512], F32)
                    psn = S.ps(ph, "psn", [128, 512], F32)
                    def stats(gi):
                        rms_rstd(ph, xT, list(range(KC)), [groups[gi]], 1.0 / D, "n", psn, sq, rstd, gis=[gi])
                    stats(0)
                    for gi, (t0, t1) in enumerate(groups):
                        n = t1 - t0
                        r = 2 if t0 < CTX else b
                        if gi + 1 < len(groups):
                            stats(gi + 1)
                        for k in range(KC):
                            tb = k % 2
                            S.op("dve", lambda k=k, t0=t0, t1=t1, n=n, r=r, tb=tb: V.scalar_tensor_tensor(
                                out=tmp[:, tb, 0:n], in0=xT[:, k, t0:t1], scalar=mv(l, r, which_g, k),
                                in1=rstd[:, t0:t1], op0=ALU.mult, op1=ALU.mult),
                                R=[(xT, None), (rstd, gi), modv], W=[(tmp, tb)])
                            S.act(hT[:, k, t0:t1], tmp[:, tb, 0:n], AF.Identity, R=[(tmp, tb), modv],
                                  W=[(hT, gkey(groups, gi))], bias=mv(l, r, which_s, k))

                with ExitStack() as ph:
                    norm_mod(ph, 0, 1, GROUPS_ALL)
                    S.barrier()

                if debug == ("h1", b, l):
                    _dump(S, nc, dbg_d, hT, es)
                    return nc

                with ExitStack() as mx:
                    yT = S.sb(mx, "yT", [128, KC, T], BF16)

                    def proj_fm(ph, wsb, wcol0, m, dst_fn, groups, ps_list, base=0):
                        for gi, (t0, t1) in enumerate(groups):
                            n = t1 - t0
                            pt = ps_list[gi % len(ps_list)]
                            for k in range(KC):
                                S.mm(pt[0:m, 0:n], wsb[:, k, wcol0:wcol0 + m], hT[:, k, t0:t1], start=(k == 0),
                                     stop=(k == KC - 1), R=[wsb, (hT, gkey(groups, gi))], W=[pt])
                            dst_fn(gi, t0, t1, pt)

                    for c in range(2):
                        with ExitStack() as ph:
                            wsb = wbuf
                            sbv = S.sb(ph, "sbv", [128, T], F32)
                            pv = S.sb(ph, "pv", [128, T], F32)
                            acc = S.sb(ph, "acc", [128, T], F32)
                            pss = [S.ps(ph, f"pss{i}", [128, 512], F32) for i in range(4)]

                            def ev_sb(gi, t0, t1, pt):
                                S.act(sbv[:, t0:t1], pt[:, 0:t1 - t0], AF.Identity, R=[pt], W=[(sbv, gi)])

                            def ev_sc(gi, t0, t1, pt):
                                S.act(pv[:, t0:t1], pt[:, 0:t1 - t0], AF.Identity, R=[pt], W=[(pv, gi)])

                            def ev_sx(gi, t0, t1, pt):
                                S.op("dve", lambda: V.tensor_tensor(out=pv[:, t0:t1], in0=pt[:, 0:t1 - t0],
                                                                    in1=pv[:, t0:t1], op=ALU.mult),
                                     R=[pt, (pv, gi)], W=[(pv, gi)])

                            proj_fm(ph, wsb, 0, 128, ev_sb, GROUPS_ALL, pss)
                            proj_fm(ph, wsb, 128, 128, ev_sc, GROUPS_ALL, pss)
                            proj_fm(ph, wsb, 256, 128, ev_sx, GROUPS_ALL, pss)
                            issue_w()
                            for (s0, s1) in ((0, CTX), (CTX, T)):
                                S.op("dve", lambda s0=s0, s1=s1: V.tensor_scalar(
                                    out=acc[:, s0:s1], in0=pv[:, s0:s1], scalar1=ppv("sconv_w", l, 1, c),
                                    scalar2=ppv("sconv_b", l, c), op0=ALU.mult, op1=ALU.add), R=[pv, ppt], W=[acc])
                                S.op("dve", lambda s0=s0, s1=s1: V.scalar_tensor_tensor(
                                    out=acc[:, s0 + 1:s1], in0=pv[:, s0:s1 - 1], scalar=ppv("sconv_w", l, 0, c),
                                    in1=acc[:, s0 + 1:s1], op0=ALU.mult, op1=ALU.add), R=[pv, ppt, acc], W=[acc])
                                S.op("dve", lambda s0=s0, s1=s1: V.scalar_tensor_tensor(
                                    out=acc[:, s0:s1 - 1], in0=pv[:, s0 + 1:s1], scalar=ppv("sconv_w", l, 2, c),
                                    in1=acc[:, s0:s1 - 1], op0=ALU.mult, op1=ALU.add), R=[pv, ppt, acc], W=[acc])
                            S.op("dve", lambda: V.tensor_tensor(out=yT[:, 6 + c, :], in0=sbv[:], in1=acc[:], op=ALU.mult),
                                 R=[sbv, acc], W=[yT])
                            S.barrier()

                    for c in range(2):
                        with ExitStack() as ph:
                            wsb = wbuf
                            wg = S.sb(ph, "wbd", [128, 2, 2, 128], F32)
                            for d in range(2):
                                for g in range(2):
                                    S.dma("sp", wg[:, d, g, :], wbd_d[l, d, g, c, :, :], wg, W=[wg])
                            rxc = S.sb(ph, "rxc", [128, T], F32)
                            a_f = S.sb(ph, "a_f", [128, T], F32)
                            a_r = S.sb(ph, "a_r", [128, T], F32)
                            bb = S.sb(ph, "bb", [128, T], F32)
                            gt = S.sb(ph, "gt", [128, 4, 512], F32)
                            psl = [S.ps(ph, f"psl{i}", [128, 512], F32) for i in range(4)]
                            rx_raw = a_f

                            def ev_rx(gi, t0, t1, pt):
                                S.act(rx_raw[:, t0:t1], pt[:, 0:t1 - t0], AF.Identity, R=[pt], W=[(rx_raw, gi)])

                            def ev_rg(gi, t0, t1, pt):
                                S.act(yT[:, 4 + c, t0:t1], pt[:, 0:t1 - t0], AF.Gelu, R=[pt], W=[(yT, gi)])

                            proj_fm(ph, wsb, 0, 128, ev_rx, GROUPS_ALL, psl)
                            proj_fm(ph, wsb, 128, 128, ev_rg, GROUPS_ALL, psl)
                            issue_w()
                            for (s0, s1) in ((0, CTX), (CTX, T)):
                                S.op("dve", lambda s0=s0, s1=s1: V.tensor_scalar(
                                    out=rxc[:, s0:s1], in0=rx_raw[:, s0:s1], scalar1=ppv("lconv_w", l, 2, c),
                                    scalar2=ppv("lconv_b", l, c), op0=ALU.mult, op1=ALU.add), R=[rx_raw, ppt], W=[rxc])
                                for (kk, sh) in ((0, 2), (1, 1)):
                                    S.op("dve", lambda s0=s0, s1=s1, kk=kk, sh=sh: V.scalar_tensor_tensor(
                                        out=rxc[:, s0 + sh:s1], in0=rx_raw[:, s0:s1 - sh], scalar=ppv("lconv_w", l, kk, c),
                                        in1=rxc[:, s0 + sh:s1], op0=ALU.mult, op1=ALU.add), R=[rx_raw, ppt, rxc], W=[rxc])
                                S.op("dve", lambda s0=s0, s1=s1: V.scalar_tensor_tensor(
                                    out=rxc[:, s0:s1 - 1], in0=rx_raw[:, s0 + 1:s1], scalar=ppv("lconv_w", l, 3, c),
                                    in1=rxc[:, s0:s1 - 1], op0=ALU.mult, op1=ALU.add), R=[rx_raw, ppt, rxc], W=[rxc])
                            for d in range(2):
                                adst = a_f if d == 0 else a_r
                                for gi, (t0, t1) in enumerate(GROUPS_ALL):
                                    n = t1 - t0
                                    if d == 0:
                                        q0, q1 = t0, t1
                                    elif t0 < CTX:
                                        q0, q1 = 0, CTX
                                    else:
                                        q0 = CTX + (T - t1)
                                        q1 = q0 + n
                                    pr, pi = psl[0 + 2 * (gi % 2)], psl[1 + 2 * (gi % 2)]
                                    S.mm(pr[:, 0:n], wg[:, d, 0, :], rxc[:, t0:t1], True, True, R=[wg, rxc], W=[pr])
                                    S.mm(pi[:, 0:n], wg[:, d, 1, :], rxc[:, t0:t1], True, True, R=[wg, rxc], W=[pi])
                                    ii = gt[:, gi % 2, 0:n]

                                    def rv(ap):
                                        return ap[:, ::-1] if d == 1 else ap
                                    S.act(adst[:, q0:q1], rv(pr[:, 0:n]), AF.Sigmoid, R=[pr, ppt], W=[(adst, gi)],
                                          bias=ppv("rg_b", l, d, 0, c))
                                    S.act(ii, pi[:, 0:n], AF.Sigmoid, R=[pi, ppt], W=[(gt, gi % 2)], bias=ppv("rg_b", l, d, 1, c))
                                    S.op("dve", lambda ii=ii, t0=t0, t1=t1, q0=q0, q1=q1, rv=rv: V.tensor_tensor(
                                        out=bb[:, q0:q1], in0=rv(ii), in1=rv(rxc[:, t0:t1]), op=ALU.mult),
                                        R=[(gt, gi % 2), rxc], W=[(bb, gi)])
                                S.act(adst[:, :], adst[:, :], AF.Exp, R=[adst, cl], W=[adst], scale=cl[:, l, d, c:c + 1])
                                for gi, (t0, t1) in enumerate(GROUPS_ALL):
                                    n = t1 - t0
                                    mm_ = gt[:, 2 + gi % 2, 0:n]
                                    S.op("pool", lambda mm_=mm_, t0=t0, t1=t1, adst=adst: G.tensor_tensor(
                                        out=mm_, in0=adst[:, t0:t1], in1=adst[:, t0:t1], op=ALU.mult),
                                        R=[adst], W=[(gt, 2 + gi % 2)])
                                    S.act(mm_, mm_, AF.Sqrt, R=[(gt, 2 + gi % 2)], W=[(gt, 2 + gi % 2)], scale=-1.0, bias=1.0)
                                    S.op("dve", lambda mm_=mm_, t0=t0, t1=t1: V.tensor_tensor(
                                        out=bb[:, t0:t1], in0=bb[:, t0:t1], in1=mm_, op=ALU.mult),
                                        R=[(gt, 2 + gi % 2), bb], W=[bb])
                                if debug == ("lru", b, l) and c == 0 and d == 0:
                                    dbgdump(0, rxc[:, :], T, [rxc])
                                    dbgdump(1, a_f[:, :], T, [a_f])
                                    dbgdump(2, bb[:, :], T, [bb])
                                    dbgdump(3, gt[:, :, :].rearrange("p a b -> p (a b)"), 2048, [gt])
                                    dbgdump(4, cl[:].rearrange("p a b c -> p (a b c)"), 8, [cl])
                                S.op("dve", lambda adst=adst: V.tensor_tensor_scan(
                                    out=adst[:, :], data0=adst[:, :], data1=bb[:, :], initial=0.0,
                                    op0=ALU.mult, op1=ALU.add), R=[adst, bb], W=[adst])
                            if debug == ("lru", b, l) and c == 0:
                                dbgdump(5, a_f[:, :], T, [a_f])
                                dbgdump(6, a_r[:, :], T, [a_r])
                                dbgdump(7, yT[:, 4 + c, :], T, [yT])
                                return nc
                            for (s0, s1) in ((0, CTX), (CTX, T)):
                                S.op("dve", lambda s0=s0, s1=s1: V.tensor_tensor(
                                    out=bb[:, s0:s1], in0=a_f[:, s0:s1], in1=a_r[:, s0:s1][:, ::-1], op=ALU.add),
                                    R=[a_f, a_r], W=[bb])
                            S.op("dve", lambda: V.tensor_tensor(out=yT[:, 4 + c, :], in0=bb[:], in1=yT[:, 4 + c, :],
                                                                op=ALU.mult), R=[bb, yT], W=[yT])
                            S.barrier()

                    qtiles = list(range(0 if not last else 2, T // 128))
                    with ExitStack() as nas:
                        bpb = [S.sb(nas, f"bp{i}", [128, 2, NSLOT, 128], BF16) for i in range(2)]

                        def load_bp(c_):
                            for hh_ in range(2):
                                S.dma("pool", bpb[c_ % 2][:, hh_, :, :], bpat_d[l, 2 * c_ + hh_, :, :, :], bpb[c_ % 2],
                                      W=[bpb[c_ % 2]])

                        load_bp(0)
                        for c in range(4):
                            with ExitStack() as ph:
                                wsb = wbuf
                                bp = bpb[c % 2]
                                QT = S.sb(ph, "QT", [128, T], BF16)
                                KT = S.sb(ph, "KT", [128, T], BF16)
                                Vp = S.sb(ph, "Vp", [128, T // 128, 2, 65], BF16)
                                PT = [S.sb(ph, f"PT{i}", [128, 7 * 128], BF16) for i in range(2)]
                                otok = [S.sb(ph, f"otok{i}", [128, 2, 64], BF16) for i in range(2)]
                                rec = [S.sb(ph, f"rec{i}", [128, 2, 1], F32) for i in range(2)]
                                psq = [S.ps(ph, f"psq{i}", [128, 512], F32) for i in range(2)]
                                pst = [S.ps(ph, f"pst{i}", [128, 1024], F32) for i in range(2)]
                                pso = S.ps(ph, "pso", [128, 2, 2, 65], F32)
                                pstr = S.ps(ph, "pstr", [128, 128], BF16)
                                S.op("dve", lambda: V.memset(Vp[:, :, :, 64:65], 1.0), W=[Vp])

                                def ev_q(gi, t0, t1, pt):
                                    S.act(QT[:, t0:t1], pt[:, 0:t1 - t0], AF.Identity, R=[pt], W=[(QT, gi)], scale=0.125)

                                def ev_k(gi, t0, t1, pt):
                                    S.op("dve", lambda: V.tensor_copy(out=KT[:, t0:t1], in_=pt[:, 0:t1 - t0]),
                                         R=[pt], W=[(KT, gi)])
                                proj_fm(ph, wsb, 0, 128, ev_q, GROUPS_LAT if last else GROUPS_ALL, psq)
                                proj_fm(ph, wsb, 128, 128, ev_k, GROUPS_ALL, psq)
                                for tt in range(T // 128):
                                    pt = psq[tt % 2]
                                    for k in range(KC):
                                        S.mm(pt[:, 0:128], hT[:, k, tt * 128:(tt + 1) * 128], wsb[:, k, 256:384], start=(k == 0),
                                             stop=(k == KC - 1), R=[wsb, (hT, None)], W=[pt])
                                    S.act(Vp[:, tt, :, 0:64], pt[:, 0:128].rearrange("p (h d) -> p h d", d=64), AF.Identity,
                                          R=[pt], W=[(Vp, tt)])
                                issue_w()
                                if c + 1 < 4:
                                    load_bp(c + 1)
                                bpf = bp[:].rearrange("p a b c -> p (a b c)")
                                S.act(bpf, bpf, AF.Exp, R=[bp], W=[bp])
                                pairs = [(qi, qt, hh) for qi, qt in enumerate(qtiles) for hh in range(2)]

                                def blk(qt):
                                    if qt < 2:
                                        return [0, 1], 0, 0
                                    return ([0, 1] + [2 + j for (j, p) in NA_TILES[qt - 2]], len(NA_TILES[qt - 2]),
                                            NA_START[qt - 2])

                                def scores(i):
                                    qi, qt, hh = pairs[i]
                                    blocks, nl, s0 = blk(qt)
                                    nbk = len(blocks)
                                    ps_s = pst[i % 2]
                                    ptb = PT[i % 2]
                                    for bi, kt in enumerate(blocks):
                                        S.mm(ps_s[:, bi * 128:(bi + 1) * 128], KT[hh * 64:(hh + 1) * 64, kt * 128:(kt + 1) * 128],
                                             QT[hh * 64:(hh + 1) * 64, qt * 128:(qt + 1) * 128], start=True, stop=True,
                                             R=[(KT, None), (QT, None)], W=[ps_s])
                                    for (c0, c1) in ((0, min(nbk, 4)), (4, nbk)):
                                        if c1 > c0:
                                            S.act(ptb[:, c0 * 128:c1 * 128], ps_s[:, c0 * 128:c1 * 128], AF.Exp, R=[ps_s], W=[ptb])
                                    if nl:
                                        S.op("dve", lambda ptb=ptb, hh=hh, nl=nl, s0=s0: V.tensor_tensor(
                                            out=ptb[:, 256:256 + nl * 128].rearrange("p (a b) -> p a b", b=128),
                                            in0=ptb[:, 256:256 + nl * 128].rearrange("p (a b) -> p a b", b=128),
                                            in1=bp[:, hh, s0:s0 + nl, :], op=ALU.mult), R=[ptb, bp], W=[ptb])

                                def pv(i):
                                    qi, qt, hh = pairs[i]
                                    blocks, nl, s0 = blk(qt)
                                    nbk = len(blocks)
                                    ptb = PT[i % 2]
                                    po = pso[:, qi % 2]
                                    for bi, kt in enumerate(blocks):
                                        S.mm(po[:, hh, :], ptb[:, bi * 128:(bi + 1) * 128], Vp[:, kt, hh, :], start=(bi == 0),
                                             stop=(bi == nbk - 1), R=[ptb, (Vp, None)], W=[(pso, qi % 2)])

                                def normalise(qi, qt):
                                    po = pso[:, qi % 2]
                                    pok = (pso, qi % 2)
                                    rc, ot = rec[qi % 2], otok[qi % 2]
                                    S.op("dve", lambda rc=rc, po=po: V.reciprocal(out=rc[:], in_=po[:, :, 64:65]), R=[pok], W=[rc])
                                    S.op("dve", lambda rc=rc, po=po, ot=ot: V.tensor_tensor(
                                        out=ot[:], in0=po[:, :, 0:64], in1=rc[:].to_broadcast([128, 2, 64]), op=ALU.mult),
                                        R=[pok, rc], W=[ot])

                                def to_fm(qi, qt):
                                    ot = otok[qi % 2]
                                    S.op("pe", lambda ot=ot: P.transpose(pstr[:], ot[:].rearrange("p h d -> p (h d)"), identb[:]),
                                         R=[ot, identb], W=[pstr])
                                    S.act(yT[:, c, qt * 128:(qt + 1) * 128], pstr[:], AF.Identity, R=[pstr], W=[(yT, ("na", qt))])

                                scores(0)
                                deferred = None
                                for i in range(len(pairs)):
                                    if i + 1 < len(pairs):
                                        scores(i + 1)
                                    if deferred is not None:
                                        to_fm(*deferred)
                                        deferred = None
                                    pv(i)
                                    qi, qt, hh = pairs[i]
                                    if hh == 1:
                                        normalise(qi, qt)
                                        deferred = (qi, qt)
                                if deferred is not None:
                                    to_fm(*deferred)
                                S.barrier()

                    if debug == ("y", b, l):
                        _dump(S, nc, dbg_d, yT, es)
                        return nc

                    with ExitStack() as ph:
                        wo = S.sb(ph, "wo", [128, KC, D], BF16)
                        for hf in range(2):
                            S.dma("pool", wo[:, :, hf * 512:(hf + 1) * 512],
                                  w_out_d[l].rearrange("(k p) n -> p k n", p=128)[:, :, hf * 512:(hf + 1) * 512], wo, W=[wo])
                        sq = S.sb(ph, "sq", [128, 2, 512], BF16)
                        rstds = [S.sb(ph, f"rstd{i}", [128, T], F32) for i in range(3)]
                        psn = S.ps(ph, "psn", [128, 512], F32)
                        pso2 = [S.ps(ph, f"pso2{i}", [128, 512], F32) for i in range(4)]
                        grp_chunks = ([0, 1, 2, 3], [4, 5], [6, 7])
                        def ostats(gi):
                            for gidx, chunks in enumerate(grp_chunks):
                                rms_rstd(ph, yT, chunks, [groups_out[gi]], 1.0 / (128 * len(chunks)), "g", psn, sq, rstds[gidx],
                                         rkey=lambda gi_: ("o", gi_), gis=[gi])
                        ostats(0)
                        for gi, (t0, t1) in enumerate(groups_out):
                            n = t1 - t0
                            r = 2 if t0 < CTX else b
                            if gi + 1 < len(groups_out):
                                ostats(gi + 1)
                            for gidx, chunks in enumerate(grp_chunks):
                                for k in chunks:
                                    S.op("dve", lambda k=k, t0=t0, t1=t1, gidx=gidx: V.scalar_tensor_tensor(
                                        out=yT[:, k, t0:t1], in0=yT[:, k, t0:t1], scalar=ppv("g_out", l, k),
                                        in1=rstds[gidx][:, t0:t1], op0=ALU.mult, op1=ALU.mult),
                                        R=[(yT, ("o", gi)), (rstds[gidx], gi), ppt], W=[(yT, ("o", gi))])
                            for oc in range(KC):
                                pt = pso2[oc % 4]
                                for k in range(KC):
                                    S.mm(pt[:, 0:n], wo[:, k, oc * 128:(oc + 1) * 128], yT[:, k, t0:t1], start=(k == 0),
                                         stop=(k == KC - 1), R=[wo, (yT, ("o", gi))], W=[pt])
                                S.op("dve", lambda oc=oc, t0=t0, t1=t1, n=n, r=r, pt=pt: V.scalar_tensor_tensor(
                                    out=xT[:, oc, t0:t1], in0=pt[:, 0:n], scalar=mv(l, r, 2, oc), in1=xT[:, oc, t0:t1],
                                    op0=ALU.mult, op1=ALU.add), R=[pt, (xT, None), modv], W=[(xT, None)])
                        S.barrier()

                if debug == ("xmix", b, l):
                    _dump(S, nc, dbg_d, xT, es)
                    return nc

                with ExitStack() as ph:
                    ng = len(groups_out)
                    tok0 = groups_out[0][0]
                    comb = S.sb(ph, "comb", [N_EXP, T], F32)
                    with ExitStack() as ph2:
                        sq = S.sb(ph2, "sq", [128, 2, 512], BF16)
                        rstd = S.sb(ph2, "rstd", [128, T], F32)
                        tmp = S.sb(ph2, "ntmp", [128, 2, 512], F32)
                        hfb = S.sb(ph2, "hfb", [128, KC, 512], F32)
                        psn = S.ps(ph2, "psn", [128, 512], F32)
                        psr = [S.ps(ph2, f"psr{i}", [128, 512], F32) for i in range(2)]
                        pscT = S.ps(ph2, "pscT", [N_EXP, 512], F32)
                        rts = [S.sb(ph2, f"rt{i}", [128, 16, 16], F32) for i in range(2)]
                        rms_rstd(ph2, xT, list(range(KC)), groups_out, 1.0 / D, "n2", psn, sq, rstd)
                        tile_i = 0
                        for gi, (t0, t1) in enumerate(groups_out):
                            n = t1 - t0
                            r = 2 if t0 < CTX else b
                            for k in range(KC):
                                tb = k % 2
                                S.op("dve", lambda k=k, t0=t0, t1=t1, n=n, r=r, tb=tb: V.scalar_tensor_tensor(
                                    out=tmp[:, tb, 0:n], in0=xT[:, k, t0:t1], scalar=mv(l, r, 3, k),
                                    in1=rstd[:, t0:t1], op0=ALU.mult, op1=ALU.mult),
                                    R=[(xT, None), (rstd, gi), modv], W=[(tmp, tb)])
                                S.act(hfb[:, k, 0:n], tmp[:, tb, 0:n], AF.Identity, R=[(tmp, tb), modv],
                                      W=[(hfb, k)], bias=mv(l, r, 4, k))
                                S.op("pool", lambda k=k, t0=t0, t1=t1, n=n: G.tensor_copy(
                                    out=hT[:, k, t0:t1], in_=hfb[:, k, 0:n]), R=[(hfb, k)], W=[(hT, None)])
                            for ti in range(n // 128):
                                pr = psr[tile_i % 2]
                                rt = rts[tile_i % 2]
                                tile_i += 1
                                for k in range(KC):
                                    S.mm(pr[:, 0:N_EXP], hfb[:, k, ti * 128:(ti + 1) * 128], wr_sb[:, k, :], start=(k == 0),
                                         stop=False, R=[(hfb, None), wr_sb], W=[pr])
                                S.mm(pr[:, 0:N_EXP], onesf[0:1, :], br_sb[:, :], start=False, stop=True, R=[onesf, br_sb], W=[pr])

                                def dv(fn, rt=rt):
                                    S.op("dve", fn, R=[rt], W=[rt])

                                def row(i, w=4, rt=rt):
                                    return rt[:, i, 0:w]
                                lg, ev, cw = row(0, 16), row(2, 16), row(15, 16)
                                nmx, gmax = rt[:, 1, 0:1], rt[:, 1, 1:2]
                                S.op("dve", lambda lg=lg, pr=pr: V.tensor_copy(out=lg, in_=pr[:, 0:N_EXP]), R=[pr], W=[rt])
                                dv(lambda: V.tensor_reduce(out=nmx, in_=lg, axis=mybir.AxisListType.X, op=ALU.max))
                                dv(lambda: V.tensor_scalar(out=nmx, in0=nmx, scalar1=-1.0, scalar2=None, op0=ALU.mult))
                                S.act(ev, lg, AF.Exp, R=[rt], W=[rt], bias=nmx)
                                e3 = ev.rearrange("p (g j) -> p g j", j=4)
                                a_, b_, c_, d_ = (e3[:, :, j] for j in range(4))

                                def tt2(out, i0, i1, op):
                                    dv(lambda: V.tensor_tensor(out=out, in0=i0, in1=i1, op=op))
                                tt2(row(3), a_, b_, ALU.max)
                                tt2(row(4), c_, d_, ALU.max)
                                tt2(row(5), a_, b_, ALU.min)
                                tt2(row(6), c_, d_, ALU.min)
                                tt2(row(7), row(3), row(4), ALU.max)
                                tt2(row(8), row(3), row(4), ALU.min)
                                tt2(row(9), row(5), row(6), ALU.max)
                                tt2(row(10), row(8), row(9), ALU.max)
                                tt2(row(11), row(7), row(10), ALU.add)
                                dv(lambda: V.tensor_reduce(out=gmax, in_=row(11), axis=mybir.AxisListType.X, op=ALU.max))
                                dv(lambda: V.tensor_scalar(out=row(12), in0=row(11), scalar1=gmax, scalar2=None, op0=ALU.is_equal))
                                dv(lambda: V.reciprocal(out=row(13), in_=row(11)))
                                tt2(row(14), row(12), row(13), ALU.mult)
                                cw3 = cw.rearrange("p (g j) -> p g j", j=4)
                                top2b = row(10).rearrange("p (g o) -> p g o", o=1).to_broadcast([128, 4, 4])
                                facb = row(14).rearrange("p (g o) -> p g o", o=1).to_broadcast([128, 4, 4])
                                tt2(cw3, e3, top2b, ALU.is_ge)
                                tt2(cw3, cw3, e3, ALU.mult)
                                tt2(cw3, cw3, facb, ALU.mult)
                                S.op("pe", lambda cw=cw, ti=ti: P.transpose(pscT[:, ti * 128:(ti + 1) * 128], cw, ident[:]),
                                     R=[rt, ident], W=[pscT])
                            S.act(comb[:, t0:t1], pscT[:, 0:n], AF.Identity, R=[pscT], W=[(comb, gi)])
                        S.barrier()

                    if debug == ("comb", b, l):
                        S.dma("sp", dbg_d[0:N_EXP, 0, :], comb[:, :], comb, R=[comb])
                        S.barrier()
                        return nc

                    wts = [[S.sb(ph, f"wg{i}", [128, KC, D_EXP], BF16), S.sb(ph, f"wu{i}", [128, KC, D_EXP], BF16),
                            S.sb(ph, f"wd{i}", [128, 4, D], BF16)] for i in range(2)]
                    cm = [S.sb(ph, f"cm{i}", [N_EXP, 512], F32) for i in range(2)]
                    cbc = [S.sb(ph, f"cbc{i}", [128, 512], F32) for i in range(2)]
                    sg = [S.sb(ph, f"sg{i}", [128, 512], F32) for i in range(2)]
                    hid = [S.sb(ph, f"hid{i}", [128, 4, 512], BF16) for i in range(2)]
                    psg = [S.ps(ph, f"psg{i}", [128, 512], F32) for i in range(2)]
                    psu = [S.ps(ph, f"psu{i}", [128, 512], F32) for i in range(2)]
                    psd = [S.ps(ph, f"psd{i}", [128, 512], F32) for i in range(3)]
                    psc = S.ps(ph, "psc", [128, 512], F32)

                    def load_expert(e):
                        wgt, wut, wdt = wts[e % 2]
                        S.dma("pool", wgt[:], w_gate_d[l, e].rearrange("(k p) n -> p k n", p=128), wgt, W=[wgt])
                        S.dma("pool", wut[:], w_up_d[l, e].rearrange("(k p) n -> p k n", p=128), wut, W=[wut])
                        S.dma("pool", wdt[:], w_down_d[l, e].rearrange("(k p) n -> p k n", p=128), wdt, W=[wdt])

                    seq = [(e, gi) for e in range(N_EXP) for gi in range(len(groups_out))]

                    def prep_dve(it):
                        e, gi = seq[it]
                        t0, t1 = groups_out[gi]
                        n = t1 - t0
                        cmt = cm[it % 2]
                        S.op("dve", lambda cmt=cmt, t0=t0, t1=t1, n=n, e=e: V.tensor_scalar(
                            out=cmt[:, 0:n], in0=comb[:, t0:t1], scalar1=ident[0:N_EXP, e:e + 1], scalar2=None,
                            op0=ALU.mult), R=[(comb, gi), ident], W=[cmt])

                    def prep_pe(it):
                        e, gi = seq[it]
                        t0, t1 = groups_out[gi]
                        n = t1 - t0
                        cb, cmt = cbc[it % 2], cm[it % 2]
                        S.mm(psc[:, 0:n], onesf[0:N_EXP, :], cmt[:, 0:n], True, True, R=[onesf, cmt], W=[psc])
                        S.act(cb[:, 0:n], psc[:, 0:n], AF.Identity, R=[psc], W=[cb])

                    load_expert(0)
                    prep_dve(0)
                    prep_pe(0)
                    for it, (e, gi) in enumerate(seq):
                        wgt, wut, wdt = wts[e % 2]
                        if gi == 0 and e + 1 < N_EXP:
                            load_expert(e + 1)
                        if it + 1 < len(seq):
                            prep_dve(it + 1)
                        t0, t1 = groups_out[gi]
                        n = t1 - t0
                        r = 2 if t0 < CTX else b
                        cb, hd = cbc[it % 2], hid[it % 2]
                        for hc in range(4):
                            pg, pu, sgt = psg[hc % 2], psu[hc % 2], sg[hc % 2]
                            for k in range(KC):
                                S.mm(pg[:, 0:n], wgt[:, k, hc * 128:(hc + 1) * 128], hT[:, k, t0:t1], start=(k == 0),
                                     stop=(k == KC - 1), R=[wgt, (hT, None)], W=[pg])
                            for k in range(KC):
                                S.mm(pu[:, 0:n], wut[:, k, hc * 128:(hc + 1) * 128], hT[:, k, t0:t1], start=(k == 0),
                                     stop=(k == KC - 1), R=[wut, (hT, None)], W=[pu])
                            S.act(sgt[:, 0:n], pg[:, 0:n], AF.Silu, R=[pg], W=[sgt])
                            S.op("dve", lambda sgt=sgt, cb=cb, n=n: V.tensor_tensor(
                                out=sgt[:, 0:n], in0=sgt[:, 0:n], in1=cb[:, 0:n], op=ALU.mult), R=[sgt, cb], W=[sgt])
                            S.op("dve", lambda hd=hd, hc=hc, pu=pu, sgt=sgt, n=n: V.tensor_tensor(
                                out=hd[:, hc, 0:n], in0=pu[:, 0:n], in1=sgt[:, 0:n], op=ALU.mult),
                                R=[pu, sgt], W=[(hd, hc)])
                            if hc == 1 and it + 1 < len(seq):
                                prep_pe(it + 1)
                        for oc in range(KC):
                            pd = psd[oc % 3]
                            for hc in range(4):
                                S.mm(pd[:, 0:n], wdt[:, hc, oc * 128:(oc + 1) * 128], hd[:, hc, 0:n], start=(hc == 0),
                                     stop=(hc == 3), R=[wdt, (hd, hc)], W=[pd])
                            S.op("dve", lambda oc=oc, t0=t0, t1=t1, n=n, r=r, pd=pd: V.scalar_tensor_tensor(
                                out=xT[:, oc, t0:t1], in0=pd[:, 0:n], scalar=mv(l, r, 5, oc), in1=xT[:, oc, t0:t1],
                                op0=ALU.mult, op1=ALU.add), R=[pd, (xT, gi), modv], W=[(xT, gi)])
                    S.barrier()

                if debug == ("xmoe", b, l):
                    _dump(S, nc, dbg_d, xT, es)
                    return nc

            with ExitStack() as ph:
                sq = S.sb(ph, "sq", [128, 2, 512], BF16)
                rstd = S.sb(ph, "rstd", [128, T], F32)
                xn = [S.sb(ph, f"xn{i}", [128, KC, 512], F32) for i in range(1)]
                ost = [S.sb(ph, f"ost{i}", [128, D], F32) for i in range(2)]
                psn = S.ps(ph, "psn", [128, 512], F32)
                ptr = [S.ps(ph, f"ptr{i}", [128, 512], F32) for i in range(4)]
                def fstats(gi):
                    rms_rstd(ph, xT, list(range(KC)), [GROUPS_LAT[gi]], 1.0 / D, "f", psn, sq, rstd, gis=[gi])
                fstats(0)
                oi = 0
                for gi, (t0, t1) in enumerate(GROUPS_LAT):
                    n = t1 - t0
                    xnb = xn[0]
                    if gi + 1 < len(GROUPS_LAT):
                        fstats(gi + 1)
                    for k in range(KC):
                        S.op("dve", lambda k=k, t0=t0, t1=t1, n=n, xnb=xnb: V.scalar_tensor_tensor(
                            out=xnb[:, k, 0:n], in0=xT[:, k, t0:t1], scalar=ppv("g_fin", k), in1=rstd[:, t0:t1],
                            op0=ALU.mult, op1=ALU.mult), R=[(xT, None), (rstd, gi), ppt], W=[(xnb, None)])
                    for ti in range(n // 128):
                        ot = ost[oi % 2]
                        for hf in range(2):
                            pt = ptr[(oi * 2 + hf) % 4]
                            for kk in range(4):
                                k = hf * 4 + kk
                                S.op("pe", lambda pt=pt, kk=kk, k=k, ti=ti, xnb=xnb: P.transpose(
                                    pt[:, kk * 128:(kk + 1) * 128], xnb[:, k, ti * 128:(ti + 1) * 128], ident[:]),
                                    R=[(xnb, None), ident], W=[pt])
                            if hf == 0:
                                S.act(ot[:, 0:512], pt[:], AF.Identity, R=[pt], W=[(ot, 0)])
                            else:
                                S.op("dve", lambda ot=ot, pt=pt: V.tensor_copy(out=ot[:, 512:1024], in_=pt[:]), R=[pt], W=[(ot, 1)])
                        row0 = t0 - CTX + ti * 128
                        S.dma("sp", out_d[b, row0:row0 + 128, :], ot[:], ot, R=[ot])
                        oi += 1
                S.barrier()

    return nc


def _finish(S, nc, bufs):
    S.barrier()


def _dump(S, nc, dbg_d, buf, es):
    with ExitStack() as ph:
        st = S.sb(ph, "dbgst", [128, T], F32)
        for k in range(KC):
            S.op("dve", lambda k=k: nc.vector.tensor_copy(out=st[:], in_=buf[:, k, :]), R=[buf, st], W=[st])
            S.dma("sp", dbg_d[:, k, :], st[:], st, R=[st])
        S.barrier()


_CACHE = {}


def _prep_shared(inp):
    pp = _pack_params(inp)
    shared = {
        "pp": pp.pack(),
        "w_ada": np.ascontiguousarray(inp["w_ada"], np.float32),
        "w_in": np.ascontiguousarray(inp["w_in"], np.float32),
        "wbd": _wbd(np.asarray(inp["rg_w"], np.float32)),
        "bpat": _build_bias_patterns(np.asarray(inp["na_rpb"], np.float32)),
        "w_out": np.ascontiguousarray(inp["w_out"], np.float32),
        "w_router": np.ascontiguousarray(inp["w_router"], np.float32),
        "b_router": np.ascontiguousarray(inp["b_router"], np.float32).reshape(1, N_EXP),
        "w_gate": np.ascontiguousarray(inp["w_gate"], np.float32),
        "w_up": np.ascontiguousarray(inp["w_up"], np.float32),
        "w_down": np.ascontiguousarray(inp["w_down"], np.float32),
        "ident": np.eye(128, dtype=np.float32),
    }
    return pp, shared


def _core_inputs(inp, shared, core, nb):
    b0 = core * nb
    c = np.asarray(inp["c"], np.float32)[b0:b0 + nb]
    rows = np.concatenate([c, np.asarray(inp["c_ctx"], np.float32)[None]], axis=0)
    if nb == 1:
        rows = np.concatenate([rows[:1], rows[:1], rows[1:]], axis=0)
    condT = np.ascontiguousarray(rows.reshape(3, KC, 128).transpose(2, 1, 0))
    m = dict(shared)
    m["x"] = np.ascontiguousarray(np.asarray(inp["x"], np.float32)[b0:b0 + nb])
    m["ctx"] = np.ascontiguousarray(np.asarray(inp["ctx"], np.float32)[b0:b0 + nb])
    m["condT"] = condT
    return m


def kernel(**inputs):
    inp = {k: np.asarray(v) for k, v in inputs.items()}
    nb = inp["x"].shape[0] // N_CORES
    pp, shared = _prep_shared(inp)
    nc = build_program(pp.off, pp.n, nb=nb)
    in_maps = [_core_inputs(inp, shared, core, nb) for core in range(N_CORES)]
    res = run_bass_kernel_spmd(nc, in_maps, core_ids=list(range(N_CORES)))
    out = np.concatenate([np.asarray(r["out"]) for r in res.results], axis=0)
    return out.astype(np.float32)
```
